# Optimizing a Trainium2 kernel written in Bass

```python
import jax
import jax.numpy as jnp
from jax import lax
import numpy as np

D_MODEL = 4096
BATCH = 2
SEQ = 4096
DEPTH = 2

GRID_W = 64
CTX_LEN = 256
POS_BASE = 10000.0
N_MOD = 6

ALPHA = (2 * DEPTH) ** 0.25
BETA = (8 * DEPTH) ** -0.25
LN_EPS = 1e-5

A_WIDTH = D_MODEL // 2
A_HEAD_DIM = 64
A_HEADS = A_WIDTH // A_HEAD_DIM
DECAY_LORA = 96
ICL_LORA = 96
GATE_LORA = 256
LNX_EPS = 64e-5
RWKV_SIZES = (A_WIDTH, A_WIDTH, A_WIDTH, DECAY_LORA, DECAY_LORA, ICL_LORA, ICL_LORA, GATE_LORA)
RWKV_SPLITS = tuple(int(s) for s in np.cumsum(RWKV_SIZES)[:-1])
RWKV_COLS = int(sum(RWKV_SIZES))

B_WIDTH = D_MODEL // 2
LRU_BLOCKS = 16
LRU_BLOCK_W = B_WIDTH // LRU_BLOCKS
CONV_W = 4
CONV_LO = 1
RGLRU_C = 8.0
EVEN_IN = RWKV_COLS + 2 * B_WIDTH
EVEN_MIX = A_WIDTH + B_WIDTH

C_HEADS = 8
C_QK_HEAD = 256
C_V_HEAD = 512
C_QK_W = C_HEADS * C_QK_HEAD
C_V_W = C_HEADS * C_V_HEAD
MLSTM_CHUNK = 64
GATE_CAP = 15.0
MLSTM_NORM_EPS = 1e-6
QO_COLS = C_QK_W + C_V_W
ODD_IN = QO_COLS + C_QK_W + C_V_W + 4 * C_HEADS

N_EXPERTS = 16
N_GROUPS = 4
EXPERTS_PER_GROUP = N_EXPERTS // N_GROUPS
GROUP_SCORE_TOPK = 2
TOP_K = 2
D_EXPERT = 1024
MOE_BLOCK = 256

kernel_name = "hybrid_rwkv7_rglru_mlstm_moe_dit"


def layer_norm(x, g, b, eps=LN_EPS):
    xf = x.astype(jnp.float32)
    xc = xf - xf.mean(-1, keepdims=True)
    var = jnp.mean(xc * xc, -1, keepdims=True)
    return (xc * lax.rsqrt(var + eps) * g + b).astype(x.dtype)


def head_norm(y, n_heads, g, b, eps):
    shp = y.shape
    yh = y.astype(jnp.float32).reshape(shp[:-1] + (n_heads, shp[-1] // n_heads))
    yc = yh - yh.mean(-1, keepdims=True)
    var = jnp.mean(yc * yc, -1, keepdims=True)
    return (yc * lax.rsqrt(var + eps)).reshape(shp) * g + b


def adaln(cvec, w, b):
    return jax.nn.silu(cvec) @ w + b


def modulate(x, shift, scale):
    return x * (1.0 + scale) + shift


def softcap(x):
    return GATE_CAP * jnp.tanh(x / GATE_CAP)


def sincos_2d(n_tokens, dim):
    rows = n_tokens // GRID_W
    row = jnp.repeat(jnp.arange(rows, dtype=jnp.float32), GRID_W)
    col = jnp.tile(jnp.arange(GRID_W, dtype=jnp.float32), rows)
    quarter = dim // 4
    omega = POS_BASE ** (-jnp.arange(quarter, dtype=jnp.float32) / quarter)
    ang_r = row[:, None] * omega[None, :]
    ang_c = col[:, None] * omega[None, :]
    return jnp.concatenate([jnp.sin(ang_r), jnp.cos(ang_r), jnp.sin(ang_c), jnp.cos(ang_c)], -1)


def token_shift(p, mu):
    pad = jnp.pad(p, ((0, 0), (1, 1), (0, 0)))
    return p + mu * (0.5 * (pad[:, :-2] + pad[:, 2:]) - p)


def dwconv_centred(x, w, b):
    s_len = x.shape[1]
    pad = jnp.pad(x, ((0, 0), (CONV_LO, CONV_W - 1 - CONV_LO), (0, 0)))
    out = b
    for j in range(CONV_W):
        out = out + pad[:, j:j + s_len] * w[j]
    return out


def rwkv_scan(r, w, k, v, a, b, s0, reverse):
    def step(s, inp):
        r_t, w_t, k_t, v_t, a_t, b_t = inp
        sa = jnp.einsum('bhvk,bhk->bhv', s, a_t)
        s = s * w_t[:, :, None, :] + sa[..., None] * b_t[:, :, None, :] + v_t[..., None] * k_t[:, :, None, :]
        return s, jnp.einsum('bhvk,bhk->bhv', s, r_t)
    xs = tuple(jnp.swapaxes(t, 0, 1) for t in (r, w, k, v, a, b))
    s_fin, ys = lax.scan(step, s0, xs, reverse=reverse)
    return jnp.swapaxes(ys, 0, 1), s_fin


def rglru_dir(xc, wa, ba, wx, bx, lam, h0, reverse):
    bsz, s_len, width = xc.shape
    xg = xc.reshape(bsz, s_len, LRU_BLOCKS, LRU_BLOCK_W)
    gate_r = jax.nn.sigmoid(jnp.einsum('bsnc,ncd->bsnd', xg, wa).reshape(bsz, s_len, width) + ba)
    gate_i = jax.nn.sigmoid(jnp.einsum('bsnc,ncd->bsnd', xg, wx).reshape(bsz, s_len, width) + bx)
    log_a = RGLRU_C * gate_r * jax.nn.log_sigmoid(lam)
    a = jnp.exp(log_a)
    u = xc * gate_i * jnp.sqrt(-jnp.expm1(2.0 * log_a))
    first = s_len - 1 if reverse else 0
    u = u.at[:, first].add(a[:, first] * h0)
    _, hs = lax.associative_scan(lambda l, r: (l[0] * r[0], r[0] * l[1] + r[1]), (a, u), axis=1, reverse=reverse)
    last = 0 if reverse else s_len - 1
    return hs, hs[:, last]


def to_chunks(t):
    bsz, s_len, n_h = t.shape[:3]
    t = t.reshape((bsz, s_len // MLSTM_CHUNK, MLSTM_CHUNK, n_h) + t.shape[3:])
    return jnp.moveaxis(jnp.moveaxis(t, 1, 0), 3, 2)


def from_chunks(t):
    t = jnp.moveaxis(jnp.moveaxis(t, 2, 3), 0, 1)
    return t.reshape((t.shape[0], t.shape[1] * t.shape[2]) + t.shape[3:])


def mlstm_chunkwise(q, k, v, ig, lf, state0, reverse):
    if reverse:
        k, v, ig, lf = (jnp.flip(t, 1) for t in (k, v, ig, lf))
        q = None if q is None else jnp.flip(q, 1)
    tri = jnp.tril(jnp.ones((MLSTM_CHUNK, MLSTM_CHUNK), dtype=bool))

    def step(carry, inp):
        c_st, n_st, m_st = carry
        kc, vc, igc, lfc = inp[:4]
        b = jnp.cumsum(lfc, axis=-1)
        carry_log = b[..., -1] + m_st
        w_in = b[..., -1:] - b + igc
        m_new = jnp.maximum(carry_log, w_in.max(-1))
        ew = jnp.exp(w_in - m_new[..., None])
        ec = jnp.exp(carry_log - m_new)
        c_new = ec[..., None, None] * c_st + jnp.einsum('bhl,bhlk,bhlv->bhkv', ew, kc, vc)
        n_new = ec[..., None] * n_st + jnp.einsum('bhl,bhlk->bhk', ew, kc)
        if len(inp) == 4:
            return (c_new, n_new, m_new), None
        qc = inp[4]
        dmat = jnp.where(tri, b[..., :, None] - b[..., None, :] + igc[..., None, :], -jnp.inf)
        inter = b + m_st[..., None]
        m_t = jnp.maximum(inter, dmat.max(-1))
        scores = jnp.einsum('bhtk,bhsk->bhts', qc, kc) * jnp.exp(dmat - m_t[..., None])
        e_inter = jnp.exp(inter - m_t)
        num = e_inter[..., None] * jnp.einsum('bhtk,bhkv->bhtv', qc, c_st) + jnp.einsum('bhts,bhsv->bhtv', scores, vc)
        den = e_inter * jnp.einsum('bhtk,bhk->bht', qc, n_st) + scores.sum(-1)
        h = num / jnp.maximum(jnp.abs(den), jnp.exp(-m_t))[..., None]
        return (c_new, n_new, m_new), h

    xs = tuple(to_chunks(t) for t in (k, v, ig, lf)) + (() if q is None else (to_chunks(q),))
    fin, hs = lax.scan(step, state0, xs)
    if q is None:
        return None, fin
    h = from_chunks(hs)
    return (jnp.flip(h, 1) if reverse else h), fin


def even_zero_state(bsz):
    s = jnp.zeros((bsz, A_HEADS, A_HEAD_DIM, A_HEAD_DIM), jnp.float32)
    h = jnp.zeros((bsz, B_WIDTH), jnp.float32)
    return (s, s, h, h)


def odd_zero_state(bsz):
    one = (jnp.zeros((bsz, C_HEADS, C_QK_HEAD, C_V_HEAD), jnp.float32),
           jnp.zeros((bsz, C_HEADS, C_QK_HEAD), jnp.float32),
           jnp.zeros((bsz, C_HEADS), jnp.float32))
    return (one, one)


def even_seq(h, state, prm):
    (w_in, w_out, mu, w0, w2, a0, a2, g2, k_k, k_a, r_k, lnx_w, lnx_b,
     conv_w, conv_b, wa, ba, wx, bx, lam) = prm
    bsz, s_len, _ = h.shape
    heads = lambda t: t.reshape(bsz, s_len, A_HEADS, A_HEAD_DIM)
    p = h @ w_in
    rw = token_shift(p[..., :RWKV_COLS], mu).astype(jnp.float32)
    r, k, v, wd_f, wd_b, ad_f, ad_b, gd = jnp.split(rw, RWKV_SPLITS, axis=-1)
    kk = heads(k * k_k)
    kk = kk * lax.rsqrt(jnp.maximum(jnp.sum(kk * kk, -1, keepdims=True), 1e-24))
    rh, vh = heads(r), heads(v)
    y_scan, bonus, finals = 0.0, 0.0, []
    for d, (wd, ad) in enumerate(((wd_f, ad_f), (wd_b, ad_b))):
        log_w = -jax.nn.softplus(-(w0[d] + jnp.tanh(wd) @ w2[d])) - 0.5
        icl = jax.nn.sigmoid(a0[d] + ad @ a2[d])
        kd = heads(k * (1.0 + (icl - 1.0) * k_a))
        y_d, s_fin = rwkv_scan(rh, heads(jnp.exp(-jnp.exp(log_w))), kd, vh, -kk, kk * heads(icl), state[d], d == 1)
        y_scan = y_scan + y_d
        bonus = bonus + jnp.sum(rh * kd * r_k, -1, keepdims=True) * vh
        finals.append(s_fin)
    g = jax.nn.sigmoid(gd) @ g2
    y_a = (head_norm(y_scan.reshape(bsz, s_len, A_WIDTH), A_HEADS, lnx_w, lnx_b, LNX_EPS)
           + bonus.reshape(bsz, s_len, A_WIDTH)) * g
    xb = p[..., RWKV_COLS:RWKV_COLS + B_WIDTH]
    gb = p[..., RWKV_COLS + B_WIDTH:]
    xc = dwconv_centred(xb, conv_w, conv_b).astype(jnp.float32)
    y_lru = 0.0
    for d in range(2):
        h_d, fin = rglru_dir(xc, wa[d], ba[d], wx[d], bx[d], lam[d], state[2 + d], d == 1)
        y_lru = y_lru + h_d
        finals.append(fin)
    y_b = y_lru * jax.nn.gelu(gb.astype(jnp.float32))
    y = jnp.concatenate([y_a, y_b], -1).astype(h.dtype) @ w_out
    return y, tuple(finals)


def odd_seq(h, state, prm, with_out):
    w_in, w_out, ig_b, fg_b, norm_w, norm_b = prm
    bsz, s_len, _ = h.shape
    if with_out:
        p = h @ w_in
        q = p[..., :C_QK_W].astype(jnp.float32).reshape(bsz, s_len, C_HEADS, C_QK_HEAD) * C_QK_HEAD ** -0.5
        o = p[..., C_QK_W:QO_COLS]
        p_kv = p[..., QO_COLS:]
    else:
        q = None
        p_kv = h @ w_in[:, QO_COLS:]
    p_kv = p_kv.astype(jnp.float32)
    k = p_kv[..., :C_QK_W].reshape(bsz, s_len, C_HEADS, C_QK_HEAD)
    v = p_kv[..., C_QK_W:C_QK_W + C_V_W].reshape(bsz, s_len, C_HEADS, C_V_HEAD)
    gp = p_kv[..., C_QK_W + C_V_W:].reshape(bsz, s_len, 2, 2, C_HEADS)
    outs, finals = [], []
    for d in range(2):
        ig = softcap(gp[:, :, d, 0] + ig_b[d])
        lf = jax.nn.log_sigmoid(softcap(gp[:, :, d, 1] + fg_b[d]))
        h_d, fin = mlstm_chunkwise(q, k, v, ig, lf, state[d], d == 1)
        outs.append(h_d)
        finals.append(fin)
    if not with_out:
        return None, tuple(finals)
    h_mix = (outs[0] + outs[1]).reshape(bsz, s_len, C_V_W)
    y = head_norm(h_mix, C_HEADS, norm_w, norm_b, MLSTM_NORM_EPS) * jax.nn.sigmoid(o.astype(jnp.float32))
    return y.astype(h.dtype) @ w_out, tuple(finals)


def moe_ffn(h, router_w, router_b, w_gate, w_up, w_down):
    n_tok, dim = h.shape
    aff = jax.nn.sigmoid(h.astype(jnp.float32) @ router_w.astype(jnp.float32))
    biased = aff + router_b.astype(jnp.float32)
    group_score = lax.top_k(biased.reshape(n_tok, N_GROUPS, EXPERTS_PER_GROUP), GROUP_SCORE_TOPK)[0].sum(-1)
    best_group = jnp.argmax(group_score, axis=-1)
    in_group = (jnp.arange(N_EXPERTS) // EXPERTS_PER_GROUP)[None, :] == best_group[:, None]
    _, expert_idx = lax.top_k(jnp.where(in_group, biased, -jnp.inf), TOP_K)
    sel = jnp.take_along_axis(aff, expert_idx, axis=1)
    gates = sel / jnp.sum(sel, -1, keepdims=True)
    n_slots = n_tok * TOP_K
    flat_e = expert_idx.reshape(n_slots)
    order = jnp.argsort(flat_e)
    e_sorted = flat_e[order]
    tok_sorted = order // TOP_K
    counts = jnp.bincount(flat_e, length=N_EXPERTS)
    padded = (counts + MOE_BLOCK - 1) // MOE_BLOCK * MOE_BLOCK
    pad_end = jnp.cumsum(padded)
    pad_start = pad_end - padded
    start = jnp.cumsum(counts) - counts
    dest = pad_start[e_sorted] + jnp.arange(n_slots) - start[e_sorted]
    n_blocks = -(-n_slots // MOE_BLOCK) + N_EXPERTS
    buf = jnp.zeros((n_blocks * MOE_BLOCK, dim), h.dtype).at[dest].set(h[tok_sorted])
    block_e = jnp.minimum(jnp.searchsorted(pad_end, jnp.arange(n_blocks) * MOE_BLOCK, side='right'), N_EXPERTS - 1)

    def expert_block(args):
        xb, e = args
        return (jax.nn.silu(xb @ w_gate[e]) * (xb @ w_up[e])) @ w_down[e]

    yb = lax.map(expert_block, (buf.reshape(n_blocks, MOE_BLOCK, dim), block_e))
    y_sorted = yb.reshape(n_blocks * MOE_BLOCK, dim)[dest].astype(jnp.float32)
    out = jnp.zeros((n_tok, dim), jnp.float32).at[tok_sorted].add(y_sorted * gates.reshape(n_slots)[order][:, None])
    return out.astype(h.dtype)


def setup_inputs(seed: int = 0) -> dict:
    key = jax.random.key(seed)
    keys = jax.random.split(key, 48)
    ki = iter(range(48))

    def nrm(shape, scale):
        return jax.random.normal(keys[next(ki)], shape, jnp.float32) * scale

    def uni(shape, lo, hi):
        return jax.random.uniform(keys[next(ki)], shape, jnp.float32, lo, hi)

    D = D_MODEL
    n_even = (DEPTH + 1) // 2
    n_odd = DEPTH // 2
    inp = {}
    inp["x"] = nrm((BATCH, SEQ, D), 1.0)
    inp["c"] = nrm((BATCH, D), 1.0)
    inp["ctx"] = nrm((BATCH, CTX_LEN, D), 1.0)
    inp["c_ctx"] = nrm((D,), 1.0)
    inp["ada_w"] = nrm((DEPTH, D, N_MOD * D), 0.5 * D ** -0.5)
    inp["ada_b"] = nrm((DEPTH, N_MOD * D), 0.02)
    inp["norm_g"] = 1.0 + nrm((DEPTH, 2, D), 0.02)
    inp["norm_b"] = nrm((DEPTH, 2, D), 0.02)
    inp["ev_w_in"] = nrm((n_even, D, EVEN_IN), D ** -0.5)
    inp["ev_w_out"] = nrm((n_even, EVEN_MIX, D), BETA * EVEN_MIX ** -0.5)
    inp["rwkv_mu"] = uni((n_even, RWKV_COLS), 0.0, 1.0)
    inp["rwkv_w0"] = uni((n_even, 2, A_WIDTH), -6.0, -1.0)
    inp["rwkv_w2"] = nrm((n_even, 2, DECAY_LORA, A_WIDTH), 0.5 * DECAY_LORA ** -0.5)
    inp["rwkv_a0"] = nrm((n_even, 2, A_WIDTH), 0.1)
    inp["rwkv_a2"] = nrm((n_even, 2, ICL_LORA, A_WIDTH), ICL_LORA ** -0.5)
    inp["rwkv_g2"] = nrm((n_even, GATE_LORA, A_WIDTH), GATE_LORA ** -0.5)
    inp["rwkv_k_k"] = 1.0 + nrm((n_even, A_WIDTH), 0.1)
    inp["rwkv_k_a"] = 1.0 + nrm((n_even, A_WIDTH), 0.1)
    inp["rwkv_r_k"] = nrm((n_even, A_HEADS, A_HEAD_DIM), 0.1)
    inp["rwkv_lnx_w"] = 1.0 + nrm((n_even, A_WIDTH), 0.02)
    inp["rwkv_lnx_b"] = nrm((n_even, A_WIDTH), 0.02)
    inp["lru_conv_w"] = nrm((n_even, CONV_W, B_WIDTH), CONV_W ** -0.5)
    inp["lru_conv_b"] = nrm((n_even, B_WIDTH), 0.02)
    inp["lru_wa"] = nrm((n_even, 2, LRU_BLOCKS, LRU_BLOCK_W, LRU_BLOCK_W), LRU_BLOCK_W ** -0.5)
    inp["lru_ba"] = nrm((n_even, 2, B_WIDTH), 0.02)
    inp["lru_wx"] = nrm((n_even, 2, LRU_BLOCKS, LRU_BLOCK_W, LRU_BLOCK_W), LRU_BLOCK_W ** -0.5)
    inp["lru_bx"] = nrm((n_even, 2, B_WIDTH), 0.02)
    sig_l = uni((n_even, 2, B_WIDTH), 0.9, 0.999) ** (1.0 / RGLRU_C)
    inp["lru_lam"] = jnp.log(sig_l) - jnp.log1p(-sig_l)
    inp["od_w_in"] = nrm((n_odd, D, ODD_IN), D ** -0.5)
    inp["od_w_out"] = nrm((n_odd, C_V_W, D), BETA * C_V_W ** -0.5)
    inp["mlstm_ig_b"] = nrm((n_odd, 2, C_HEADS), 0.1)
    inp["mlstm_fg_b"] = uni((n_odd, 2, C_HEADS), 3.0, 6.0)
    inp["mlstm_norm_w"] = 1.0 + nrm((n_odd, C_V_W), 0.02)
    inp["mlstm_norm_b"] = nrm((n_odd, C_V_W), 0.02)
    inp["router_w"] = nrm((D, N_EXPERTS), D ** -0.5)
    inp["router_b"] = nrm((N_EXPERTS,), 0.01)
    inp["moe_w_gate"] = nrm((DEPTH, N_EXPERTS, D, D_EXPERT), D ** -0.5)
    inp["moe_w_up"] = nrm((DEPTH, N_EXPERTS, D, D_EXPERT), D ** -0.5)
    inp["moe_w_down"] = nrm((DEPTH, N_EXPERTS, D_EXPERT, D), BETA * D_EXPERT ** -0.5)
    return inp


def reference(x, c, ctx, c_ctx, ada_w, ada_b, norm_g, norm_b,
              ev_w_in, ev_w_out, rwkv_mu, rwkv_w0, rwkv_w2, rwkv_a0, rwkv_a2, rwkv_g2,
              rwkv_k_k, rwkv_k_a, rwkv_r_k, rwkv_lnx_w, rwkv_lnx_b,
              lru_conv_w, lru_conv_b, lru_wa, lru_ba, lru_wx, lru_bx, lru_lam,
              od_w_in, od_w_out, mlstm_ig_b, mlstm_fg_b, mlstm_norm_w, mlstm_norm_b,
              router_w, router_b, moe_w_gate, moe_w_up, moe_w_down):
    bsz, s_len, dim = x.shape
    n_ctx = ctx.shape[1]
    lat = x + sincos_2d(s_len, dim).astype(x.dtype)
    cx = ctx
    for layer in range(DEPTH):
        last = layer == DEPTH - 1
        m_lat = jnp.split(adaln(c, ada_w[layer], ada_b[layer])[:, None, :], N_MOD, axis=-1)
        n_ctx_mod = 2 if last else N_MOD
        m_ctx = jnp.split(adaln(c_ctx, ada_w[layer, :, :n_ctx_mod * dim], ada_b[layer, :n_ctx_mod * dim]),
                          n_ctx_mod, axis=-1)
        h_lat = modulate(lat, m_lat[0], m_lat[1])
        h_ctx = modulate(cx, m_ctx[0], m_ctx[1])
        i = layer // 2
        if layer % 2 == 0:
            prm = (ev_w_in[i], ev_w_out[i], rwkv_mu[i], rwkv_w0[i], rwkv_w2[i], rwkv_a0[i], rwkv_a2[i],
                   rwkv_g2[i], rwkv_k_k[i], rwkv_k_a[i], rwkv_r_k[i], rwkv_lnx_w[i], rwkv_lnx_b[i],
                   lru_conv_w[i], lru_conv_b[i], lru_wa[i], lru_ba[i], lru_wx[i], lru_bx[i], lru_lam[i])
            y_ctx, ctx_state = even_seq(h_ctx, even_zero_state(bsz), prm)
            y_lat, _ = even_seq(h_lat, ctx_state, prm)
        else:
            prm = (od_w_in[i], od_w_out[i], mlstm_ig_b[i], mlstm_fg_b[i], mlstm_norm_w[i], mlstm_norm_b[i])
            y_ctx, ctx_state = odd_seq(h_ctx, odd_zero_state(bsz), prm, not last)
            y_lat, _ = odd_seq(h_lat, ctx_state, prm, True)
        lat = layer_norm(ALPHA * lat + m_lat[2] * y_lat, norm_g[layer, 0], norm_b[layer, 0])
        moe_args = (router_w, router_b, moe_w_gate[layer], moe_w_up[layer], moe_w_down[layer])
        if last:
            f_lat = moe_ffn(modulate(lat, m_lat[3], m_lat[4]).reshape(-1, dim), *moe_args).reshape(lat.shape)
        else:
            cx = layer_norm(ALPHA * cx + m_ctx[2] * y_ctx, norm_g[layer, 0], norm_b[layer, 0])
            h_all = jnp.concatenate([modulate(cx, m_ctx[3], m_ctx[4]), modulate(lat, m_lat[3], m_lat[4])], axis=1)
            f_all = moe_ffn(h_all.reshape(-1, dim), *moe_args).reshape(h_all.shape)
            f_lat = f_all[:, n_ctx:]
            cx = layer_norm(ALPHA * cx + m_ctx[5] * f_all[:, :n_ctx], norm_g[layer, 1], norm_b[layer, 1])
        lat = layer_norm(ALPHA * lat + m_lat[5] * f_lat, norm_g[layer, 1], norm_b[layer, 1])
    return lat
```

```python
import contextlib
import numpy as np
import concourse.bass as bass
import concourse.mybir as mybir
from concourse.bass_utils import run_bass_kernel_spmd

F32 = mybir.dt.float32
BF16 = mybir.dt.bfloat16
AF = mybir.ActivationFunctionType
ALU = mybir.AluOpType
AX = mybir.AxisListType


class Tl:
    __slots__ = ("t", "key")

    def __init__(self, t, key):
        self.t = t
        self.key = key

    def __getitem__(self, idx):
        return self.t[idx]


class KB:
    ENG = ("pe", "act", "dve", "pool", "sp")

    def __init__(self):
        self.nc = bass.Bass("TRN2", target_bir_lowering=False)
        self.es = contextlib.ExitStack()
        self.es.enter_context(self.nc.allow_low_precision("bf16 matmul operands, fp32 accumulation"))
        nc = self.nc
        self.eng = {"pe": nc.tensor, "act": nc.scalar, "dve": nc.vector, "pool": nc.gpsimd, "sp": nc.sync}
        self.sem = {}
        self.cnt = {}
        for e in self.ENG:
            self.sem[e] = self.es.enter_context(nc.semaphore("s_" + e))
            self.cnt[e] = 0
        self.seen = {e: {} for e in self.ENG}
        self.dep = {}
        self.dsem = {}
        self.semobj = {}
        self.nuniq = 0
        self.stores = []
        self.free_dsem = []
        self.es_perm = self.es

    def sb(self, name, shape, dtype=F32):
        self.nalloc = getattr(self, "nalloc", 0) + 1
        name = "sb%d_%s" % (self.nalloc, name)
        t = self.es.enter_context(self.nc.sbuf_tensor(name, list(shape), dtype))
        return Tl(t, name)

    def ps(self, name, shape, dtype=F32):
        self.nalloc = getattr(self, "nalloc", 0) + 1
        name = "ps%d_%s" % (self.nalloc, name)
        t = self.es.enter_context(self.nc.psum_tensor(name, list(shape), dtype))
        return Tl(t, name)

    def dram(self, name, shape, dtype=F32, kind="Internal"):
        t = self.nc.dram_tensor(name, list(shape), dtype, kind=kind)
        return Tl(t.ap(), name)

    def _needs(self, e, r, w):
        waits = {}

        def need(tok, same_ok):
            if tok is None:
                return
            sname, val = tok
            if same_ok and sname == "s_" + e:
                return
            if self.seen[e].get(sname, 0) >= val:
                return
            if waits.get(sname, 0) < val:
                waits[sname] = val

        for t in r:
            d = self.dep.get(t.key)
            if d:
                need(d["w"], False)
        for t in w:
            d = self.dep.get(t.key)
            if d:
                need(d["w"], False)
                for sname, val in d["r"].items():
                    need((sname, val), False)
        for sname, val in waits.items():
            self.eng[e].wait_ge(self.semobj[sname], val)
            self.seen[e][sname] = val

    def _commit(self, tok, r, w):
        for t in r:
            d = self.dep.setdefault(t.key, {"w": None, "r": {}})
            if d["r"].get(tok[0], 0) < tok[1]:
                d["r"][tok[0]] = tok[1]
        for t in w:
            self.dep[t.key] = {"w": tok, "r": {}}

    def op(self, e, fn, r=(), w=()):
        self.semobj.setdefault("s_" + e, self.sem[e])
        self._needs(e, r, w)
        ins = fn(self.eng[e])
        self.cnt[e] += 1
        ins.then_inc(self.sem[e], 1)
        self._commit(("s_" + e, self.cnt[e]), r, w)

    def dma(self, q, out, in_, r=(), w=(), semkey=None, store=False):
        if semkey is None:
            semkey = (r[0].key if store else w[0].key)
        if semkey not in self.dsem:
            if self.free_dsem:
                self.dsem[semkey] = self.free_dsem.pop()
            else:
                nm = "d%d" % self.nuniq
                self.nuniq += 1
                s = self.es_perm.enter_context(self.nc.semaphore(nm))
                self.dsem[semkey] = [nm, 0]
                self.semobj[nm] = s
        ent = self.dsem[semkey]
        self._needs(q, r, w)
        pairs = out if isinstance(out, list) else [(out, in_)]
        for o, i in pairs:
            self.eng[q].dma_start(out=o, in_=i).then_inc(self.semobj[ent[0]], 16)
            ent[1] += 16
        tok = (ent[0], ent[1])
        self._commit(tok, r, w)
        if store:
            self.stores.append(tok)

    def dump(self, tl, name):
        shp = list(tl.t.shape)
        d = self.dram(name, shp, tl.t.dtype, kind="ExternalOutput")
        self.dma("sp", d[:], tl[:], r=[tl], store=True)

    def barrier(self):
        for e in self.ENG:
            for e2 in self.ENG:
                if e2 != e and self.cnt[e2] > self.seen[e].get("s_" + e2, 0):
                    self.semobj.setdefault("s_" + e2, self.sem[e2])
                    self.eng[e].wait_ge(self.sem[e2], self.cnt[e2])
                    self.seen[e]["s_" + e2] = self.cnt[e2]
            for key, (nm, val) in self.dsem.items():
                if val > self.seen[e].get(nm, 0):
                    self.eng[e].wait_ge(self.semobj[nm], val)
                    self.seen[e][nm] = val
        self.dep = {}

    @contextlib.contextmanager
    def scope(self):
        saved = self.es
        self.es = contextlib.ExitStack()
        keys_before = set(self.dsem.keys())
        try:
            yield
        finally:
            self.barrier()
            for k in list(self.dsem.keys()):
                if k not in keys_before:
                    self.free_dsem.append(self.dsem.pop(k))
            self.es.close()
            self.es = saved

    def finish(self):
        last = {}
        for sname, val in self.stores:
            last[sname] = max(last.get(sname, 0), val)
        for sname, val in last.items():
            self.eng["sp"].wait_ge(self.semobj[sname], val)
        self.es.close()
        return self.nc


_TRACE = False
_TIMES = []


def _run(nc, in_maps):
    if _TRACE:
        res = run_bass_kernel_spmd(nc, in_maps, core_ids=list(range(len(in_maps))), trace=True)
        _TIMES.append(res.exec_time_ns)
    else:
        res = run_bass_kernel_spmd(nc, in_maps, core_ids=list(range(len(in_maps))))
    return res.results


def build_p0(D=4096, NCOL=3072, NV=3, CW=512):
    kb = KB()
    nc = kb.nc
    KC = D // 128
    cvT = kb.dram("cvT", [128, KC, NV], kind="ExternalInput")
    W = kb.dram("w", [D, NCOL], kind="ExternalInput")
    bias = kb.dram("b", [1, NCOL], kind="ExternalInput")
    out = kb.dram("out", [NV, NCOL], kind="ExternalOutput")
    cv = kb.sb("cv", [128, KC, NV])
    cs = kb.sb("cs", [128, KC, NV])
    bt = kb.sb("bt", [NV, NCOL])
    ot = kb.sb("ot", [NV, NCOL])
    NB = 3
    wt = [kb.sb("wt%d" % i, [128, 8, CW]) for i in range(NB)]
    pss = [kb.ps("ps%d" % i, [128, CW]) for i in range(2)]
    kb.dma("sp", cv[:], cvT[:], w=[cv])
    kb.dma("sp", [(bt[v:v + 1, :], bias[:]) for v in range(NV)], None, w=[bt])
    kb.op("act", lambda e: e.activation(out=cs[:], in_=cv[:], func=AF.Silu), r=[cv], w=[cs])
    Wv = W[:].rearrange("(c p) n -> p c n", p=128)
    nblk = NCOL // CW
    li = 0
    for j in range(nblk):
        p = pss[j % 2]
        for c8 in range(KC // 8):
            wb = wt[li % NB]
            li += 1
            kb.dma("sp" if li % 2 else "pool", wb[:], Wv[:, c8 * 8:(c8 + 1) * 8, j * CW:(j + 1) * CW], w=[wb])

            def mm(e, c8=c8, wb=wb, p=p):
                ins = None
                for cc in range(8):
                    c = c8 * 8 + cc
                    ins = e.matmul(p[0:NV, :], lhsT=cs[:, c, :], rhs=wb[:, cc, :], start=(c == 0), stop=(c == KC - 1))
                return ins
            kb.op("pe", mm, r=[cs, wb], w=[p])
        kb.op("dve", lambda e, p=p, j=j: e.tensor_tensor(out=ot[:, j * CW:(j + 1) * CW], in0=p[0:NV, :],
                                                        in1=bt[:, j * CW:(j + 1) * CW], op=ALU.add),
              r=[p, bt], w=[ot])
    kb.dma("sp", out[:], ot[:], r=[ot], store=True)
    return kb.finish()


def run_p0(c, c_ctx, ada_w, ada_b):
    D = c.shape[1]
    L = ada_w.shape[0]
    ncol = ada_w.shape[2]
    cvs = np.concatenate([c, c_ctx[None, :]], 0).astype(np.float32)
    cvT = np.ascontiguousarray(cvs.T.reshape(D // 128, 128, 3).transpose(1, 0, 2))
    per = ncol * L // 8
    nc = build_p0(D=D, NCOL=per)
    maps = []
    for j in range(8):
        l = (j * per) // ncol
        c0 = (j * per) % ncol
        maps.append({"cvT": cvT, "w": np.ascontiguousarray(ada_w[l][:, c0:c0 + per]),
                     "b": np.ascontiguousarray(ada_b[l][None, c0:c0 + per])})
    res = _run(nc, maps)
    flat = np.concatenate([r["out"] for r in res], axis=1)
    return flat.reshape(3, L, ncol).transpose(1, 0, 2)


def seq_tiles(TC, TL, W):
    out = []
    for t0 in range(0, TC, W):
        out.append((t0, min(W, TC - t0), False))
    for t0 in range(0, TL, W):
        out.append((TC + t0, min(W, TL - t0), True))
    return out


def make_consts():
    c = np.zeros((128, 20, 128), np.float32)
    i = np.arange(128)
    c[:, 0, :] = np.eye(128)
    c[:, 1, :] = (i[:, None] > i[None, :])
    c[:, 2, :] = (i[:, None] < i[None, :])
    c[:, 3, :] = (i[:, None] >= i[None, :])
    c[:, 4, :] = (i[:, None] <= i[None, :])
    c[:, 5, :] = ((i[:, None] // 64) == (i[None, :] // 64))
    for l in range(7):
        t, s_ = i[:, None], i[None, :]
        m = ((t >> (l + 1)) == (s_ >> (l + 1))) & (((t >> l) & 1) == 1) & (((s_ >> l) & 1) == 0)
        c[:, 6 + l, :] = m
        c[:, 13 + l, :] = m.T
    return c


def make_consts2():
    c = make_consts()
    o = np.zeros((128, 15, 2, 128), np.float32)
    for l in range(7):
        o[:, l, :, :] = c[:, 6 + l, None, :]
        o[:, 7 + l, :, :] = c[:, 13 + l, None, :]
    o[:, 14, :, :] = c[:, 0, None, :]
    return o


def gemm_fm(kb, Wd, NCH, K, XT, NT, tiles, evac, GS=4, xdt=BF16):
    KC = K // 128
    TW = max(w for _, w, _ in tiles)
    with kb.scope():
        wst = [kb.sb("g_wst%d" % i, [128, KC, 128]) for i in range(2)]
        wb = [kb.sb("g_wb%d" % i, [128, KC, 128], BF16) for i in range(GS)]
        xt = [kb.sb("g_xt%d" % i, [128, KC, TW], xdt) for i in range(2)]
        pss = [kb.ps("g_ps%d" % i, [128, 512]) for i in range(GS)]
        XTv = XT[:].rearrange("(c p) t -> p c t", p=128)
        li = 0
        xi = 0
        for g0 in range(0, NCH, GS):
            js = list(range(g0, min(NCH, g0 + GS)))
            for jj, j in enumerate(js):
                st = wst[li % 2]
                li += 1
                kb.dma("sp", st[:], Wd[j].rearrange("(c p) n -> p c n", p=128), w=[st])
                kb.op("pool", lambda e, st=st, jj=jj: e.tensor_copy(out=wb[jj][:], in_=st[:]), r=[st], w=[wb[jj]])
            for (t0, W, _) in tiles:
                x = xt[xi % 2]
                xi += 1
                kb.dma("act", x[:, :, 0:W], XTv[:, :, t0:t0 + W], w=[x])
                for jj, j in enumerate(js):
                    p = pss[jj]

                    def mm(e, jj=jj, p=p, x=x, W=W):
                        ins = None
                        for c in range(KC):
                            ins = e.matmul(p[:, 0:W], lhsT=wb[jj][:, c, :], rhs=x[:, c, 0:W], start=(c == 0), stop=(c == KC - 1))
                        return ins
                    kb.op("pe", mm, r=[wb[jj], x], w=[p])
                    evac(j, t0, W, p)


NVR = 7
NVL = 11


def build_p1(D=4096, TC=256, TL=4096, NBLK=4, NLB=4, dbg=False, upto=None):
    kb = KB()
    KC = D // 128
    NT = TC + TL
    NCK = NT // 128
    RW = NBLK * 128
    NCHR = 3 * NBLK + 6
    NCH = NCHR + 2 * NLB
    CK_C = TC // 128
    I = lambda name, shape, dt=F32: kb.dram(name, shape, dt, kind="ExternalInput")
    xT = I("xT", [D, TL]); cT = I("cT", [D, TC]); ptab_d = I("ptab", [128, KC, 64]); mods_d = I("mods", [128, KC, 4])
    win = I("win", [NCH, D, 128]); mu_d = I("mu", [128, NCHR])
    w2_d = I("w2", [2, 128, RW]); a2_d = I("a2", [2, 128, RW]); g2_d = I("g2", [2, 128, RW])
    rvec_d = I("rvec", [128, NBLK, NVR]); lnx_d = I("lnx", [1, 2 * RW])
    lvec_d = I("lvec", [128, NLB, NVL]); lwa_d = I("lwa", [2, NLB, 128, 128]); lwx_d = I("lwx", [2, NLB, 128, 128])
    cst_d = I("cst", [128, 20, 128]); cst2_d = I("cst2", [128, 15, 2, 128])
    ya = kb.dram("ya", [NT, RW], kind="ExternalOutput")
    yb = kb.dram("yb", [NLB * 128, NT], kind="ExternalOutput")
    hT = kb.dram("hT", [D, NT], BF16)
    P = kb.dram("P", [NCH, 128, NT])
    OPS = kb.dram("OPS", [2, 4, 128, NT])

    with kb.scope():
        ptab = kb.sb("ptab", [128, KC, 64]); mods = kb.sb("mods", [128, KC, 4])
        kb.dma("sp", ptab[:], ptab_d[:], w=[ptab]); kb.dma("sp", mods[:], mods_d[:], w=[mods])
        kb.op("dve", lambda e: e.tensor_scalar_add(out=mods[:, :, 1:2], in0=mods[:, :, 1:2], scalar1=1.0), r=[mods], w=[mods])
        kb.op("dve", lambda e: e.tensor_scalar_add(out=mods[:, :, 3:4], in0=mods[:, :, 3:4], scalar1=1.0), r=[mods], w=[mods])
        GA = min(8, KC // 2)
        xs = [kb.sb("xs%d" % i, [128, GA, 512]) for i in range(2)]
        hb = [kb.sb("hb%d" % i, [128, GA, 512], BF16) for i in range(2)]
        hTv = hT[:].rearrange("(c p) t -> p c t", p=128)
        it = 0
        for (t0, W, lat) in seq_tiles(TC, TL, 512):
            src, s0 = (xT, t0 - TC) if lat else (cT, t0)
            srcv = src[:].rearrange("(c p) t -> p c t", p=128)
            for g in range(KC // GA):
                x = xs[it % 2]; h = hb[it % 2]; it += 1
                c0 = g * GA
                kb.dma("sp", x[:, :, 0:W], srcv[:, c0:c0 + GA, s0:s0 + W], w=[x])
                if lat:
                    nr = W // 64; r0 = s0 // 64
                    xv = x[:, :, 0:W].rearrange("p c (r q) -> p c r q", q=64)
                    if c0 < KC // 2:
                        pin = ptab[:, c0:c0 + GA, r0:r0 + nr].unsqueeze(3).to_broadcast([128, GA, nr, 64])
                    else:
                        pin = ptab[:, c0:c0 + GA, 0:64].unsqueeze(2).to_broadcast([128, GA, nr, 64])
                    kb.op("pool", lambda e, xv=xv, pin=pin: e.tensor_tensor(out=xv, in0=xv, in1=pin, op=ALU.add), r=[x, ptab], w=[x])
                mo = 0 if lat else 2
                for cc in range(GA):
                    c = c0 + cc
                    kb.op("dve" if cc % 2 else "act",
                          (lambda e, cc=cc, c=c, x=x, h=h, W=W, mo=mo: e.tensor_scalar(out=h[:, cc, 0:W], in0=x[:, cc, 0:W], scalar1=mods[:, c, mo + 1:mo + 2], scalar2=mods[:, c, mo:mo + 1], op0=ALU.mult, op1=ALU.add))
                          if cc % 2 else
                          (lambda e, cc=cc, c=c, x=x, h=h, W=W, mo=mo: e.activation(out=h[:, cc, 0:W], in_=x[:, cc, 0:W], func=AF.Identity, scale=mods[:, c, mo + 1:mo + 2], bias=mods[:, c, mo:mo + 1])),
                          r=[x, mods], w=[h])
                kb.dma("sp", hTv[:, c0:c0 + GA, t0:t0 + W], h[:, :, 0:W], r=[h], store=True)

    if upto == "A":
        return kb.finish()
    oi = [0]

    def evacB(j, t0, W, p):
        o = obs[oi[0] % 4]; oi[0] += 1
        if oi[0] % 2:
            kb.op("act", lambda e: e.activation(out=o[:, 0:W], in_=p[:, 0:W], func=AF.Copy), r=[p], w=[o])
        else:
            kb.op("dve", lambda e: e.tensor_copy(out=o[:, 0:W], in_=p[:, 0:W]), r=[p], w=[o])
        kb.dma("sp", P[j][:, t0:t0 + W], o[:, 0:W], r=[o], store=True)
    with kb.scope():
        obs = [kb.sb("ob%d" % i, [128, 512]) for i in range(4)]
        gemm_fm(kb, win, NCH, D, hT, NT, seq_tiles(TC, TL, 512), evacB, GS=7)

    if upto == "B":
        return kb.finish()
    cst = kb.sb("cst", [128, 20, 128]); kb.dma("sp", cst[:], cst_d[:], w=[cst])
    IDN, ML_S, MU_S, ML_I, MU_I, BONE = (cst[:, i, :] for i in range(6))
    mu = kb.sb("mu", [128, NCHR]); kb.dma("sp", mu[:], mu_d[:], w=[mu])
    omm = kb.sb("omm", [128, NCHR]); hmu = kb.sb("hmu", [128, NCHR])
    kb.op("dve", lambda e: e.tensor_scalar(out=omm[:], in0=mu[:], scalar1=-1.0, scalar2=1.0, op0=ALU.mult, op1=ALU.add), r=[mu], w=[omm])
    kb.op("dve", lambda e: e.tensor_scalar_mul(out=hmu[:], in0=mu[:], scalar1=0.5), r=[mu], w=[hmu])
    TW = 512
    ctiles = seq_tiles(TC, TL, TW)

    def load_shift(j, t0, W, lat, dst, raw, eng="dve"):
        s_lo, s_hi = (TC, NT) if lat else (0, TC)
        lo = t0 - 1 if t0 > s_lo else t0
        hi = t0 + W + 1 if t0 + W < s_hi else t0 + W
        if lo == t0:
            kb.op("pool", lambda e: e.memset(raw[:, 0:1], 0.0), w=[raw])
        if hi == t0 + W:
            kb.op("pool", lambda e: e.memset(raw[:, W + 1:W + 2], 0.0), w=[raw])
        kb.dma("sp", raw[:, 1 - (t0 - lo):1 - (t0 - lo) + (hi - lo)], P[j][:, lo:hi], w=[raw])
        kb.op(eng, lambda e: e.tensor_tensor(out=dst[:, 0:W], in0=raw[:, 0:W], in1=raw[:, 2:W + 2], op=ALU.add), r=[raw], w=[dst])
        kb.op(eng, lambda e: e.tensor_scalar_mul(out=dst[:, 0:W], in0=dst[:, 0:W], scalar1=hmu[:, j:j + 1]), r=[dst, hmu], w=[dst])
        kb.op(eng, lambda e: e.scalar_tensor_tensor(out=dst[:, 0:W], in0=raw[:, 1:W + 1], scalar=omm[:, j:j + 1], in1=dst[:, 0:W], op0=ALU.mult, op1=ALU.add), r=[raw, dst, omm], w=[dst])

    LR = kb.dram("LR", [6, 128, NT])
    with kb.scope():
        raw = [kb.sb("c0raw%d" % i, [128, TW + 2]) for i in range(2)]
        dst = [kb.sb("c0dst%d" % i, [128, TW]) for i in range(2)]
        n = 0
        for q in range(6):
            j = 3 * NBLK + q
            fn = AF.Tanh if q < 2 else (AF.Identity if q < 4 else AF.Sigmoid)
            for (t0, W, lat) in ctiles:
                rw_, ds_ = raw[n % 2], dst[n % 2]; n += 1
                load_shift(j, t0, W, lat, ds_, rw_)
                kb.op("act", lambda e, ds_=ds_, W=W, fn=fn: e.activation(out=ds_[:, 0:W], in_=ds_[:, 0:W], func=fn), r=[ds_], w=[ds_])
                kb.dma("sp", LR[q][:, t0:t0 + W], ds_[:, 0:W], r=[ds_], store=True)

    if upto == "C0":
        return kb.finish()
    with kb.scope():
        rvec = kb.sb("rvec", [128, NBLK, NVR]); kb.dma("sp", rvec[:], rvec_d[:], w=[rvec])
        omka = kb.sb("omka", [128, NBLK])
        kb.op("dve", lambda e: e.tensor_scalar(out=omka[:], in0=rvec[:, :, 5], scalar1=-1.0, scalar2=1.0, op0=ALU.mult, op1=ALU.add), r=[rvec], w=[omka])
        lnw = kb.sb("lnw", [128, RW]); lnb = kb.sb("lnb", [128, RW])
        kb.dma("sp", lnw[:], lnx_d[:, 0:RW].partition_broadcast(128), w=[lnw])
        kb.dma("sp", lnb[:], lnx_d[:, RW:2 * RW].partition_broadcast(128), w=[lnb])
        w2 = kb.sb("w2", [128, 2, RW]); a2 = kb.sb("a2", [128, 2, RW]); g2 = kb.sb("g2", [128, 2, RW])
        kb.dma("sp", w2[:], w2_d[:].rearrange("d k n -> k d n"), w=[w2])
        kb.dma("sp", a2[:], a2_d[:].rearrange("d k n -> k d n"), w=[a2])
        kb.dma("sp", g2[:], g2_d[:].rearrange("d k n -> k d n"), w=[g2])
        hsel = kb.sb("hsel", [128, 2])
        kb.op("dve", lambda e: e.tensor_copy(out=hsel[:, 0:1], in_=cst[:, 5, 0:1]), r=[cst], w=[hsel])
        kb.op("dve", lambda e: e.tensor_copy(out=hsel[:, 1:2], in_=cst[:, 5, 64:65]), r=[cst], w=[hsel])
        raw = kb.sb("raw", [128, TW + 2])
        Rs = kb.sb("Rs", [128, TW]); Ks = kb.sb("Ks", [128, TW]); Vs = kb.sb("Vs", [128, TW]); KKn = kb.sb("KKn", [128, TW])
        RK = kb.sb("RK", [128, TW]); LD = kb.sb("LD", [128, TW]); CL = kb.sb("CL", [128, TW]); ICL = kb.sb("ICL", [128, TW])
        KD = kb.sb("KD", [128, TW]); T0 = kb.sb("T0", [128, TW]); T1 = kb.sb("T1", [128, TW])
        O4 = [kb.sb("O4_%d" % i, [128, TW]) for i in range(4)]
        ones = kb.sb("ones", [128, 128]); kb.op("pool", lambda e: e.memset(ones[:], 1.0), w=[ones])
        lt = [kb.sb("lt%d" % i, [128, 512]) for i in range(2)]
        Vtm = kb.sb("Vtm", [128, NCK, 128]); Yd = [kb.sb("Yd%d" % d, [128, NCK, 128]) for d in range(2)]
        sbon = kb.sb("sbon", [128, NCK, 2]); EL = [kb.sb("EL%d" % d, [128, NCK]) for d in range(2)]
        ST = [[kb.sb("ST%d_%d" % (d, hh), [128, 64]) for hh in range(2)] for d in range(2)]
        PADS = [[[[kb.sb("pad%d_%d_%d_%d" % (d, q, hh, par), [128, 128]) for par in range(2)] for hh in range(2)] for q in range(3)] for d in range(2)]
        for d in range(2):
            for q in range(3):
                for hh in range(2):
                    for par in range(2):
                        kb.op("pool", lambda e: e.memset(PADS[d][q][hh][par][:], 0.0), w=[PADS[d][q][hh][par]])
        cst2 = kb.sb("cst2", [128, 15, 2, 128]); kb.dma("sp", cst2[:], cst2_d[:], w=[cst2])
        banks = [kb.ps("bank%d" % i, [128, 512]) for i in range(8)]
        pW = banks[0]
        pV = banks[1]
        OPB = [[[kb.sb("opb%d_%d_%d" % (d, b, q), [128, 128]) for q in range(4)] for b in range(2)] for d in range(2)]
        BKb = [[kb.sb("bkb%d_%d" % (d, q), [128, 128]) for q in range(2)] for d in range(2)]
        BKt = [[kb.sb("bkt%d_%d" % (d, q), [128, 128]) for q in range(2)] for d in range(2)]
        M3 = lambda nm: [kb.sb("%s%d" % (nm, d), [128, 2, 128]) for d in range(2)]
        Mf = M3("Mf"); MTf = M3("MTf"); Mm = M3("Mm"); MTm = M3("MTm"); T1s = M3("T1s"); T2s = M3("T2s")
        Xb = [[kb.sb("X%d_%d" % (d, b), [128, 2, 128]) for b in range(2)] for d in range(2)]
        XTb = [[kb.sb("XT%d_%d" % (d, b), [128, 2, 128]) for b in range(2)] for d in range(2)]
        Mak = [kb.sb("Mak%d" % d, [128, 2, 128]) for d in range(2)]
        Nrb = [kb.sb("Nrb%d" % d, [128, 2, 128]) for d in range(2)]
        Nrk = [kb.sb("Nrk%d" % d, [128, 2, 128]) for d in range(2)]
        Ub = [[kb.sb("U%d_%d" % (d, b), [128, 128]) for b in range(2)] for d in range(2)]
        bk = lambda i, a, b: Tl(banks[i].t[:, a:b], banks[i].key)
        pP = [bk(2 + 3 * d, 0, 256) for d in range(2)]
        pM = [bk(2 + 3 * d, 256, 512) for d in range(2)]
        pPT = [bk(3 + 3 * d, 0, 256) for d in range(2)]
        pT2 = [bk(3 + 3 * d, 256, 512) for d in range(2)]
        pU = [bk(4 + 3 * d, 0, 128) for d in range(2)]
        pY = [bk(4 + 3 * d, 128, 256) for d in range(2)]
        pS = [bk(4 + 3 * d, 256, 384) for d in range(2)]
        st1 = kb.sb("st1", [128, NCK * 2]); st2 = kb.sb("st2", [128, NCK * 2])
        gl = [kb.sb("gl%d" % i, [128, 2, 512]) for i in range(2)]
        order = [list(range(NCK)), list(range(CK_C - 1, -1, -1)) + list(range(NCK - 1, CK_C - 1, -1))]

        for bi in range(NBLK):
            jr, jk, jv = bi, NBLK + bi, 2 * NBLK + bi
            cs = slice(bi * 128, (bi + 1) * 128)
            V_ = lambda i: rvec[:, bi, i:i + 1]
            for (t0, W, lat) in ctiles:
                ck0 = t0 // 128; nck = W // 128
                load_shift(jr, t0, W, lat, Rs, raw)
                load_shift(jk, t0, W, lat, Ks, raw)
                load_shift(jv, t0, W, lat, Vs, raw)
                for cc in range(nck):
                    kb.op("pe", lambda e, cc=cc: e.transpose(pV[:, 0:128], Vs[:, cc * 128:(cc + 1) * 128], IDN), r=[Vs, cst], w=[pV])
                    kb.op("act", lambda e, cc=cc: e.activation(out=Vtm[:, ck0 + cc, :], in_=pV[:, 0:128], func=AF.Copy), r=[pV], w=[Vtm])
                kb.op("dve", lambda e: e.tensor_scalar_mul(out=KKn[:, 0:W], in0=Ks[:, 0:W], scalar1=V_(4)), r=[Ks, rvec], w=[KKn])
                kb.op("act", lambda e: e.activation(out=T0[:, 0:W], in_=KKn[:, 0:W], func=AF.Square), r=[KKn], w=[T0])
                for s0 in range(0, W, 512):
                    sw = min(512, W - s0)
                    kb.op("pe", lambda e, s0=s0, sw=sw: e.matmul(pW[:, 0:sw], lhsT=BONE, rhs=T0[:, s0:s0 + sw], start=True, stop=True), r=[T0, cst], w=[pW])
                    kb.op("dve", lambda e, s0=s0, sw=sw: e.tensor_scalar_max(out=T1[:, s0:s0 + sw], in0=pW[:, 0:sw], scalar1=1e-24), r=[pW], w=[T1])
                kb.op("act", lambda e: e.activation(out=T1[:, 0:W], in_=T1[:, 0:W], func=AF.Sqrt), r=[T1], w=[T1])
                kb.op("dve", lambda e: e.reciprocal(out=T1[:, 0:W], in_=T1[:, 0:W]), r=[T1], w=[T1])
                kb.op("dve", lambda e: e.tensor_tensor(out=KKn[:, 0:W], in0=KKn[:, 0:W], in1=T1[:, 0:W], op=ALU.mult), r=[KKn, T1], w=[KKn])
                for d in range(2):
                    for s0 in range(0, W, 512):
                        sw = min(512, W - s0)
                        for (q, wts, dstT, biasv) in ((d, w2, LD, V_(d)), (2 + d, a2, ICL, V_(2 + d))):
                            l = lt[(s0 // 512 + q) % 2]
                            kb.dma("sp", l[:, 0:sw], LR[q][:, t0 + s0:t0 + s0 + sw], w=[l])
                            kb.op("pe", lambda e, l=l, wts=wts, sw=sw: e.matmul(pW[:, 0:sw], lhsT=wts[:, d, cs], rhs=l[:, 0:sw], start=True, stop=True), r=[l, wts], w=[pW])
                            kb.op("act", lambda e, dstT=dstT, biasv=biasv, s0=s0, sw=sw: e.activation(out=dstT[:, s0:s0 + sw], in_=pW[:, 0:sw], func=AF.Sigmoid, bias=biasv, scale=1.0), r=[pW, rvec], w=[dstT])
                    kb.op("dve", lambda e: e.tensor_scalar_mul(out=LD[:, 0:W], in0=LD[:, 0:W], scalar1=-0.6065306597126334), r=[LD], w=[LD])
                    kb.op("dve", lambda e: e.tensor_scalar(out=KD[:, 0:W], in0=ICL[:, 0:W], scalar1=V_(5), scalar2=omka[:, bi:bi + 1], op0=ALU.mult, op1=ALU.add), r=[ICL, rvec, omka], w=[KD])
                    kb.op("dve", lambda e: e.tensor_tensor(out=KD[:, 0:W], in0=KD[:, 0:W], in1=Ks[:, 0:W], op=ALU.mult), r=[KD, Ks], w=[KD])
                    if d == 0:
                        kb.op("pool", lambda e: e.tensor_tensor(out=RK[:, 0:W], in0=Rs[:, 0:W], in1=KD[:, 0:W], op=ALU.mult), r=[Rs, KD], w=[RK])
                    else:
                        kb.op("pool", lambda e: e.tensor_tensor(out=T0[:, 0:W], in0=Rs[:, 0:W], in1=KD[:, 0:W], op=ALU.mult), r=[Rs, KD], w=[T0])
                        kb.op("pool", lambda e: e.tensor_tensor(out=RK[:, 0:W], in0=RK[:, 0:W], in1=T0[:, 0:W], op=ALU.add), r=[RK, T0], w=[RK])
                    for cc in range(nck):
                        sl = slice(cc * 128, (cc + 1) * 128)
                        if d == 0:
                            kb.op("dve", lambda e, sl=sl: e.tensor_tensor_scan(out=CL[:, sl], data0=ones[:], data1=LD[:, sl], initial=0.0, op0=ALU.mult, op1=ALU.add), r=[LD, ones], w=[CL])
                        else:
                            kb.op("dve", lambda e, sl=sl: e.tensor_tensor_scan(out=CL[:, sl][:, ::-1], data0=ones[:], data1=LD[:, sl][:, ::-1], initial=0.0, op0=ALU.mult, op1=ALU.add), r=[LD, ones], w=[CL])
                    CLv = CL[:, 0:W].rearrange("p (c l) -> p c l", l=128)
                    last = CLv[:, :, 127] if d == 0 else CLv[:, :, 0]
                    kb.op("act", lambda e, last=last: e.activation(out=EL[d][:, ck0:ck0 + nck], in_=last, func=AF.Exp), r=[CL], w=[EL[d]])
                    kb.op("act", lambda e: e.activation(out=T0[:, 0:W], in_=CL[:, 0:W], func=AF.Exp), r=[CL], w=[T0])
                    kb.op("dve", lambda e: e.tensor_tensor(out=O4[1][:, 0:W], in0=Rs[:, 0:W], in1=T0[:, 0:W], op=ALU.mult), r=[Rs, T0], w=[O4[1]])
                    kb.op("dve", lambda e: e.tensor_tensor(out=T1[:, 0:W], in0=CL[:, 0:W], in1=LD[:, 0:W], op=ALU.subtract), r=[CL, LD], w=[T1])
                    kb.op("act", lambda e: e.activation(out=T1[:, 0:W], in_=T1[:, 0:W], func=AF.Exp), r=[T1], w=[T1])
                    kb.op("dve", lambda e: e.scalar_tensor_tensor(out=O4[0][:, 0:W], in0=KKn[:, 0:W], scalar=-1.0, in1=T1[:, 0:W], op0=ALU.mult, op1=ALU.mult), r=[KKn, T1], w=[O4[0]])
                    kb.op("act", lambda e: e.activation(out=T0[:, 0:W], in_=CL[:, 0:W], func=AF.Exp, scale=-1.0), r=[CL], w=[T0])
                    kb.op("pool", lambda e: e.tensor_tensor(out=T1[:, 0:W], in0=KKn[:, 0:W], in1=ICL[:, 0:W], op=ALU.mult), r=[KKn, ICL], w=[T1])
                    kb.op("dve", lambda e: e.tensor_tensor(out=O4[2][:, 0:W], in0=T1[:, 0:W], in1=T0[:, 0:W], op=ALU.mult), r=[T1, T0], w=[O4[2]])
                    kb.op("dve", lambda e: e.tensor_tensor(out=O4[3][:, 0:W], in0=KD[:, 0:W], in1=T0[:, 0:W], op=ALU.mult), r=[KD, T0], w=[O4[3]])
                    for q in range(4):
                        kb.dma("sp", OPS[d, q][:, t0:t0 + W], O4[q][:, 0:W], r=[O4[q]], store=True)
                kb.op("dve", lambda e: e.tensor_scalar_mul(out=RK[:, 0:W], in0=RK[:, 0:W], scalar1=V_(6)), r=[RK, rvec], w=[RK])
                for cc in range(nck):
                    kb.op("pe", lambda e, cc=cc: e.matmul(pV[:, 256 + 2 * cc:258 + 2 * cc], lhsT=RK[:, cc * 128:(cc + 1) * 128], rhs=hsel[:], start=True, stop=True), r=[RK, hsel], w=[pV])
                kb.op("dve", lambda e: e.tensor_copy(out=sbon[:, ck0:ck0 + nck, :], in_=pV[:, 256:256 + 2 * nck].rearrange("p (c h) -> p c h", h=2)), r=[pV], w=[sbon])
            kb.barrier()
            if upto == "C":
                break
            for d in range(2):
                for hh in range(2):
                    kb.op("pool", lambda e, d=d, hh=hh: e.memset(ST[d][hh][:], 0.0), w=[ST[d][hh]])
            for step in range(NCK):
                cks = [order[0][step], order[1][step]]
                ob = [OPB[d][step % 2] for d in range(2)]
                for d in range(2):
                    c = cks[d]
                    for q in range(4):
                        kb.dma("sp" if q % 2 else "act", ob[d][q][:], OPS[d, q][:, c * 128:(c + 1) * 128], w=[ob[d][q]])
                AHd = [ob[d][0] for d in range(2)]; RHd = [ob[d][1] for d in range(2)]; BHd = [ob[d][2] for d in range(2)]; KHd = [ob[d][3] for d in range(2)]
                mS = [(ML_S, MU_S), (MU_S, ML_S)]
                mI = [MU_I, ML_I]
                HS = (slice(0, 64), slice(64, 128))

                def mm2(e, out_t, lhs, rhs):
                    ins = None
                    for hh in range(2):
                        ins = e.matmul(out_t[:, hh * 128:(hh + 1) * 128], lhsT=lhs[hh][:], rhs=rhs[:], start=True, stop=True)
                    return ins
                HSs = (slice(0, 64), slice(64, 128))
                for d in range(2):
                    c = cks[d]
                    for q, oq in ((0, 0), (1, 2), (2, 3)):
                        for hh in range(2):
                            pd = PADS[d][q][hh][step % 2]
                            kb.dma("sp" if (q + hh) % 2 else "act", pd[HSs[hh], :], OPS[d, oq][HSs[hh], c * 128:(c + 1) * 128], w=[pd])
                AP_ = [[PADS[d][0][hh][step % 2] for hh in range(2)] for d in range(2)]; BP_ = [[PADS[d][1][hh][step % 2] for hh in range(2)] for d in range(2)]; KP_ = [[PADS[d][2][hh][step % 2] for hh in range(2)] for d in range(2)]

                def evm(eng, dst, src, mask):
                    kb.op(eng, lambda e: e.tensor_tensor(out=dst[:], in0=src[:].rearrange("p (h s) -> p h s", h=2), in1=mask.unsqueeze(1).to_broadcast([128, 2, 128]), op=ALU.mult), r=[src, cst], w=[dst])
                for d in range(2):
                    kb.op("pe", lambda e, d=d: mm2(e, pP[d], AP_[d], BHd[d]), r=AP_[d] + [BHd[d]], w=[pP[d]])
                    kb.op("pe", lambda e, d=d: mm2(e, pPT[d], BP_[d], AHd[d]), r=BP_[d] + [AHd[d]], w=[pPT[d]])
                    kb.op("act", lambda e, d=d: e.activation(out=Mf[d][:].rearrange("p h s -> p (h s)"), in_=pP[d][:], func=AF.Copy), r=[pP[d]], w=[Mf[d]])
                    kb.op("act", lambda e, d=d: e.activation(out=MTf[d][:].rearrange("p h s -> p (h s)"), in_=pPT[d][:], func=AF.Copy), r=[pPT[d]], w=[MTf[d]])
                    kb.op("pe", lambda e, d=d: mm2(e, pM[d], KP_[d], AHd[d]), r=KP_[d] + [AHd[d]], w=[pM[d]])
                    evm("dve", Mak[d], pM[d], mS[d][1])
                    c = cks[d]
                    for q, src in ((0, BHd[d]), (1, KHd[d])):
                        kb.op("dve", lambda e, q=q, src=src, d=d, c=c: e.tensor_scalar_mul(out=BKb[d][q][:], in0=src[:], scalar1=EL[d][:, c:c + 1]), r=[src, EL[d]], w=[BKb[d][q]])
                        kb.op("pe", lambda e, q=q, d=d: e.transpose(pT2[d][:, q * 128:(q + 1) * 128], BKb[d][q][:], IDN), r=[BKb[d][q], cst], w=[pT2[d]])
                        kb.op("act", lambda e, q=q, d=d: e.activation(out=BKt[d][q][:], in_=pT2[d][:, q * 128:(q + 1) * 128], func=AF.Copy), r=[pT2[d]], w=[BKt[d][q]])
                for d in range(2):
                    c = cks[d]

                    def rhs0(e, d=d, c=c):
                        ins = None
                        for hh in range(2):
                            e.matmul(pU[d][:, hh * 64:(hh + 1) * 64], lhsT=AHd[d][:], rhs=ST[d][hh][:], start=True, stop=False)
                            ins = e.matmul(pU[d][:, hh * 64:(hh + 1) * 64], lhsT=Mak[d][:, hh, :], rhs=Vtm[:, c, hh * 64:(hh + 1) * 64], start=False, stop=True)
                        return ins
                    kb.op("pe", rhs0, r=[AHd[d], ST[d][0], ST[d][1], Mak[d], Vtm], w=[pU[d]])
                    kb.op("act", lambda e, d=d: e.activation(out=Ub[d][0][:], in_=pU[d][:], func=AF.Copy), r=[pU[d]], w=[Ub[d][0]])
                    if dbg and step == 1 and d == 0 and bi == 0:
                        kb.dump(Ub[0][0], "d_U0"); kb.dump(Mak[0], "d_Mak"); kb.dump(AHd[0], "d_AH1")
                    kb.op("pe", lambda e, d=d: mm2(e, pM[d], BP_[d], RHd[d]), r=BP_[d] + [RHd[d]], w=[pM[d]])
                    evm("dve", Nrb[d], pM[d], mI[d])
                    kb.op("pe", lambda e, d=d: mm2(e, pM[d], KP_[d], RHd[d]), r=KP_[d] + [RHd[d]], w=[pM[d]])
                    evm("dve", Nrk[d], pM[d], mI[d])
                MKa = lambda d, l: cst2[:, (0 if d == 0 else 7) + l, :, :]
                MKb = lambda d, l: cst2[:, (7 if d == 0 else 0) + l, :, :]
                ID2 = cst2[:, 14, :, :]

                def mmh(e, out_t, lhs, rhs):
                    ins = None
                    for hh in range(2):
                        ins = e.matmul(out_t[:, hh * 128:(hh + 1) * 128], lhsT=lhs[:, hh, :], rhs=rhs[:, hh, :], start=True, stop=True)
                    return ins
                flat = lambda t: t[:].rearrange("p h s -> p (h s)")
                for d in range(2):
                    kb.op("pool", lambda e, d=d: e.tensor_tensor(out=Xb[d][0][:], in0=Mf[d][:], in1=MKa(d, 0), op=ALU.mult), r=[Mf[d], cst2], w=[Xb[d][0]])
                    kb.op("pool", lambda e, d=d: e.tensor_tensor(out=Xb[d][0][:], in0=Xb[d][0][:], in1=ID2, op=ALU.add), r=[Xb[d][0], cst2], w=[Xb[d][0]])
                    kb.op("pool", lambda e, d=d: e.tensor_tensor(out=XTb[d][0][:], in0=MTf[d][:], in1=MKb(d, 0), op=ALU.mult), r=[MTf[d], cst2], w=[XTb[d][0]])
                    kb.op("pool", lambda e, d=d: e.tensor_tensor(out=XTb[d][0][:], in0=XTb[d][0][:], in1=ID2, op=ALU.add), r=[XTb[d][0], cst2], w=[XTb[d][0]])
                for l in range(1, 7):
                    a, b = (l - 1) % 2, l % 2
                    for d in range(2):
                        kb.op("pool", lambda e, d=d, l=l: e.tensor_tensor(out=Mm[d][:], in0=Mf[d][:], in1=MKa(d, l), op=ALU.mult), r=[Mf[d], cst2], w=[Mm[d]])
                        kb.op("pool", lambda e, d=d, l=l: e.tensor_tensor(out=MTm[d][:], in0=MTf[d][:], in1=MKb(d, l), op=ALU.mult), r=[MTf[d], cst2], w=[MTm[d]])
                        kb.op("pe", lambda e, d=d, a=a: mmh(e, pP[d], MTm[d], Xb[d][a]), r=[MTm[d], Xb[d][a]], w=[pP[d]])
                        kb.op("pe", lambda e, d=d, a=a: mmh(e, pPT[d], Mm[d], XTb[d][a]), r=[Mm[d], XTb[d][a]], w=[pPT[d]])
                        kb.op("act", lambda e, d=d: e.activation(out=flat(T1s[d]), in_=pP[d][:], func=AF.Copy), r=[pP[d]], w=[T1s[d]])
                        kb.op("dve", lambda e, d=d: e.tensor_copy(out=flat(T2s[d]), in_=pPT[d][:]), r=[pPT[d]], w=[T2s[d]])
                    for d in range(2):
                        if l < 6:
                            kb.op("pe", lambda e, d=d, a=a: mmh(e, pM[d], XTb[d][a], T1s[d]), r=[XTb[d][a], T1s[d]], w=[pM[d]])
                            kb.op("dve", lambda e, d=d, a=a, b=b: e.tensor_tensor(out=flat(Xb[d][b]), in0=pM[d][:], in1=flat(Xb[d][a]), op=ALU.add), r=[pM[d], Xb[d][a]], w=[Xb[d][b]])
                        kb.op("pe", lambda e, d=d, a=a: mmh(e, pT2[d], Xb[d][a], T2s[d]), r=[Xb[d][a], T2s[d]], w=[pT2[d]])
                        kb.op("dve", lambda e, d=d, a=a, b=b: e.tensor_tensor(out=flat(XTb[d][b]), in0=pT2[d][:], in1=flat(XTb[d][a]), op=ALU.add), r=[pT2[d], XTb[d][a]], w=[XTb[d][b]])
                for d in range(2):
                    def app(e, d=d):
                        ins = None
                        for hh in range(2):
                            ins = e.matmul(pU[d][:, hh * 64:(hh + 1) * 64], lhsT=XTb[d][0][:, hh, :], rhs=Ub[d][0][:, hh * 64:(hh + 1) * 64], start=True, stop=True)
                        return ins
                    kb.op("pe", app, r=[XTb[d][0], Ub[d][0]], w=[pU[d]])
                    kb.op("act", lambda e, d=d: e.activation(out=Ub[d][1][:], in_=pU[d][:], func=AF.Copy), r=[pU[d]], w=[Ub[d][1]])
                UF = 1
                for d in range(2):
                    c = cks[d]
                    U = Ub[d][UF]

                    def ymm(e, d=d, c=c, U=U):
                        ins = None
                        for hh in range(2):
                            o = pY[d][:, hh * 64:(hh + 1) * 64]
                            e.matmul(o, lhsT=RHd[d][:], rhs=ST[d][hh][:], start=True, stop=False)
                            e.matmul(o, lhsT=Nrb[d][:, hh, :], rhs=U[:, hh * 64:(hh + 1) * 64], start=False, stop=False)
                            ins = e.matmul(o, lhsT=Nrk[d][:, hh, :], rhs=Vtm[:, c, hh * 64:(hh + 1) * 64], start=False, stop=True)
                        return ins
                    kb.op("pe", ymm, r=[RHd[d], ST[d][0], ST[d][1], Nrb[d], Nrk[d], U, Vtm], w=[pY[d]])
                    kb.op("act", lambda e, d=d, c=c: e.activation(out=Yd[d][:, c, :], in_=pY[d][:], func=AF.Copy), r=[pY[d]], w=[Yd[d]])
                    if dbg and step == 1 and d == 0 and bi == 0:
                        kb.dump(U, "d_U1"); kb.dump(Nrb[0], "d_Nrb"); kb.dump(Nrk[0], "d_Nrk"); kb.dump(RHd[0], "d_RH1")

                    def smm(e, d=d, c=c, U=U):
                        e.matmul(pS[d][:], lhsT=BKt[d][0][:], rhs=U[:], start=True, stop=False)
                        return e.matmul(pS[d][:], lhsT=BKt[d][1][:], rhs=Vtm[:, c, :], start=False, stop=True)
                    kb.op("pe", smm, r=[BKt[d][0], BKt[d][1], U, Vtm], w=[pS[d]])
                    for hh in range(2):
                        kb.op("dve", lambda e, d=d, c=c, hh=hh: e.scalar_tensor_tensor(out=ST[d][hh][HS[hh], :], in0=ST[d][hh][HS[hh], :], scalar=EL[d][HS[hh], c:c + 1], in1=pS[d][HS[hh], hh * 64:(hh + 1) * 64], op0=ALU.mult, op1=ALU.add), r=[ST[d][hh], EL[d], pS[d]], w=[ST[d][hh]])
                    if dbg and step == 0 and d == 0 and bi == 0:
                        kb.dump(BKt[0][0], "d_BKt0"); kb.dump(BKt[0][1], "d_BKt1"); kb.dump(EL[0], "d_EL"); kb.dump(U, "d_U"); kb.dump(BKb[0][0], "d_BKb0")
            if upto == "D":
                break
            if dbg:
                dbgo = kb.dram("dbgo", [2, NT, 128], kind="ExternalOutput")
                for d in range(2):
                    kb.dma("sp", dbgo[d].rearrange("(c p) n -> p c n", p=128), Yd[d][:], r=[Yd[d]], store=True)
                kb.barrier()
            Y = Yd[0]
            Yv = lambda: Y[:].rearrange("p c (h v) -> p (c h) v", v=64)
            NG = NCK * 2
            kb.op("dve", lambda e: e.tensor_tensor(out=Y[:], in0=Yd[0][:], in1=Yd[1][:], op=ALU.add), r=[Yd[0], Yd[1]], w=[Y])
            kb.op("dve", lambda e: e.tensor_reduce(out=st1[:], in_=Yv(), axis=AX.X, op=ALU.add), r=[Y], w=[st1])
            kb.op("dve", lambda e: e.tensor_scalar_mul(out=st1[:], in0=st1[:], scalar1=1.0 / 64), r=[st1], w=[st1])
            kb.op("dve", lambda e: e.tensor_tensor(out=Yv(), in0=Yv(), in1=st1[:].unsqueeze(2).to_broadcast([128, NG, 64]), op=ALU.subtract), r=[Y, st1], w=[Y])
            Y2 = Yd[1]
            kb.op("dve", lambda e: e.tensor_tensor(out=Y2[:], in0=Y[:], in1=Y[:], op=ALU.mult), r=[Y], w=[Y2])
            kb.op("dve", lambda e: e.tensor_reduce(out=st2[:], in_=Y2[:].rearrange("p c (h v) -> p (c h) v", v=64), axis=AX.X, op=ALU.add), r=[Y2], w=[st2])
            kb.op("dve", lambda e: e.tensor_scalar(out=st2[:], in0=st2[:], scalar1=1.0 / 64, scalar2=64e-5, op0=ALU.mult, op1=ALU.add), r=[st2], w=[st2])
            kb.op("act", lambda e: e.activation(out=st2[:], in_=st2[:], func=AF.Sqrt), r=[st2], w=[st2])
            kb.op("dve", lambda e: e.reciprocal(out=st2[:], in_=st2[:]), r=[st2], w=[st2])
            kb.op("dve", lambda e: e.tensor_tensor(out=Yv(), in0=Yv(), in1=st2[:].unsqueeze(2).to_broadcast([128, NG, 64]), op=ALU.mult), r=[Y, st2], w=[Y])
            kb.op("dve", lambda e: e.tensor_tensor(out=Y[:], in0=Y[:], in1=lnw[:, cs].unsqueeze(1).to_broadcast([128, NCK, 128]), op=ALU.mult), r=[Y, lnw], w=[Y])
            kb.op("dve", lambda e: e.tensor_tensor(out=Y[:], in0=Y[:], in1=lnb[:, cs].unsqueeze(1).to_broadcast([128, NCK, 128]), op=ALU.add), r=[Y, lnb], w=[Y])
            kb.op("dve", lambda e: e.tensor_tensor(out=Y2[:].rearrange("p c (h v) -> p (c h) v", v=64), in0=Vtm[:].rearrange("p c (h v) -> p (c h) v", v=64), in1=sbon[:].rearrange("p c h -> p (c h)").unsqueeze(2).to_broadcast([128, NG, 64]), op=ALU.mult), r=[Vtm, sbon], w=[Y2])
            kb.op("dve", lambda e: e.tensor_tensor(out=Y[:], in0=Y[:], in1=Y2[:], op=ALU.add), r=[Y, Y2], w=[Y])
            for c4 in range(0, NCK, 4):
                n4 = min(4, NCK - c4)
                g = gl[(c4 // 4) % 2]
                kb.dma("sp", g[:, :, 0:n4 * 128], LR[4:6].rearrange("q p t -> p q t")[:, :, c4 * 128:(c4 + n4) * 128], w=[g])

                def gmm(e, g=g, n4=n4):
                    ins = None
                    for cc in range(n4):
                        for kq in range(2):
                            ins = e.matmul(pW[:, cc * 128:(cc + 1) * 128], lhsT=g[:, kq, cc * 128:(cc + 1) * 128], rhs=g2[:, kq, cs], start=(kq == 0), stop=(kq == 1))
                    return ins
                kb.op("pe", gmm, r=[g, g2], w=[pW])
                kb.op("dve", lambda e, c4=c4, n4=n4: e.tensor_tensor(out=Y[:, c4:c4 + n4, :], in0=Y[:, c4:c4 + n4, :], in1=pW[:, 0:n4 * 128].rearrange("p (c n) -> p c n", n=128), op=ALU.mult), r=[Y, pW], w=[Y])
            kb.dma("sp", ya[:, cs].rearrange("(c p) n -> p c n", p=128), Y[:], r=[Y], store=True)
            kb.barrier()
    if upto in ("C", "D", "E"):
        return kb.finish()
    with kb.scope():
        lvec = kb.sb("lvec", [128, NLB, NVL]); kb.dma("sp", lvec[:], lvec_d[:], w=[lvec])
        c8 = kb.sb("c8", [128, NLB, 2])
        kb.op("act", lambda e: e.activation(out=c8[:], in_=lvec[:, :, 9:11], func=AF.Exp, scale=-1.0), r=[lvec], w=[c8])
        kb.op("act", lambda e: e.activation(out=c8[:], in_=c8[:], func=AF.Ln, bias=1.0), r=[c8], w=[c8])
        kb.op("dve", lambda e: e.tensor_scalar_mul(out=c8[:], in0=c8[:], scalar1=-8.0), r=[c8], w=[c8])
        wa = kb.sb("wa", [128, 2, NLB, 128]); wx = kb.sb("wx", [128, 2, NLB, 128])
        kb.dma("sp", wa[:], lwa_d[:].rearrange("d n c o -> c d n o"), w=[wa])
        kb.dma("sp", wx[:], lwx_d[:].rearrange("d n c o -> c d n o"), w=[wx])
        raw = kb.sb("lraw", [128, TW + 3]); XC = kb.sb("XC", [128, TW]); GB = kb.sb("GB", [128, TW])
        A_ = kb.sb("A_", [128, TW]); GI = kb.sb("GI", [128, TW]); U_ = kb.sb("U_", [128, TW]); H = kb.sb("H", [128, TW]); TT = kb.sb("TT", [128, TW])
        HF = kb.sb("HF", [128, NT]); hst = kb.sb("hst", [128, 1])
        pA = kb.ps("lpA", [128, 512]); pB = kb.ps("lpB", [128, 512])
        C2 = 0.7978845608028654 * 2.0
        ctx_t = [t for t in ctiles if not t[2]]; lat_t = [t for t in ctiles if t[2]]
        for lb in range(NLB):
            jx = NCHR + lb; jg = NCHR + NLB + lb
            L_ = lambda i: lvec[:, lb, i:i + 1]
            for d in range(2):
                tl = ctiles if d == 0 else ctx_t[::-1] + lat_t[::-1]
                first = True
                for (t0, W, lat) in tl:
                    s_lo, s_hi = (TC, NT) if lat else (0, TC)
                    lo = max(s_lo, t0 - 1); hi = min(s_hi, t0 + W + 2)
                    kb.op("pool", lambda e: e.memset(raw[:], 0.0), w=[raw])
                    kb.dma("sp", raw[:, 1 - (t0 - lo):1 - (t0 - lo) + (hi - lo)], P[jx][:, lo:hi], w=[raw])
                    kb.op("dve", lambda e: e.tensor_scalar(out=XC[:, 0:W], in0=raw[:, 0:W], scalar1=L_(0), scalar2=L_(4), op0=ALU.mult, op1=ALU.add), r=[raw, lvec], w=[XC])
                    for j in range(1, 4):
                        kb.op("dve", lambda e: e.scalar_tensor_tensor(out=XC[:, 0:W], in0=raw[:, j:j + W], scalar=L_(j), in1=XC[:, 0:W], op0=ALU.mult, op1=ALU.add), r=[raw, lvec, XC], w=[XC])
                    for s0 in range(0, W, 512):
                        sw = min(512, W - s0)
                        kb.op("pe", lambda e: e.matmul(pA[:, 0:sw], lhsT=wa[:, d, lb, :], rhs=XC[:, s0:s0 + sw], start=True, stop=True), r=[wa, XC], w=[pA])
                        kb.op("act", lambda e: e.activation(out=A_[:, s0:s0 + sw], in_=pA[:, 0:sw], func=AF.Sigmoid, bias=L_(5 + d), scale=1.0), r=[pA, lvec], w=[A_])
                        kb.op("pe", lambda e: e.matmul(pB[:, 0:sw], lhsT=wx[:, d, lb, :], rhs=XC[:, s0:s0 + sw], start=True, stop=True), r=[wx, XC], w=[pB])
                        kb.op("act", lambda e: e.activation(out=GI[:, s0:s0 + sw], in_=pB[:, 0:sw], func=AF.Sigmoid, bias=L_(7 + d), scale=1.0), r=[pB, lvec], w=[GI])
                    kb.op("act", lambda e: e.activation(out=A_[:, 0:W], in_=A_[:, 0:W], func=AF.Exp, scale=c8[:, lb, d:d + 1]), r=[A_, c8], w=[A_])
                    kb.op("dve", lambda e: e.tensor_tensor(out=U_[:, 0:W], in0=XC[:, 0:W], in1=GI[:, 0:W], op=ALU.mult), r=[XC, GI], w=[U_])
                    kb.op("pool", lambda e: e.tensor_tensor(out=TT[:, 0:W], in0=A_[:, 0:W], in1=A_[:, 0:W], op=ALU.mult), r=[A_], w=[TT])
                    kb.op("dve", lambda e: e.tensor_scalar(out=TT[:, 0:W], in0=TT[:, 0:W], scalar1=-1.0, scalar2=1.0, op0=ALU.mult, op1=ALU.add), r=[TT], w=[TT])
                    kb.op("act", lambda e: e.activation(out=TT[:, 0:W], in_=TT[:, 0:W], func=AF.Sqrt), r=[TT], w=[TT])
                    kb.op("dve", lambda e: e.tensor_tensor(out=U_[:, 0:W], in0=U_[:, 0:W], in1=TT[:, 0:W], op=ALU.mult), r=[U_, TT], w=[U_])
                    init = 0.0 if first else hst[:, 0:1]
                    rr = [A_, U_] + ([] if first else [hst])
                    if d == 0:
                        kb.op("dve", lambda e: e.tensor_tensor_scan(out=H[:, 0:W], data0=A_[:, 0:W], data1=U_[:, 0:W], initial=init, op0=ALU.mult, op1=ALU.add), r=rr, w=[H])
                        kb.op("dve", lambda e: e.tensor_copy(out=hst[:], in_=H[:, W - 1:W]), r=[H], w=[hst])
                        kb.op("pool", lambda e: e.tensor_copy(out=HF[:, t0:t0 + W], in_=H[:, 0:W]), r=[H], w=[HF])
                    else:
                        kb.op("dve", lambda e: e.tensor_tensor_scan(out=H[:, 0:W][:, ::-1], data0=A_[:, 0:W][:, ::-1], data1=U_[:, 0:W][:, ::-1], initial=init, op0=ALU.mult, op1=ALU.add), r=rr, w=[H])
                        kb.op("dve", lambda e: e.tensor_copy(out=hst[:], in_=H[:, 0:1]), r=[H], w=[hst])
                        kb.dma("sp", GB[:, 0:W], P[jg][:, t0:t0 + W], w=[GB])
                        kb.op("act", lambda e: e.activation(out=TT[:, 0:W], in_=GB[:, 0:W], func=AF.Square), r=[GB], w=[TT])
                        kb.op("dve", lambda e: e.tensor_scalar(out=TT[:, 0:W], in0=TT[:, 0:W], scalar1=C2 * 0.044715, scalar2=C2, op0=ALU.mult, op1=ALU.add), r=[TT], w=[TT])
                        kb.op("dve", lambda e: e.tensor_tensor(out=TT[:, 0:W], in0=TT[:, 0:W], in1=GB[:, 0:W], op=ALU.mult), r=[TT, GB], w=[TT])
                        kb.op("act", lambda e: e.activation(out=TT[:, 0:W], in_=TT[:, 0:W], func=AF.Sigmoid), r=[TT], w=[TT])
                        kb.op("dve", lambda e: e.tensor_tensor(out=GB[:, 0:W], in0=GB[:, 0:W], in1=TT[:, 0:W], op=ALU.mult), r=[GB, TT], w=[GB])
                        kb.op("dve", lambda e: e.tensor_tensor(out=H[:, 0:W], in0=H[:, 0:W], in1=HF[:, t0:t0 + W], op=ALU.add), r=[H, HF], w=[H])
                        kb.op("dve", lambda e: e.tensor_tensor(out=GB[:, 0:W], in0=GB[:, 0:W], in1=H[:, 0:W], op=ALU.mult), r=[GB, H], w=[GB])
                        kb.dma("sp", yb[lb * 128:(lb + 1) * 128, t0:t0 + W], GB[:, 0:W], r=[GB], store=True)
                    first = False
    nc = kb.finish()
    return nc


def sincos_tab(TL, D):
    quarter = D // 4
    omega = (10000.0 ** (-np.arange(quarter, dtype=np.float32) / quarter)).astype(np.float32)
    idx = np.arange(64, dtype=np.float32)
    ang = idx[:, None] * omega[None, :]
    blk = np.concatenate([np.sin(ang), np.cos(ang)], -1).astype(np.float32)
    return np.concatenate([blk, blk], -1).T.copy()


def _pm(v, KC):
    return np.ascontiguousarray(v.reshape(KC, 128).T)


def p1_core_inputs(b, g, x, ctx, mods0, prm, NBLK, NLB):
    D = x.shape[2]; KC = D // 128; TL = x.shape[1]
    RW = NBLK * 128; LW = NLB * 128
    AW = prm["w0"].shape[-1]; BW = prm["lam"].shape[-1]
    DL = prm["w2"].shape[1]; GLo = prm["g2"].shape[0]
    w_in = prm["w_in"]; mu_full = prm["mu"]
    base = 3 * AW
    chunks = []
    for q in range(3):
        for bi in range(NBLK):
            c0 = q * AW + g * RW + bi * 128
            chunks.append(np.arange(c0, c0 + 128))
    for q in range(4):
        chunks.append(np.arange(base + q * DL, base + (q + 1) * DL))
    for q in range(2):
        chunks.append(np.arange(base + 4 * DL + q * 128, base + 4 * DL + (q + 1) * 128))
    NCHR = len(chunks)
    rc = 3 * AW + 4 * DL + GLo
    for q in range(2):
        for lb in range(NLB):
            c0 = rc + q * BW + g * LW + lb * 128
            chunks.append(np.arange(c0, c0 + 128))
    NCH = len(chunks)
    win = np.zeros((NCH, D, 128), np.float32)
    mu = np.zeros((128, NCHR), np.float32)
    for j, cols in enumerate(chunks):
        win[j, :, :len(cols)] = w_in[:, cols]
        if j < NCHR:
            mu[:len(cols), j] = mu_full[cols]
    sl = slice(g * RW, (g + 1) * RW)
    w2 = np.zeros((2, 128, RW), np.float32); w2[:, :DL] = prm["w2"][:, :, sl]
    a2 = np.zeros((2, 128, RW), np.float32); a2[:, :DL] = prm["a2"][:, :, sl]
    g2 = np.ascontiguousarray(prm["g2"][:, sl].reshape(2, 128, RW))
    vecs = [prm["w0"][0], prm["w0"][1], prm["a0"][0], prm["a0"][1], prm["k_k"], prm["k_a"], prm["r_k"].reshape(-1)]
    rvec = np.stack([v[sl].reshape(NBLK, 128).T for v in vecs], -1).astype(np.float32)
    lnx = np.concatenate([prm["lnx_w"][sl], prm["lnx_b"][sl]])[None, :].astype(np.float32)
    ls = slice(g * LW, (g + 1) * LW)
    lv = [prm["conv_w"][i] for i in range(4)] + [prm["conv_b"], prm["ba"][0], prm["ba"][1], prm["bx"][0], prm["bx"][1], prm["lam"][0], prm["lam"][1]]
    lvec = np.stack([v[ls].reshape(NLB, 128).T for v in lv], -1).astype(np.float32)
    lwa = np.ascontiguousarray(prm["wa"][:, g * NLB:(g + 1) * NLB]); lwx = np.ascontiguousarray(prm["wx"][:, g * NLB:(g + 1) * NLB])
    m = mods0
    mods = np.stack([_pm(m[b, 0:D], KC), _pm(m[b, D:2 * D], KC), _pm(m[2, 0:D], KC), _pm(m[2, D:2 * D], KC)], -1).astype(np.float32)
    tab = sincos_tab(TL, D)
    ptab = np.ascontiguousarray(tab.reshape(KC, 128, 64).transpose(1, 0, 2))
    return {"xT": np.ascontiguousarray(x[b].T), "cT": np.ascontiguousarray(ctx[b].T), "ptab": ptab, "mods": mods,
            "win": win, "mu": mu, "w2": w2, "a2": a2, "g2": g2, "rvec": np.ascontiguousarray(rvec), "lnx": lnx,
            "lvec": np.ascontiguousarray(lvec), "lwa": lwa, "lwx": lwx, "cst": make_consts(), "cst2": make_consts2()}


ALPHA_DN = 4.0 ** 0.25
LN_EPS_ = 1e-5


def build_post(D=4096, KM=4096, segs=((1024, 0), (64, 1)), mode="proj", router=True, hnext=True):
    kb = KB()
    KC = D // 128
    NTK = sum(n for n, _ in segs)
    nsets = max(sset for _, sset in segs) + 1
    NBK = (NTK + 127) // 128
    tiles = []
    t = 0
    for si, (n, sset) in enumerate(segs):
        for t0 in range(0, n, 512):
            tiles.append((t + t0, min(512, n - t0), sset, si == 0, t0))
        t += n
    tinfo = {tl[0]: (i, tl) for i, tl in enumerate(tiles)}
    I = lambda name, shape, dt=F32: kb.dram(name, shape, dt, kind="ExternalInput")
    resT = I("resT", [D, NTK]); modp_d = I("modp", [128, KC, nsets, 3]); lnp_d = I("lnp", [128, KC, 2])
    NR0 = segs[0][0] // 64
    prow_d = I("prow", [128, KC // 2, NR0]); pcol_d = I("pcol", [128, KC // 2, 64])
    if mode == "proj":
        yT = I("yT", [KM, NTK]); wout = I("wout", [KC, KM, 128])
    else:
        y1T = I("y1T", [D, NTK]); y2T = I("y2T", [D, NTK]); g12_d = I("g12", [2, NTK])
    if router:
        rw_d = I("rw", [128, KC, 16]); rb_d = I("rb", [1, 16]); cst_d = I("cst", [128, 20, 128])
        Gout = kb.dram("G", [NBK * 128, 16], kind="ExternalOutput")
    res_out = kb.dram("res_out", [D, NTK], kind="ExternalOutput")
    if hnext:
        h_out = kb.dram("h_out", [D, NTK], BF16, kind="ExternalOutput")
    zT = kb.dram("zT", [D, NTK])
    modp = kb.sb("modp", [128, KC, nsets, 3]); kb.dma("sp", modp[:], modp_d[:], w=[modp])
    lnp = kb.sb("lnp", [128, KC, 2]); kb.dma("sp", lnp[:], lnp_d[:], w=[lnp])
    prow = kb.sb("prow", [128, KC // 2, NR0]); kb.dma("sp", prow[:], prow_d[:], w=[prow])
    pcol = kb.sb("pcol", [128, KC // 2, 64]); kb.dma("sp", pcol[:], pcol_d[:], w=[pcol])
    if hnext:
        kb.op("dve", lambda e: e.tensor_scalar_add(out=modp[:, :, :, 2:3], in0=modp[:, :, :, 2:3], scalar1=1.0), r=[modp], w=[modp])
    acc1 = kb.sb("acc1", [128, NTK]); acc2 = kb.sb("acc2", [128, NTK])
    kb.op("pool", lambda e: e.memset(acc1[:], 0.0), w=[acc1]); kb.op("pool", lambda e: e.memset(acc2[:], 0.0), w=[acc2])
    ones = kb.sb("ones", [128, 128]); kb.op("pool", lambda e: e.memset(ones[:], 1.0), w=[ones])
    rts = [kb.sb("rt%d" % i, [128, 512]) for i in range(3)]
    zts = [kb.sb("zt%d" % i, [128, 512]) for i in range(3)]
    sqs = [kb.sb("sq%d" % i, [128, 512]) for i in range(2)]
    cnt = [0]

    def zstage(j, t0, W, o_ap, o_tl):
        _, (tt0, _, sset, seg0, off) = tinfo[t0]
        k = cnt[0]; cnt[0] += 1
        rt = rts[k % 3]; zt = zts[k % 3]; sq = sqs[k % 2]
        kb.dma("sp", rt[:, 0:W], resT[j * 128:(j + 1) * 128, t0:t0 + W], w=[rt])
        if seg0:
            nr = W // 64; r0 = off // 64
            rv = rt[:, 0:W].rearrange("p (r q) -> p r q", q=64)
            if j < KC // 2:
                pin = prow[:, j, r0:r0 + nr].unsqueeze(2).to_broadcast([128, nr, 64])
            else:
                pin = pcol[:, j - KC // 2, :].unsqueeze(1).to_broadcast([128, nr, 64])
            kb.op("dve", lambda e: e.tensor_tensor(out=rv, in0=rv, in1=pin, op=ALU.add), r=[rt, prow, pcol], w=[rt])
        kb.op("act", lambda e: e.activation(out=rt[:, 0:W], in_=rt[:, 0:W], func=AF.Copy, scale=ALPHA_DN), r=[rt], w=[rt])
        kb.op("dve", lambda e: e.scalar_tensor_tensor(out=zt[:, 0:W], in0=o_ap, scalar=modp[:, j, sset, 0:1], in1=rt[:, 0:W], op0=ALU.mult, op1=ALU.add), r=[o_tl, modp, rt], w=[zt])
        kb.op("pool", lambda e: e.tensor_tensor(out=acc1[:, t0:t0 + W], in0=acc1[:, t0:t0 + W], in1=zt[:, 0:W], op=ALU.add), r=[acc1, zt], w=[acc1])
        kb.op("act", lambda e: e.activation(out=sq[:, 0:W], in_=zt[:, 0:W], func=AF.Square), r=[zt], w=[sq])
        kb.op("pool", lambda e: e.tensor_tensor(out=acc2[:, t0:t0 + W], in0=acc2[:, t0:t0 + W], in1=sq[:, 0:W], op=ALU.add), r=[acc2, sq], w=[acc2])
        kb.dma("sp", zT[j * 128:(j + 1) * 128, t0:t0 + W], zt[:, 0:W], r=[zt], store=True)

    gtiles = [(t0, W, True) for (t0, W, _, _, _) in tiles]
    if mode == "proj":
        yTb = kb.dram("yTb", [KM, NTK], BF16)
        KMC = KM // 128
        with kb.scope():
            GA = min(8, KMC)
            xs = [kb.sb("cxs%d" % i, [128, GA, 512]) for i in range(2)]
            hb = [kb.sb("chb%d" % i, [128, GA, 512], BF16) for i in range(2)]
            yv = yT[:].rearrange("(c p) t -> p c t", p=128); ybv = yTb[:].rearrange("(c p) t -> p c t", p=128)
            it = 0
            for (t0, W, _) in gtiles:
                for g in range(KMC // GA):
                    x = xs[it % 2]; h = hb[it % 2]; it += 1
                    kb.dma("sp", x[:, :, 0:W], yv[:, g * GA:(g + 1) * GA, t0:t0 + W], w=[x])
                    kb.op("act" if it % 2 else "dve", (lambda e: e.activation(out=h[:, :, 0:W], in_=x[:, :, 0:W], func=AF.Copy)) if it % 2 else (lambda e: e.tensor_copy(out=h[:, :, 0:W], in_=x[:, :, 0:W])), r=[x], w=[h])
                    kb.dma("sp", ybv[:, g * GA:(g + 1) * GA, t0:t0 + W], h[:, :, 0:W], r=[h], store=True)
        gemm_fm(kb, wout, KC, KM, yTb, NTK, gtiles, lambda j, t0, W, p: zstage(j, t0, W, p[:, 0:W], p))
    else:
        with kb.scope():
            G1 = kb.sb("G1b", [128, NTK]); G2 = kb.sb("G2b", [128, NTK])
            kb.dma("sp", G1[:], g12_d[0:1, :].partition_broadcast(128), w=[G1])
            kb.dma("sp", G2[:], g12_d[1:2, :].partition_broadcast(128), w=[G2])
            y1s = [kb.sb("y1s%d" % i, [128, 512]) for i in range(2)]; y2s = [kb.sb("y2s%d" % i, [128, 512]) for i in range(2)]
            n = 0
            for (t0, W, _) in gtiles:
                for j in range(KC):
                    a = y1s[n % 2]; b = y2s[n % 2]; n += 1
                    kb.dma("sp", a[:, 0:W], y1T[j * 128:(j + 1) * 128, t0:t0 + W], w=[a])
                    kb.dma("act", b[:, 0:W], y2T[j * 128:(j + 1) * 128, t0:t0 + W], w=[b])
                    kb.op("dve", lambda e: e.tensor_tensor(out=a[:, 0:W], in0=a[:, 0:W], in1=G1[:, t0:t0 + W], op=ALU.mult), r=[a, G1], w=[a])
                    kb.op("pool", lambda e: e.tensor_tensor(out=b[:, 0:W], in0=b[:, 0:W], in1=G2[:, t0:t0 + W], op=ALU.mult), r=[b, G2], w=[b])
                    kb.op("dve", lambda e: e.tensor_tensor(out=a[:, 0:W], in0=a[:, 0:W], in1=b[:, 0:W], op=ALU.add), r=[a, b], w=[a])
                    zstage(j, t0, W, a[:, 0:W], a)
    kb.barrier()
    with kb.scope():
        pst = kb.ps("pst", [128, 512]); pr = kb.ps("pr", [128, 512]); ptr = kb.ps("ptr", [128, 512])
        mean = kb.sb("mean", [128, NTK]); rstd = kb.sb("rstd", [128, NTK]); tmp = kb.sb("tmpst", [128, 512])
        if router:
            rw = kb.sb("rw", [128, KC, 16]); kb.dma("sp", rw[:], rw_d[:], w=[rw])
            rbb = kb.sb("rbb", [128, 16]); kb.dma("sp", rbb[:], rb_d[:].partition_broadcast(128), w=[rbb])
            cst = kb.sb("cstp", [128, 128]); kb.dma("sp", cst[:], cst_d[:, 0, :], w=[cst])
            lg = kb.sb("lg", [16, 512])
            A = kb.sb("Aaff", [128, NBK, 16]); kb.op("pool", lambda e: e.memset(A[:], 0.0), w=[A])
        zl = [kb.sb("zl%d" % i, [128, 512]) for i in range(3)]
        hf = [kb.sb("hf%d" % i, [128, 512]) for i in range(2)]
        hbf = [kb.sb("hbf%d" % i, [128, 512], BF16) for i in range(2)]
        n = 0
        for (t0, W, sset, seg0, off) in tiles:
            kb.op("pe", lambda e: e.matmul(pst[:, 0:W], lhsT=ones[:], rhs=acc1[:, t0:t0 + W], start=True, stop=True), r=[ones, acc1], w=[pst])
            kb.op("act", lambda e: e.activation(out=mean[:, t0:t0 + W], in_=pst[:, 0:W], func=AF.Copy, scale=1.0 / D), r=[pst], w=[mean])
            kb.op("pe", lambda e: e.matmul(pst[:, 0:W], lhsT=ones[:], rhs=acc2[:, t0:t0 + W], start=True, stop=True), r=[ones, acc2], w=[pst])
            kb.op("dve", lambda e: e.tensor_tensor(out=tmp[:, 0:W], in0=mean[:, t0:t0 + W], in1=mean[:, t0:t0 + W], op=ALU.mult), r=[mean], w=[tmp])
            kb.op("dve", lambda e: e.scalar_tensor_tensor(out=rstd[:, t0:t0 + W], in0=pst[:, 0:W], scalar=1.0 / D, in1=tmp[:, 0:W], op0=ALU.mult, op1=ALU.subtract), r=[pst, tmp], w=[rstd])
            kb.op("dve", lambda e: e.tensor_scalar_add(out=rstd[:, t0:t0 + W], in0=rstd[:, t0:t0 + W], scalar1=LN_EPS_), r=[rstd], w=[rstd])
            kb.op("act", lambda e: e.activation(out=rstd[:, t0:t0 + W], in_=rstd[:, t0:t0 + W], func=AF.Sqrt), r=[rstd], w=[rstd])
            kb.op("dve", lambda e: e.reciprocal(out=rstd[:, t0:t0 + W], in_=rstd[:, t0:t0 + W]), r=[rstd], w=[rstd])
            for j in range(KC):
                z = zl[n % 3]; h = hf[n % 2]; hb_ = hbf[n % 2]; n += 1
                kb.dma("sp", z[:, 0:W], zT[j * 128:(j + 1) * 128, t0:t0 + W], w=[z])
                kb.op("dve", lambda e: e.tensor_tensor(out=z[:, 0:W], in0=z[:, 0:W], in1=mean[:, t0:t0 + W], op=ALU.subtract), r=[z, mean], w=[z])
                kb.op("pool", lambda e: e.tensor_tensor(out=z[:, 0:W], in0=z[:, 0:W], in1=rstd[:, t0:t0 + W], op=ALU.mult), r=[z, rstd], w=[z])
                kb.op("act", lambda e: e.activation(out=z[:, 0:W], in_=z[:, 0:W], func=AF.Identity, scale=lnp[:, j, 0:1], bias=lnp[:, j, 1:2]), r=[z, lnp], w=[z])
                kb.dma("sp", res_out[j * 128:(j + 1) * 128, t0:t0 + W], z[:, 0:W], r=[z], store=True)
                if hnext:
                    kb.op("dve", lambda e: e.tensor_scalar(out=h[:, 0:W], in0=z[:, 0:W], scalar1=modp[:, j, sset, 2:3], scalar2=modp[:, j, sset, 1:2], op0=ALU.mult, op1=ALU.add), r=[z, modp], w=[h])
                    kb.op("act", lambda e: e.activation(out=hb_[:, 0:W], in_=h[:, 0:W], func=AF.Copy), r=[h], w=[hb_])
                    kb.dma("act", h_out[j * 128:(j + 1) * 128, t0:t0 + W], hb_[:, 0:W], r=[hb_], store=True)
                    if router:
                        kb.op("pe", lambda e: e.matmul(pr[0:16, 0:W], lhsT=rw[:, j, :], rhs=h[:, 0:W], start=(j == 0), stop=(j == KC - 1)), r=[rw, h], w=[pr])
            if router:
                kb.op("dve", lambda e: e.tensor_copy(out=lg[:, 0:W], in_=pr[0:16, 0:W]), r=[pr], w=[lg])
                for b0 in range(0, W, 128):
                    bw = min(128, W - b0); blk = (t0 + b0) // 128
                    kb.op("pe", lambda e: e.transpose(ptr[0:bw, 0:16], lg[:, b0:b0 + bw], cst[0:16, 0:16]), r=[lg, cst], w=[ptr])
                    kb.op("act", lambda e: e.activation(out=A[0:bw, blk, :], in_=ptr[0:bw, 0:16], func=AF.Sigmoid), r=[ptr], w=[A])
        if router:
            T = lambda nm, shp: kb.sb(nm, shp)
            Bz = T("Bz", [128, NBK, 16]); gs = T("gs", [128, NBK, 4]); ps_ = T("pairs", [128, NBK, 4]); gm = T("gm", [128, NBK]); ing = T("ing", [128, NBK, 4])
            mb = T("mb", [128, NBK, 16]); mbias = T("mbias", [128, NBK, 4]); m1 = T("m1", [128, NBK]); s1 = T("s1", [128, NBK, 16]); s2 = T("s2", [128, NBK, 16]); gsum = T("gsum", [128, NBK])
            kb.op("dve", lambda e: e.tensor_tensor(out=Bz[:], in0=A[:], in1=rbb[:].unsqueeze(1).to_broadcast([128, NBK, 16]), op=ALU.add), r=[A, rbb], w=[Bz])
            B4 = Bz[:].rearrange("p b (g m) -> p b g m", m=4)
            first = True
            for i in range(4):
                for jx in range(i + 1, 4):
                    dst = gs if first else ps_
                    kb.op("dve", lambda e: e.tensor_tensor(out=dst[:], in0=B4[:, :, :, i], in1=B4[:, :, :, jx], op=ALU.add), r=[Bz], w=[dst])
                    if not first:
                        kb.op("dve", lambda e: e.tensor_tensor(out=gs[:], in0=gs[:], in1=ps_[:], op=ALU.max), r=[gs, ps_], w=[gs])
                    first = False
            kb.op("dve", lambda e: e.tensor_reduce(out=gm[:], in_=gs[:], axis=AX.X, op=ALU.max), r=[gs], w=[gm])
            kb.op("dve", lambda e: e.tensor_tensor(out=ing[:], in0=gs[:], in1=gm[:].unsqueeze(2).to_broadcast([128, NBK, 4]), op=ALU.is_equal), r=[gs, gm], w=[ing])
            kb.op("dve", lambda e: e.tensor_scalar(out=mbias[:], in0=ing[:], scalar1=1e30, scalar2=-1e30, op0=ALU.mult, op1=ALU.add), r=[ing], w=[mbias])
            M4 = mb[:].rearrange("p b (g m) -> p b g m", m=4)
            kb.op("dve", lambda e: e.tensor_tensor(out=M4, in0=B4, in1=ing[:].unsqueeze(3).to_broadcast([128, NBK, 4, 4]), op=ALU.mult), r=[Bz, ing], w=[mb])
            kb.op("dve", lambda e: e.tensor_tensor(out=M4, in0=M4, in1=mbias[:].unsqueeze(3).to_broadcast([128, NBK, 4, 4]), op=ALU.add), r=[mb, mbias], w=[mb])
            bc16 = lambda t_: t_[:].unsqueeze(2).to_broadcast([128, NBK, 16])
            kb.op("dve", lambda e: e.tensor_reduce(out=m1[:], in_=mb[:], axis=AX.X, op=ALU.max), r=[mb], w=[m1])
            kb.op("dve", lambda e: e.tensor_tensor(out=s1[:], in0=mb[:], in1=bc16(m1), op=ALU.is_equal), r=[mb, m1], w=[s1])
            kb.op("dve", lambda e: e.scalar_tensor_tensor(out=mb[:], in0=s1[:], scalar=-1e30, in1=mb[:], op0=ALU.mult, op1=ALU.add), r=[s1, mb], w=[mb])
            kb.op("dve", lambda e: e.tensor_reduce(out=m1[:], in_=mb[:], axis=AX.X, op=ALU.max), r=[mb], w=[m1])
            kb.op("dve", lambda e: e.tensor_tensor(out=s2[:], in0=mb[:], in1=bc16(m1), op=ALU.is_equal), r=[mb, m1], w=[s2])
            kb.op("dve", lambda e: e.tensor_tensor(out=s1[:], in0=s1[:], in1=s2[:], op=ALU.add), r=[s1, s2], w=[s1])
            kb.op("dve", lambda e: e.tensor_tensor(out=s1[:], in0=s1[:], in1=A[:], op=ALU.mult), r=[s1, A], w=[s1])
            kb.op("dve", lambda e: e.tensor_reduce(out=gsum[:], in_=s1[:], axis=AX.X, op=ALU.add), r=[s1], w=[gsum])
            kb.op("dve", lambda e: e.tensor_scalar_max(out=gsum[:], in0=gsum[:], scalar1=1e-30), r=[gsum], w=[gsum])
            kb.op("dve", lambda e: e.reciprocal(out=gsum[:], in_=gsum[:]), r=[gsum], w=[gsum])
            kb.op("dve", lambda e: e.tensor_tensor(out=s1[:], in0=s1[:], in1=bc16(gsum), op=ALU.mult), r=[s1, gsum], w=[s1])
            kb.dma("sp", Gout[:].rearrange("(b p) e -> p b e", p=128), s1[:], r=[s1], store=True)
    return kb.finish()


def build_ffn(D=4096, DE=1024, R=1536):
    kb = KB()
    I = lambda name, shape, dt=F32: kb.dram(name, shape, dt, kind="ExternalInput")
    XT = I("XT", [D, R], BF16)
    wgu = I("wgu", [2 * DE // 128, D, 128])
    wd = I("wd", [D // 128, DE, 128])
    YT = kb.dram("YT", [D, R], kind="ExternalOutput")
    HT = kb.dram("HT", [DE, R], BF16)
    tiles = [(t0, min(512, R - t0), True) for t0 in range(0, R, 512)]
    with kb.scope():
        sg = [kb.sb("sg%d" % i, [128, 512]) for i in range(2)]
        hb = [kb.sb("hbb%d" % i, [128, 512], BF16) for i in range(2)]

        def ev1(j, t0, W, p):
            q = j // 2
            if j % 2 == 0:
                kb.op("act", lambda e: e.activation(out=sg[q % 2][:, 0:W], in_=p[:, 0:W], func=AF.Silu), r=[p], w=[sg[q % 2]])
            else:
                kb.op("dve", lambda e: e.tensor_tensor(out=hb[q % 2][:, 0:W], in0=p[:, 0:W], in1=sg[q % 2][:, 0:W], op=ALU.mult), r=[p, sg[q % 2]], w=[hb[q % 2]])
                kb.dma("sp", HT[q * 128:(q + 1) * 128, t0:t0 + W], hb[q % 2][:, 0:W], r=[hb[q % 2]], store=True)
        gemm_fm(kb, wgu, 2 * DE // 128, D, XT, R, tiles, ev1)
    with kb.scope():
        ob = [kb.sb("fob%d" % i, [128, 512]) for i in range(4)]
        n = [0]

        def ev2(j, t0, W, p):
            o = ob[n[0] % 4]; n[0] += 1
            if n[0] % 2:
                kb.op("act", lambda e: e.activation(out=o[:, 0:W], in_=p[:, 0:W], func=AF.Copy), r=[p], w=[o])
            else:
                kb.op("dve", lambda e: e.tensor_copy(out=o[:, 0:W], in_=p[:, 0:W]), r=[p], w=[o])
            kb.dma("sp", YT[j * 128:(j + 1) * 128, t0:t0 + W], o[:, 0:W], r=[o], store=True)
        gemm_fm(kb, wd, D // 128, DE, HT, R, tiles, ev2)
    return kb.finish()


def build_p5(D=4096, TC=256, TL=4096, NB=2):
    kb = KB()
    NT = TC + TL
    NTB = NB * NT
    NCK = NT // 128
    CKC = TC // 128
    NCH = 13
    I = lambda name, shape, dt=F32: kb.dram(name, shape, dt, kind="ExternalInput")
    hT = I("hT", [D, NTB], BF16); win = I("win", [NCH, D, 128]); cst_d = I("cst", [128, 20, 128])
    gb_d = I("gbias", [1, 4]); nw_d = I("nw", [1, 1024])
    Y = kb.dram("Y", [NB * TL, 512], kind="ExternalOutput")
    P = kb.dram("P5P", [NCH, 128, NTB]); PB = kb.dram("P5PB", [4, 128, NTB], BF16)
    KTM = kb.dram("KTM", [NB * NCK, 128, 256], BF16); VTM = kb.dram("VTM", [NB * NCK, 128, 512], BF16)
    OTM = kb.dram("OTM", [NB * NCK, 128, 512]); HF = kb.dram("HFs", [NB * NCK, 128, 512])
    tiles = []
    for b in range(NB):
        tiles += [(b * NT + t0, W, lat) for (t0, W, lat) in seq_tiles(TC, TL, 512)]
    with kb.scope():
        obs = [kb.sb("ob%d" % i, [128, 512]) for i in range(4)]
        obb = [kb.sb("obb%d" % i, [128, 512], BF16) for i in range(2)]
        n = [0]

        def ev(j, t0, W, p):
            o = obs[n[0] % 4]; n[0] += 1
            kb.op("act" if n[0] % 2 else "dve", (lambda e: e.activation(out=o[:, 0:W], in_=p[:, 0:W], func=AF.Copy)) if n[0] % 2 else (lambda e: e.tensor_copy(out=o[:, 0:W], in_=p[:, 0:W])), r=[p], w=[o])
            kb.dma("sp", P[j][:, t0:t0 + W], o[:, 0:W], r=[o], store=True)
            if j < 4:
                ob_ = obb[n[0] % 2]
                kb.op("pool", lambda e: e.tensor_copy(out=ob_[:, 0:W], in_=o[:, 0:W]), r=[o], w=[ob_])
                kb.dma("sp", PB[j][:, t0:t0 + W], ob_[:, 0:W], r=[ob_], store=True)
        gemm_fm(kb, win, NCH, D, hT, NTB, tiles, ev, GS=7)
    cst = kb.sb("cst", [128, 20, 128]); kb.dma("sp", cst[:], cst_d[:], w=[cst])
    IDN, ML_I, MU_I = cst[:, 0, :], cst[:, 3, :], cst[:, 4, :]
    TRI = [MU_I, ML_I]
    capm = [kb.sb("cap%d" % d, [128, 128]) for d in range(2)]
    for d in range(2):
        kb.op("dve", lambda e: e.tensor_scalar(out=capm[d][:], in0=TRI[d], scalar1=2e30, scalar2=-1e30, op0=ALU.mult, op1=ALU.add), r=[cst], w=[capm[d]])
    ones = kb.sb("ones", [128, 128]); kb.op("pool", lambda e: e.memset(ones[:], 1.0), w=[ones])
    onesb = kb.sb("onesb", [128, 1], BF16); kb.op("pool", lambda e: e.memset(onesb[:], 1.0), w=[onesb])
    G = kb.sb("Gtm", [128, NB * NCK, 4])
    banks = [kb.ps("bank%d" % i, [128, 512]) for i in range(8)]
    bk = lambda i, a, b_: Tl(banks[i].t[:, a:b_], banks[i].key)
    with kb.scope():
        ld = [kb.sb("tld%d" % i, [128, 11, 128]) for i in range(2)]
        kt = [kb.sb("tkt%d" % i, [128, 256], BF16) for i in range(2)]
        vt = [kb.sb("tvt%d" % i, [128, 512], BF16) for i in range(2)]
        ot = [kb.sb("tot%d" % i, [128, 512]) for i in range(2)]
        for cg in range(NB * NCK):
            l = ld[cg % 2]; k_ = kt[cg % 2]; v_ = vt[cg % 2]; o_ = ot[cg % 2]
            is_lat = (cg % NCK) >= CKC
            kb.dma("sp", l[:], P[2:13].rearrange("j p t -> p j t")[:, :, cg * 128:(cg + 1) * 128], w=[l])
            pk = bk(0, 0, 256); pv = banks[1]; po = banks[2]; pg = bk(3, 0, 128)

            def tr(e, dst, j0, nj):
                ins = None
                for q in range(nj):
                    ins = e.transpose(dst[:, q * 128:(q + 1) * 128], l[:, j0 + q, :], IDN)
                return ins
            kb.op("pe", lambda e: tr(e, pk, 0, 2), r=[l, cst], w=[pk])
            kb.op("act", lambda e: e.activation(out=k_[:], in_=pk[:], func=AF.Copy), r=[pk], w=[k_])
            kb.dma("sp", KTM[cg], k_[:], r=[k_], store=True)
            kb.op("pe", lambda e: tr(e, pv, 2, 4), r=[l, cst], w=[pv])
            kb.op("dve", lambda e: e.tensor_copy(out=v_[:], in_=pv[:]), r=[pv], w=[v_])
            kb.dma("sp", VTM[cg], v_[:], r=[v_], store=True)
            if is_lat:
                kb.op("pe", lambda e: tr(e, po, 6, 4), r=[l, cst], w=[po])
                kb.op("act", lambda e: e.activation(out=o_[:], in_=po[:], func=AF.Sigmoid), r=[po], w=[o_])
                kb.dma("sp", OTM[cg], o_[:], r=[o_], store=True)
            kb.op("pe", lambda e: tr(e, pg, 10, 1), r=[l, cst], w=[pg])
            kb.op("dve", lambda e: e.tensor_copy(out=G[:, cg, :], in_=pg[:, 0:4]), r=[pg], w=[G])
    gbb = kb.sb("gbb", [128, 4]); kb.dma("sp", gbb[:], gb_d[:].partition_broadcast(128), w=[gbb])
    SC = kb.sb("SC", [128, NB * NCK, 4]); NLF = kb.sb("NLF", [128, NB * NCK, 4])
    kb.op("dve", lambda e: e.tensor_tensor(out=SC[:], in0=G[:], in1=gbb[:].unsqueeze(1).to_broadcast([128, NB * NCK, 4]), op=ALU.add), r=[G, gbb], w=[SC])
    kb.op("act", lambda e: e.activation(out=SC[:], in_=SC[:], func=AF.Tanh, scale=1.0 / 15.0), r=[SC], w=[SC])
    kb.op("dve", lambda e: e.tensor_scalar_mul(out=SC[:], in0=SC[:], scalar1=15.0), r=[SC], w=[SC])
    kb.op("act", lambda e: e.activation(out=NLF[:], in_=SC[:], func=AF.Exp, scale=-1.0), r=[SC], w=[NLF])
    kb.op("act", lambda e: e.activation(out=NLF[:], in_=NLF[:], func=AF.Ln, bias=1.0), r=[NLF], w=[NLF])
    nwb = kb.sb("nwb", [128, 1024]); kb.dma("sp", nwb[:], nw_d[:].partition_broadcast(128), w=[nwb])
    kb.barrier()
    C = kb.sb("Cst", [128, 2, 512]); Cb = kb.sb("Cbf", [128, 2, 512], BF16); nst = kb.sb("nst", [128, 2]); nb16 = kb.sb("nb16", [128, 2], BF16)
    qk = [kb.sb("qk%d" % i, [128, 4, 128], BF16) for i in range(2)]
    ktm = [kb.sb("ktm%d" % i, [128, 256], BF16) for i in range(2)]
    vtm = [kb.sb("vtm%d" % i, [128, 512], BF16) for i in range(2)]
    kw = kb.sb("kw", [128, 256], BF16)
    rows = kb.sb("rows", [1, 256]); cols = kb.sb("cols", [128, 2]); ew = kb.sb("ew", [128, 1]); eb = kb.sb("eb", [128, 1]); ebL = kb.sb("ebL", [128, 1])
    Dm = kb.sb("Dm", [128, 128]); SD = kb.sb("SD", [128, 128], BF16)
    tq = kb.sb("tq", [128, 512]); Hc = kb.sb("Hc", [128, 512]); hf = kb.sb("hfl", [128, 512]); ol = kb.sb("ol", [128, 512]); sqj = kb.sb("sqj", [128, 512])
    dn = kb.sb("dn", [128, 2]); st = kb.sb("stt", [128, 4])
    pR = bk(0, 0, 256); pCo = bk(0, 256, 258); pD = bk(1, 0, 128); pSc = bk(1, 128, 256)
    pN = banks[2]; pQ = banks[3]; pDen = bk(4, 0, 2); pNn = bk(4, 2, 4); pC0 = banks[5]; pC1 = banks[6]
    order = [list(range(NCK)), list(range(CKC - 1, -1, -1)) + list(range(NCK - 1, CKC - 1, -1))]
    it = 0
    for b in range(NB):
        for d in range(2):
            kb.op("pool", lambda e: e.memset(C[:], 0.0), w=[C]); kb.op("pool", lambda e: e.memset(Cb[:], 0.0), w=[Cb])
            kb.op("pool", lambda e: e.memset(nst[:], 0.0), w=[nst]); kb.op("pool", lambda e: e.memset(nb16[:], 0.0), w=[nb16])
            for c in order[d]:
                cg = b * NCK + c
                lat = c >= CKC
                q_ = qk[it % 2]; k_ = ktm[it % 2]; v_ = vtm[it % 2]; it += 1
                kb.dma("sp", q_[:], PB[:].rearrange("j p t -> p j t")[:, :, cg * 128:(cg + 1) * 128], w=[q_])
                kb.dma("act", k_[:], KTM[cg], w=[k_]); kb.dma("sp", v_[:], VTM[cg], w=[v_])
                igc = SC[:, cg, 2 * d:2 * d + 1]; nlf = NLF[:, cg, 2 * d + 1:2 * d + 2]

                def small(e):
                    e.matmul(pR[0:1, 0:128], lhsT=nlf, rhs=TRI[d], start=True, stop=True)
                    e.matmul(pR[0:1, 128:256], lhsT=igc, rhs=IDN, start=True, stop=False)
                    e.matmul(pR[0:1, 128:256], lhsT=nlf, rhs=TRI[d], start=False, stop=True)
                    e.matmul(pCo[:, 0:1], lhsT=TRI[d], rhs=nlf, start=True, stop=True)
                    return e.matmul(pCo[:, 1:2], lhsT=ones[:], rhs=nlf, start=True, stop=True)
                kb.op("pe", small, r=[NLF, SC, cst, ones], w=[pR])
                kb.op("act", lambda e: e.activation(out=rows[0:1, 0:128], in_=pR[0:1, 0:128], func=AF.Copy, scale=-1.0), r=[pR], w=[rows])
                kb.op("act", lambda e: e.activation(out=rows[0:1, 128:256], in_=pR[0:1, 128:256], func=AF.Copy), r=[pR], w=[rows])
                kb.op("dve", lambda e: e.tensor_copy(out=cols[:], in_=pCo[:, 0:2]), r=[pR], w=[cols])
                kb.op("dve", lambda e: e.scalar_tensor_tensor(out=ew[:], in0=cols[:, 0:1], scalar=igc, in1=cols[:, 1:2], op0=ALU.add, op1=ALU.subtract), r=[cols, SC], w=[ew])
                kb.op("act", lambda e: e.activation(out=ew[:], in_=ew[:], func=AF.Exp), r=[ew], w=[ew])
                kb.op("act", lambda e: e.activation(out=ebL[:], in_=cols[:, 1:2], func=AF.Exp, scale=-1.0), r=[cols], w=[ebL])
                if lat:
                    kb.op("act", lambda e: e.activation(out=eb[:], in_=cols[:, 0:1], func=AF.Exp, scale=-1.0), r=[cols], w=[eb])
                    kb.op("dve", lambda e: e.tensor_scalar_mul(out=eb[:], in0=eb[:], scalar1=1.0 / 16.0), r=[eb], w=[eb])

                    def dlog(e):
                        e.matmul(pD[:], lhsT=ones[0:1, :], rhs=rows[0:1, 0:128], start=True, stop=False)
                        return e.matmul(pD[:], lhsT=rows[0:1, 128:256], rhs=ones[0:1, :], start=False, stop=True)
                    kb.op("pe", dlog, r=[rows, ones], w=[pD])
                    kb.op("dve", lambda e: e.tensor_tensor(out=Dm[:], in0=pD[:], in1=capm[d][:], op=ALU.min), r=[pD, capm[d]], w=[Dm])
                    kb.op("act", lambda e: e.activation(out=Dm[:], in_=Dm[:], func=AF.Exp), r=[Dm], w=[Dm])

                    def scm(e):
                        e.matmul(pSc[:], lhsT=q_[:, 2, :], rhs=q_[:, 0, :], start=True, stop=False)
                        return e.matmul(pSc[:], lhsT=q_[:, 3, :], rhs=q_[:, 1, :], start=False, stop=True)
                    kb.op("pe", scm, r=[q_], w=[pD])
                    kb.op("dve", lambda e: e.scalar_tensor_tensor(out=SD[:], in0=pSc[:], scalar=1.0 / 16.0, in1=Dm[:], op0=ALU.mult, op1=ALU.mult), r=[pD, Dm], w=[SD])
                    kb.op("pe", lambda e: e.matmul(pN[:], lhsT=SD[:], rhs=v_[:], start=True, stop=True), r=[SD, v_], w=[pN])

                    def qc(e):
                        e.matmul(pQ[:], lhsT=q_[:, 0, :], rhs=Cb[:, 0, :], start=True, stop=False)
                        return e.matmul(pQ[:], lhsT=q_[:, 1, :], rhs=Cb[:, 1, :], start=False, stop=True)
                    kb.op("pe", qc, r=[q_, Cb], w=[pQ])

                    def den(e):
                        e.matmul(pDen[:, 0:1], lhsT=SD[:], rhs=onesb[:], start=True, stop=True)
                        e.matmul(pDen[:, 1:2], lhsT=q_[:, 0, :], rhs=nb16[:, 0:1], start=True, stop=False)
                        return e.matmul(pDen[:, 1:2], lhsT=q_[:, 1, :], rhs=nb16[:, 1:2], start=False, stop=True)
                    kb.op("pe", den, r=[SD, onesb, q_, nb16], w=[pDen])
                    kb.op("act", lambda e: e.activation(out=tq[:], in_=pQ[:], func=AF.Identity, scale=eb[:, 0:1]), r=[pQ, eb], w=[tq])
                    kb.op("dve", lambda e: e.tensor_tensor(out=Hc[:], in0=pN[:], in1=tq[:], op=ALU.add), r=[pN, tq], w=[Hc])
                    kb.op("dve", lambda e: e.tensor_copy(out=dn[:], in_=pDen[:, 0:2]), r=[pDen], w=[dn])
                    kb.op("dve", lambda e: e.scalar_tensor_tensor(out=dn[:, 0:1], in0=dn[:, 1:2], scalar=eb[:, 0:1], in1=dn[:, 0:1], op0=ALU.mult, op1=ALU.add), r=[dn, eb], w=[dn])
                    kb.op("dve", lambda e: e.tensor_scalar_mul(out=dn[:, 1:2], in0=dn[:, 0:1], scalar1=-1.0), r=[dn], w=[dn])
                    kb.op("dve", lambda e: e.tensor_tensor(out=dn[:, 0:1], in0=dn[:, 0:1], in1=dn[:, 1:2], op=ALU.max), r=[dn], w=[dn])
                    kb.op("dve", lambda e: e.tensor_scalar_max(out=dn[:, 0:1], in0=dn[:, 0:1], scalar1=1.0), r=[dn], w=[dn])
                    kb.op("dve", lambda e: e.reciprocal(out=dn[:, 0:1], in_=dn[:, 0:1]), r=[dn], w=[dn])
                    kb.op("dve", lambda e: e.tensor_scalar_mul(out=Hc[:], in0=Hc[:], scalar1=dn[:, 0:1]), r=[Hc, dn], w=[Hc])
                    if d == 0:
                        kb.dma("sp", HF[cg], Hc[:], r=[Hc], store=True)
                    else:
                        kb.dma("sp", hf[:], HF[cg], w=[hf]); kb.dma("act", ol[:], OTM[cg], w=[ol])
                        kb.op("dve", lambda e: e.tensor_tensor(out=Hc[:], in0=Hc[:], in1=hf[:], op=ALU.add), r=[Hc, hf], w=[Hc])
                        kb.op("dve", lambda e: e.tensor_reduce(out=st[:, 0:1], in_=Hc[:], axis=AX.X, op=ALU.add), r=[Hc], w=[st])
                        kb.op("dve", lambda e: e.tensor_scalar_mul(out=st[:, 0:1], in0=st[:, 0:1], scalar1=1.0 / 512), r=[st], w=[st])
                        kb.op("dve", lambda e: e.tensor_scalar_sub(out=Hc[:], in0=Hc[:], scalar1=st[:, 0:1]), r=[Hc, st], w=[Hc])
                        kb.op("dve", lambda e: e.tensor_tensor(out=sqj[:], in0=Hc[:], in1=Hc[:], op=ALU.mult), r=[Hc], w=[sqj])
                        kb.op("dve", lambda e: e.tensor_reduce(out=st[:, 1:2], in_=sqj[:], axis=AX.X, op=ALU.add), r=[sqj], w=[st])
                        kb.op("dve", lambda e: e.tensor_scalar(out=st[:, 1:2], in0=st[:, 1:2], scalar1=1.0 / 512, scalar2=1e-6, op0=ALU.mult, op1=ALU.add), r=[st], w=[st])
                        kb.op("act", lambda e: e.activation(out=st[:, 1:2], in_=st[:, 1:2], func=AF.Sqrt), r=[st], w=[st])
                        kb.op("dve", lambda e: e.reciprocal(out=st[:, 1:2], in_=st[:, 1:2]), r=[st], w=[st])
                        kb.op("dve", lambda e: e.scalar_tensor_tensor(out=Hc[:], in0=Hc[:], scalar=st[:, 1:2], in1=nwb[:, 0:512], op0=ALU.mult, op1=ALU.mult), r=[Hc, st, nwb], w=[Hc])
                        kb.op("dve", lambda e: e.tensor_tensor(out=Hc[:], in0=Hc[:], in1=nwb[:, 512:1024], op=ALU.add), r=[Hc, nwb], w=[Hc])
                        kb.op("dve", lambda e: e.tensor_tensor(out=Hc[:], in0=Hc[:], in1=ol[:], op=ALU.mult), r=[Hc, ol], w=[Hc])
                        r0 = b * TL + (c - CKC) * 128
                        kb.dma("sp", Y[r0:r0 + 128, :], Hc[:], r=[Hc], store=True)
                kb.op("dve", lambda e: e.tensor_scalar_mul(out=kw[:], in0=k_[:], scalar1=ew[:, 0:1]), r=[k_, ew], w=[kw])
                kb.op("pe", lambda e: e.matmul(pC0[:], lhsT=kw[:, 0:128], rhs=v_[:], start=True, stop=True), r=[kw, v_], w=[pC0])
                kb.op("pe", lambda e: e.matmul(pC1[:], lhsT=kw[:, 128:256], rhs=v_[:], start=True, stop=True), r=[kw, v_], w=[pC1])

                def nmm(e):
                    e.matmul(pNn[:, 0:1], lhsT=kw[:, 0:128], rhs=onesb[:], start=True, stop=True)
                    return e.matmul(pNn[:, 1:2], lhsT=kw[:, 128:256], rhs=onesb[:], start=True, stop=True)
                kb.op("pe", nmm, r=[kw, onesb], w=[pDen])
                kb.op("dve", lambda e: e.scalar_tensor_tensor(out=C[:, 0, :], in0=C[:, 0, :], scalar=ebL[:, 0:1], in1=pC0[:], op0=ALU.mult, op1=ALU.add), r=[C, ebL, pC0], w=[C])
                kb.op("dve", lambda e: e.scalar_tensor_tensor(out=C[:, 1, :], in0=C[:, 1, :], scalar=ebL[:, 0:1], in1=pC1[:], op0=ALU.mult, op1=ALU.add), r=[C, ebL, pC1], w=[C])
                kb.op("act", lambda e: e.activation(out=Cb[:], in_=C[:], func=AF.Copy), r=[C], w=[Cb])
                kb.op("dve", lambda e: e.scalar_tensor_tensor(out=nst[:], in0=nst[:], scalar=ebL[:, 0:1], in1=pNn[:, 0:2], op0=ALU.mult, op1=ALU.add), r=[nst, ebL, pDen], w=[nst])
                kb.op("dve", lambda e: e.tensor_copy(out=nb16[:], in_=nst[:]), r=[nst], w=[nb16])
            kb.barrier()
    return kb.finish()


def p5_core_inputs(hd, hT_full, prm):
    w = prm["w_in"]; D = w.shape[0]
    H = prm["ig_b"].shape[1]
    QW = H * 256; VW = H * 512; QO = QW + VW
    cols = []
    for q in range(2): cols.append(np.arange(hd * 256 + q * 128, hd * 256 + (q + 1) * 128))
    for q in range(2): cols.append(np.arange(QO + hd * 256 + q * 128, QO + hd * 256 + (q + 1) * 128))
    for q in range(4): cols.append(np.arange(QO + QW + hd * 512 + q * 128, QO + QW + hd * 512 + (q + 1) * 128))
    for q in range(4): cols.append(np.arange(QW + hd * 512 + q * 128, QW + hd * 512 + (q + 1) * 128))
    gbase = QO + QW + VW
    cols.append(np.array([gbase + d * 2 * H + io * H + hd for d in range(2) for io in range(2)]))
    win = np.zeros((13, D, 128), np.float32)
    for j, cc in enumerate(cols):
        win[j, :, :len(cc)] = w[:, cc]
    gb = np.array([[prm["ig_b"][0, hd], prm["fg_b"][0, hd], prm["ig_b"][1, hd], prm["fg_b"][1, hd]]], np.float32)
    nw = np.concatenate([prm["norm_w"][hd * 512:(hd + 1) * 512], prm["norm_b"][hd * 512:(hd + 1) * 512]])[None].astype(np.float32)
    return {"hT": hT_full, "win": win, "cst": make_consts(), "gbias": gb, "nw": nw}


_HOOK = None


def _hook(name, arr):
    if _HOOK is not None:
        _HOOK(name, arr)


def run_p1(x, ctx, mods0, prm):
    B, TL, D = x.shape
    TC = ctx.shape[1]
    AW = prm["w0"].shape[-1]; BW = prm["lam"].shape[-1]
    G = 8 // B
    NBLK = AW // 128 // G; NLB = BW // 128 // G
    nc = build_p1(D=D, TC=TC, TL=TL, NBLK=NBLK, NLB=NLB)
    cores = [(b, g) for b in range(B) for g in range(G)]
    maps = [p1_core_inputs(b, g, x, ctx, mods0, prm, NBLK, NLB) for (b, g) in cores]
    res = _run(nc, maps)
    y = np.zeros((B, TC + TL, AW + BW), np.float32)
    for i, (b, g) in enumerate(cores):
        y[b, :, g * NBLK * 128:(g + 1) * NBLK * 128] = res[i]["ya"]
        y[b, :, AW + g * NLB * 128:AW + (g + 1) * NLB * 128] = res[i]["yb"].T
    return y


def _modp(vsets, KC):
    return np.ascontiguousarray(np.stack([np.stack([_pm(v, KC) for v in vs], -1) for vs in vsets], 2).astype(np.float32))


def _chunk_cols(w):
    K_, N = w.shape
    return np.ascontiguousarray(w.reshape(K_, N // 128, 128).transpose(1, 0, 2))


def moe_capacity(G_all):
    cnt = int((G_all > 0).sum(0).max())
    return int(min(3072, max(1024, -(-cnt // 512) * 512)))


def run_moe(ffn_nc, HT_all, G_all, wg, wu, wd, R):
    D, N = HT_all.shape
    ex = np.argsort(-G_all, axis=1, kind="stable")[:, :2]
    ex.sort(axis=1)
    gv = np.take_along_axis(G_all, ex, 1).astype(np.float32)
    items = []
    for e in range(G_all.shape[1]):
        idx = np.nonzero((ex == e).any(1))[0]
        for s0 in range(0, len(idx), R):
            items.append((e, idx[s0:s0 + R]))
    y1T = np.zeros((D, N), np.float32); y2T = np.zeros((D, N), np.float32)
    cache = {}

    def wl(e):
        if e not in cache:
            cache.clear()
            a = np.empty((2 * wg.shape[2] // 128, D, 128), np.float32)
            a[0::2] = _chunk_cols(wg[e]); a[1::2] = _chunk_cols(wu[e])
            cache[e] = (a, _chunk_cols(wd[e]))
        return cache[e]
    for l0 in range(0, len(items), 8):
        batch = items[l0:l0 + 8]
        full = batch + [(batch[0][0], np.zeros((0,), np.int64))] * (8 - len(batch))
        maps = []
        for (e, idx) in full:
            XT = np.zeros((D, R), HT_all.dtype)
            XT[:, :len(idx)] = HT_all[:, idx]
            a, b_ = wl(e)
            maps.append({"XT": XT, "wgu": a, "wd": b_})
        res = _run(ffn_nc, maps)
        for (e, idx), r in zip(batch, res):
            YT = r["YT"][:, :len(idx)]
            m0 = ex[idx, 0] == e
            y1T[:, idx[m0]] = YT[:, m0]
            y2T[:, idx[~m0]] = YT[:, ~m0]
    return y1T, y2T, np.ascontiguousarray(gv.T)


def kernel(x, c, ctx, c_ctx, ada_w, ada_b, norm_g, norm_b,
           ev_w_in, ev_w_out, rwkv_mu, rwkv_w0, rwkv_w2, rwkv_a0, rwkv_a2, rwkv_g2,
           rwkv_k_k, rwkv_k_a, rwkv_r_k, rwkv_lnx_w, rwkv_lnx_b,
           lru_conv_w, lru_conv_b, lru_wa, lru_ba, lru_wx, lru_bx, lru_lam,
           od_w_in, od_w_out, mlstm_ig_b, mlstm_fg_b, mlstm_norm_w, mlstm_norm_b,
           router_w, router_b, moe_w_gate, moe_w_up, moe_w_down):
    f = lambda a: np.asarray(a, dtype=np.float32)
    x = f(x); ctx = f(ctx)
    B, TL, D = x.shape
    TC = ctx.shape[1]
    KC = D // 128
    NQ = 8 // B
    LT = TL // NQ; CT = TC // NQ
    cores = [(b, q) for b in range(B) for q in range(NQ)]
    mods = run_p0(f(c), f(c_ctx), f(ada_w), f(ada_b))
    _hook("mods", mods)
    M = lambda l, v, k: mods[l, v, k * D:(k + 1) * D]
    prm = dict(w_in=f(ev_w_in[0]), mu=f(rwkv_mu[0]), w0=f(rwkv_w0[0]), w2=f(rwkv_w2[0]), a0=f(rwkv_a0[0]), a2=f(rwkv_a2[0]),
               g2=f(rwkv_g2[0]), k_k=f(rwkv_k_k[0]), k_a=f(rwkv_k_a[0]), r_k=f(rwkv_r_k[0]), lnx_w=f(rwkv_lnx_w[0]), lnx_b=f(rwkv_lnx_b[0]),
               conv_w=f(lru_conv_w[0]), conv_b=f(lru_conv_b[0]), wa=f(lru_wa[0]), ba=f(lru_ba[0]), wx=f(lru_wx[0]), bx=f(lru_bx[0]), lam=f(lru_lam[0]))
    y0 = run_p1(x, ctx, mods[0], prm)
    _hook("y0", y0)
    tab = sincos_tab(TL, D)
    rw_l = np.ascontiguousarray(f(router_w).reshape(KC, 128, 16).transpose(1, 0, 2)); rb_l = f(router_b)[None]
    cstc = make_consts()
    zrow = np.zeros((128, KC // 2, LT // 64), np.float32); zcol = np.zeros((128, KC // 2, 64), np.float32)
    ffn_cache = {}

    def get_ffn(R):
        if R not in ffn_cache:
            ffn_cache[R] = build_ffn(D=D, DE=moe_w_gate.shape[3], R=R)
        return ffn_cache[R]

    nc_a = build_post(D=D, KM=y0.shape[2], segs=((LT, 0), (CT, 1)), mode="proj", router=True, hnext=True)
    wout0 = _chunk_cols(f(ev_w_out[0]))
    lnp = lambda l, i: np.ascontiguousarray(np.stack([_pm(f(norm_g[l, i]), KC), _pm(f(norm_b[l, i]), KC)], -1))
    maps = []
    for (b, q) in cores:
        ls = slice(q * LT, (q + 1) * LT); cs_ = slice(q * CT, (q + 1) * CT)
        maps.append({"resT": np.ascontiguousarray(np.concatenate([x[b, ls], ctx[b, cs_]], 0).T),
                     "yT": np.ascontiguousarray(np.concatenate([y0[b, TC + q * LT:TC + (q + 1) * LT], y0[b, cs_]], 0).T),
                     "modp": _modp([(M(0, b, 2), M(0, b, 3), M(0, b, 4)), (M(0, 2, 2), M(0, 2, 3), M(0, 2, 4))], KC),
                     "lnp": lnp(0, 0), "wout": wout0, "rw": rw_l, "rb": rb_l, "cst": cstc,
                     "prow": np.ascontiguousarray(tab[:D // 2, q * (LT // 64):(q + 1) * (LT // 64)].reshape(KC // 2, 128, LT // 64).transpose(1, 0, 2)),
                     "pcol": np.ascontiguousarray(tab[D // 2:, :].reshape(KC // 2, 128, 64).transpose(1, 0, 2))})
    ra = _run(nc_a, maps)
    del maps, y0
    NTK = LT + CT
    resA = [r["res_out"] for r in ra]
    HT_all = np.concatenate([r["h_out"] for r in ra], 1)
    G_all = np.concatenate([r["G"][:NTK] for r in ra], 0)
    _hook("resA0", resA); _hook("G0", G_all)
    R_CAP = moe_capacity(G_all)
    y1T, y2T, g12 = run_moe(get_ffn(R_CAP), HT_all, G_all, f(moe_w_gate[0]), f(moe_w_up[0]), f(moe_w_down[0]), R_CAP)
    _hook("moe0", (y1T, y2T, g12))
    nc_b = build_post(D=D, segs=((LT, 0), (CT, 1)), mode="comb", router=False, hnext=True)
    maps = []
    for i, (b, q) in enumerate(cores):
        sl = slice(i * NTK, (i + 1) * NTK)
        maps.append({"resT": resA[i], "y1T": np.ascontiguousarray(y1T[:, sl]), "y2T": np.ascontiguousarray(y2T[:, sl]), "g12": np.ascontiguousarray(g12[:, sl]),
                     "modp": _modp([(M(0, b, 5), M(1, b, 0), M(1, b, 1)), (M(0, 2, 5), M(1, 2, 0), M(1, 2, 1))], KC),
                     "lnp": lnp(0, 1), "prow": zrow, "pcol": zcol})
    rb = _run(nc_b, maps)
    del maps, y1T, y2T
    resB = [r["res_out"] for r in rb]
    _hook("resB0", resB)
    NT = TC + TL
    hT_full = np.zeros((D, B * NT), rb[0]["h_out"].dtype)
    for i, (b, q) in enumerate(cores):
        h = rb[i]["h_out"]
        hT_full[:, b * NT + TC + q * LT:b * NT + TC + (q + 1) * LT] = h[:, 0:LT]
        hT_full[:, b * NT + q * CT:b * NT + (q + 1) * CT] = h[:, LT:LT + CT]
    prm5 = dict(w_in=f(od_w_in[0]), ig_b=f(mlstm_ig_b[0]), fg_b=f(mlstm_fg_b[0]), norm_w=f(mlstm_norm_w[0]), norm_b=f(mlstm_norm_b[0]))
    nc5 = build_p5(D=D, TC=TC, TL=TL, NB=B)
    r5 = _run(nc5, [p5_core_inputs(hd, hT_full, prm5) for hd in range(8)])
    y1 = np.zeros((B, TL, 8 * 512), np.float32)
    for hd in range(8):
        y1[:, :, hd * 512:(hd + 1) * 512] = r5[hd]["Y"].reshape(B, TL, 512)
    del hT_full, r5
    _hook("y1", y1)
    nc_c = build_post(D=D, KM=y1.shape[2], segs=((LT, 0),), mode="proj", router=True, hnext=True)
    wout1 = _chunk_cols(f(od_w_out[0]))
    maps = []
    for i, (b, q) in enumerate(cores):
        maps.append({"resT": np.ascontiguousarray(resB[i][:, 0:LT]), "yT": np.ascontiguousarray(y1[b, q * LT:(q + 1) * LT].T),
                     "modp": _modp([(M(1, b, 2), M(1, b, 3), M(1, b, 4))], KC), "lnp": lnp(1, 0), "wout": wout1,
                     "rw": rw_l, "rb": rb_l, "cst": cstc, "prow": zrow, "pcol": zcol})
    rc = _run(nc_c, maps)
    del maps, y1
    resC = [r["res_out"] for r in rc]
    HT_all = np.concatenate([r["h_out"] for r in rc], 1)
    G_all = np.concatenate([r["G"][:LT] for r in rc], 0)
    _hook("resC1", resC)
    R_CAP = moe_capacity(G_all)
    y1T, y2T, g12 = run_moe(get_ffn(R_CAP), HT_all, G_all, f(moe_w_gate[1]), f(moe_w_up[1]), f(moe_w_down[1]), R_CAP)
    nc_d = build_post(D=D, segs=((LT, 0),), mode="comb", router=False, hnext=False)
    maps = []
    for i, (b, q) in enumerate(cores):
        sl = slice(i * LT, (i + 1) * LT)
        maps.append({"resT": resC[i], "y1T": np.ascontiguousarray(y1T[:, sl]), "y2T": np.ascontiguousarray(y2T[:, sl]), "g12": np.ascontiguousarray(g12[:, sl]),
                     "modp": _modp([(M(1, b, 5), M(1, b, 5), M(1, b, 5))], KC), "lnp": lnp(1, 1), "prow": zrow, "pcol": zcol})
    rd = _run(nc_d, maps)
    out = np.zeros((B, TL, D), np.float32)
    for i, (b, q) in enumerate(cores):
        out[b, q * LT:(q + 1) * LT] = rd[i]["res_out"].T
    return out
```

```python
import contextlib
import numpy as np
import concourse.bass as bass
import concourse.mybir as mybir
from concourse.bass_utils import run_bass_kernel_spmd

F32 = mybir.dt.float32
BF16 = mybir.dt.bfloat16
AF = mybir.ActivationFunctionType
ALU = mybir.AluOpType
AX = mybir.AxisListType


class Tl:
    __slots__ = ("t", "key")

    def __init__(self, t, key):
        self.t = t
        self.key = key

    def __getitem__(self, idx):
        return self.t[idx]


class KB:
    ENG = ("pe", "act", "dve", "pool", "sp")

    def __init__(self):
        self.nc = bass.Bass("TRN2", target_bir_lowering=False)
        self.es = contextlib.ExitStack()
        self.es.enter_context(self.nc.allow_low_precision("bf16 matmul operands, fp32 accumulation"))
        nc = self.nc
        self.eng = {"pe": nc.tensor, "act": nc.scalar, "dve": nc.vector, "pool": nc.gpsimd, "sp": nc.sync}
        self.sem = {}
        self.cnt = {}
        for e in self.ENG:
            self.sem[e] = self.es.enter_context(nc.semaphore("s_" + e))
            self.cnt[e] = 0
        self.seen = {e: {} for e in self.ENG}
        self.dep = {}
        self.dsem = {}
        self.semobj = {}
        self.nuniq = 0
        self.stores = []
        self.free_dsem = []
        self.es_perm = self.es

    def sb(self, name, shape, dtype=F32):
        self.nalloc = getattr(self, "nalloc", 0) + 1
        name = "sb%d_%s" % (self.nalloc, name)
        t = self.es.enter_context(self.nc.sbuf_tensor(name, list(shape), dtype))
        return Tl(t, name)

    def ps(self, name, shape, dtype=F32):
        self.nalloc = getattr(self, "nalloc", 0) + 1
        name = "ps%d_%s" % (self.nalloc, name)
        t = self.es.enter_context(self.nc.psum_tensor(name, list(shape), dtype))
        return Tl(t, name)

    def dram(self, name, shape, dtype=F32, kind="Internal"):
        t = self.nc.dram_tensor(name, list(shape), dtype, kind=kind)
        return Tl(t.ap(), name)

    def _needs(self, e, r, w):
        waits = {}

        def need(tok, same_ok):
            if tok is None:
                return
            sname, val = tok
            if same_ok and sname == "s_" + e:
                return
            if self.seen[e].get(sname, 0) >= val:
                return
            if waits.get(sname, 0) < val:
                waits[sname] = val

        for t in r:
            d = self.dep.get(t.key)
            if d:
                need(d["w"], False)
        for t in w:
            d = self.dep.get(t.key)
            if d:
                need(d["w"], False)
                for sname, val in d["r"].items():
                    need((sname, val), False)
        for sname, val in waits.items():
            self.eng[e].wait_ge(self.semobj[sname], val)
            self.seen[e][sname] = val

    def _commit(self, tok, r, w):
        for t in r:
            d = self.dep.setdefault(t.key, {"w": None, "r": {}})
            if d["r"].get(tok[0], 0) < tok[1]:
                d["r"][tok[0]] = tok[1]
        for t in w:
            self.dep[t.key] = {"w": tok, "r": {}}

    def op(self, e, fn, r=(), w=()):
        self.semobj.setdefault("s_" + e, self.sem[e])
        self._needs(e, r, w)
        ins = fn(self.eng[e])
        self.cnt[e] += 1
        ins.then_inc(self.sem[e], 1)
        self._commit(("s_" + e, self.cnt[e]), r, w)

    def dma(self, q, out, in_, r=(), w=(), semkey=None, store=False):
        if semkey is None:
            semkey = (r[0].key if store else w[0].key)
        if semkey not in self.dsem:
            if self.free_dsem:
                self.dsem[semkey] = self.free_dsem.pop()
            else:
                nm = "d%d" % self.nuniq
                self.nuniq += 1
                s = self.es_perm.enter_context(self.nc.semaphore(nm))
                self.dsem[semkey] = [nm, 0]
                self.semobj[nm] = s
        ent = self.dsem[semkey]
        self._needs(q, r, w)
        pairs = out if isinstance(out, list) else [(out, in_)]
        for o, i in pairs:
            self.eng[q].dma_start(out=o, in_=i).then_inc(self.semobj[ent[0]], 16)
            ent[1] += 16
        tok = (ent[0], ent[1])
        self._commit(tok, r, w)
        if store:
            self.stores.append(tok)

    def dump(self, tl, name):
        shp = list(tl.t.shape)
        d = self.dram(name, shp, tl.t.dtype, kind="ExternalOutput")
        self.dma("sp", d[:], tl[:], r=[tl], store=True)

    def barrier(self):
        for e in self.ENG:
            for e2 in self.ENG:
                if e2 != e and self.cnt[e2] > self.seen[e].get("s_" + e2, 0):
                    self.semobj.setdefault("s_" + e2, self.sem[e2])
                    self.eng[e].wait_ge(self.sem[e2], self.cnt[e2])
                    self.seen[e]["s_" + e2] = self.cnt[e2]
            for key, (nm, val) in self.dsem.items():
                if val > self.seen[e].get(nm, 0):
                    self.eng[e].wait_ge(self.semobj[nm], val)
                    self.seen[e][nm] = val
        self.dep = {}

    @contextlib.contextmanager
    def scope(self):
        saved = self.es
        self.es = contextlib.ExitStack()
        keys_before = set(self.dsem.keys())
        try:
            yield
        finally:
            self.barrier()
            for k in list(self.dsem.keys()):
                if k not in keys_before:
                    self.free_dsem.append(self.dsem.pop(k))
            self.es.close()
            self.es = saved

    def finish(self):
        last = {}
        for sname, val in self.stores:
            last[sname] = max(last.get(sname, 0), val)
        for sname, val in last.items():
            self.eng["sp"].wait_ge(self.semobj[sname], val)
        self.es.close()
        return self.nc


_TRACE = False
_TIMES = []


def _run(nc, in_maps):
    if _TRACE:
        res = run_bass_kernel_spmd(nc, in_maps, core_ids=list(range(len(in_maps))), trace=True)
        _TIMES.append(res.exec_time_ns)
    else:
        res = run_bass_kernel_spmd(nc, in_maps, core_ids=list(range(len(in_maps))))
    return res.results


def build_p0(D=4096, NCOL=3072, NV=3, CW=512):
    kb = KB()
    nc = kb.nc
    KC = D // 128
    cvT = kb.dram("cvT", [128, KC, NV], kind="ExternalInput")
    W = kb.dram("w", [D, NCOL], kind="ExternalInput")
    bias = kb.dram("b", [1, NCOL], kind="ExternalInput")
    out = kb.dram("out", [NV, NCOL], kind="ExternalOutput")
    cv = kb.sb("cv", [128, KC, NV])
    cs = kb.sb("cs", [128, KC, NV])
    bt = kb.sb("bt", [NV, NCOL])
    ot = kb.sb("ot", [NV, NCOL])
    NB = 3
    wt = [kb.sb("wt%d" % i, [128, 8, CW]) for i in range(NB)]
    pss = [kb.ps("ps%d" % i, [128, CW]) for i in range(2)]
    kb.dma("sp", cv[:], cvT[:], w=[cv])
    kb.dma("sp", [(bt[v:v + 1, :], bias[:]) for v in range(NV)], None, w=[bt])
    kb.op("act", lambda e: e.activation(out=cs[:], in_=cv[:], func=AF.Silu), r=[cv], w=[cs])
    Wv = W[:].rearrange("(c p) n -> p c n", p=128)
    nblk = NCOL // CW
    li = 0
    for j in range(nblk):
        p = pss[j % 2]
        for c8 in range(KC // 8):
            wb = wt[li % NB]
            li += 1
            kb.dma("sp" if li % 2 else "pool", wb[:], Wv[:, c8 * 8:(c8 + 1) * 8, j * CW:(j + 1) * CW], w=[wb])

            def mm(e, c8=c8, wb=wb, p=p):
                ins = None
                for cc in range(8):
                    c = c8 * 8 + cc
                    ins = e.matmul(p[0:NV, :], lhsT=cs[:, c, :], rhs=wb[:, cc, :], start=(c == 0), stop=(c == KC - 1))
                return ins
            kb.op("pe", mm, r=[cs, wb], w=[p])
        kb.op("dve", lambda e, p=p, j=j: e.tensor_tensor(out=ot[:, j * CW:(j + 1) * CW], in0=p[0:NV, :],
                                                        in1=bt[:, j * CW:(j + 1) * CW], op=ALU.add),
              r=[p, bt], w=[ot])
    kb.dma("sp", out[:], ot[:], r=[ot], store=True)
    return kb.finish()


def run_p0(c, c_ctx, ada_w, ada_b):
    D = c.shape[1]
    L = ada_w.shape[0]
    ncol = ada_w.shape[2]
    cvs = np.concatenate([c, c_ctx[None, :]], 0).astype(np.float32)
    cvT = np.ascontiguousarray(cvs.T.reshape(D // 128, 128, 3).transpose(1, 0, 2))
    per = ncol * L // 8
    nc = build_p0(D=D, NCOL=per)
    maps = []
    for j in range(8):
        l = (j * per) // ncol
        c0 = (j * per) % ncol
        maps.append({"cvT": cvT, "w": np.ascontiguousarray(ada_w[l][:, c0:c0 + per]),
                     "b": np.ascontiguousarray(ada_b[l][None, c0:c0 + per])})
    res = _run(nc, maps)
    flat = np.concatenate([r["out"] for r in res], axis=1)
    return flat.reshape(3, L, ncol).transpose(1, 0, 2)


def seq_tiles(TC, TL, W):
    out = []
    for t0 in range(0, TC, W):
        out.append((t0, min(W, TC - t0), False))
    for t0 in range(0, TL, W):
        out.append((TC + t0, min(W, TL - t0), True))
    return out


def make_consts():
    c = np.zeros((128, 20, 128), np.float32)
    i = np.arange(128)
    c[:, 0, :] = np.eye(128)
    c[:, 1, :] = (i[:, None] > i[None, :])
    c[:, 2, :] = (i[:, None] < i[None, :])
    c[:, 3, :] = (i[:, None] >= i[None, :])
    c[:, 4, :] = (i[:, None] <= i[None, :])
    c[:, 5, :] = ((i[:, None] // 64) == (i[None, :] // 64))
    for l in range(7):
        t, s_ = i[:, None], i[None, :]
        m = ((t >> (l + 1)) == (s_ >> (l + 1))) & (((t >> l) & 1) == 1) & (((s_ >> l) & 1) == 0)
        c[:, 6 + l, :] = m
        c[:, 13 + l, :] = m.T
    return c


def make_consts2():
    c = make_consts()
    o = np.zeros((128, 15, 2, 128), np.float32)
    for l in range(7):
        o[:, l, :, :] = c[:, 6 + l, None, :]
        o[:, 7 + l, :, :] = c[:, 13 + l, None, :]
    o[:, 14, :, :] = c[:, 0, None, :]
    return o


def gemm_fm(kb, Wd, NCH, K, XT, NT, tiles, evac, GS=4, xdt=BF16, dbuf=False):
    KC = K // 128
    TW = max(w for _, w, _ in tiles)
    NS = 2 if dbuf else 1
    with kb.scope():
        wst = [kb.sb("g_wst%d" % i, [128, KC, 128]) for i in range(2)]
        wb = [[kb.sb("g_wb%d_%d" % (s_, i), [128, KC, 128], BF16) for i in range(GS)] for s_ in range(NS)]
        xt = [kb.sb("g_xt%d" % i, [128, KC, TW], xdt) for i in range(2)]
        pss = [kb.ps("g_ps%d" % i, [128, 512]) for i in range(GS)]
        XTv = XT[:].rearrange("(c p) t -> p c t", p=128)
        groups = [list(range(g0, min(NCH, g0 + GS))) for g0 in range(0, NCH, GS)]
        li = [0]

        def load_group(gi):
            for jj, j in enumerate(groups[gi]):
                st = wst[li[0] % 2]
                li[0] += 1
                kb.dma("sp", st[:], Wd[j].rearrange("(c p) n -> p c n", p=128), w=[st])
                dst = wb[gi % NS][jj]
                kb.op("pool", lambda e: e.tensor_copy(out=dst[:], in_=st[:]), r=[st], w=[dst])
        xi = 0
        load_group(0)
        for gi, js in enumerate(groups):
            if dbuf and gi + 1 < len(groups):
                load_group(gi + 1)
            wset = wb[gi % NS]
            for (t0, W, _) in tiles:
                x = xt[xi % 2]
                xi += 1
                kb.dma("act", x[:, :, 0:W], XTv[:, :, t0:t0 + W], w=[x])
                for jj, j in enumerate(js):
                    p = pss[jj]

                    def mm(e):
                        ins = None
                        for c in range(KC):
                            ins = e.matmul(p[:, 0:W], lhsT=wset[jj][:, c, :], rhs=x[:, c, 0:W], start=(c == 0), stop=(c == KC - 1))
                        return ins
                    kb.op("pe", mm, r=[wset[jj], x], w=[p])
                    evac(j, t0, W, p)
            if not dbuf and gi + 1 < len(groups):
                load_group(gi + 1)


NVR = 7
NVL = 11


def build_p1(D=4096, TC=256, TL=4096, NBLK=4, NLB=4, dbg=False, upto=None):
    kb = KB()
    KC = D // 128
    NT = TC + TL
    NCK = NT // 128
    RW = NBLK * 128
    NCHR = 3 * NBLK + 6
    NCH = NCHR + 2 * NLB
    CK_C = TC // 128
    I = lambda name, shape, dt=F32: kb.dram(name, shape, dt, kind="ExternalInput")
    xT = I("xT", [D, TL]); cT = I("cT", [D, TC]); ptab_d = I("ptab", [128, KC, 64]); mods_d = I("mods", [128, KC, 4])
    win = I("win", [NCH, D, 128]); mu_d = I("mu", [128, NCHR])
    w2_d = I("w2", [2, 128, RW]); a2_d = I("a2", [2, 128, RW]); g2_d = I("g2", [2, 128, RW])
    rvec_d = I("rvec", [128, NBLK, NVR]); lnx_d = I("lnx", [1, 2 * RW])
    lvec_d = I("lvec", [128, NLB, NVL]); lwa_d = I("lwa", [2, NLB, 128, 128]); lwx_d = I("lwx", [2, NLB, 128, 128])
    cst_d = I("cst", [128, 20, 128]); cst2_d = I("cst2", [128, 15, 2, 128])
    ya = kb.dram("ya", [NT, RW], kind="ExternalOutput")
    yb = kb.dram("yb", [NLB * 128, NT], kind="ExternalOutput")
    hT = kb.dram("hT", [D, NT], BF16)
    P = kb.dram("P", [NCH, 128, NT])
    OPS = kb.dram("OPS", [2, 4, 128, NT])

    with kb.scope():
        ptab = kb.sb("ptab", [128, KC, 64]); mods = kb.sb("mods", [128, KC, 4])
        kb.dma("sp", ptab[:], ptab_d[:], w=[ptab]); kb.dma("sp", mods[:], mods_d[:], w=[mods])
        kb.op("dve", lambda e: e.tensor_scalar_add(out=mods[:, :, 1:2], in0=mods[:, :, 1:2], scalar1=1.0), r=[mods], w=[mods])
        kb.op("dve", lambda e: e.tensor_scalar_add(out=mods[:, :, 3:4], in0=mods[:, :, 3:4], scalar1=1.0), r=[mods], w=[mods])
        GA = min(8, KC // 2)
        xs = [kb.sb("xs%d" % i, [128, GA, 512]) for i in range(2)]
        hb = [kb.sb("hb%d" % i, [128, GA, 512], BF16) for i in range(2)]
        hTv = hT[:].rearrange("(c p) t -> p c t", p=128)
        it = 0
        for (t0, W, lat) in seq_tiles(TC, TL, 512):
            src, s0 = (xT, t0 - TC) if lat else (cT, t0)
            srcv = src[:].rearrange("(c p) t -> p c t", p=128)
            for g in range(KC // GA):
                x = xs[it % 2]; h = hb[it % 2]; it += 1
                c0 = g * GA
                kb.dma("sp", x[:, :, 0:W], srcv[:, c0:c0 + GA, s0:s0 + W], w=[x])
                if lat:
                    nr = W // 64; r0 = s0 // 64
                    xv = x[:, :, 0:W].rearrange("p c (r q) -> p c r q", q=64)
                    if c0 < KC // 2:
                        pin = ptab[:, c0:c0 + GA, r0:r0 + nr].unsqueeze(3).to_broadcast([128, GA, nr, 64])
                    else:
                        pin = ptab[:, c0:c0 + GA, 0:64].unsqueeze(2).to_broadcast([128, GA, nr, 64])
                    kb.op("pool", lambda e, xv=xv, pin=pin: e.tensor_tensor(out=xv, in0=xv, in1=pin, op=ALU.add), r=[x, ptab], w=[x])
                mo = 0 if lat else 2
                for cc in range(GA):
                    c = c0 + cc
                    kb.op("dve" if cc % 2 else "act",
                          (lambda e, cc=cc, c=c, x=x, h=h, W=W, mo=mo: e.tensor_scalar(out=h[:, cc, 0:W], in0=x[:, cc, 0:W], scalar1=mods[:, c, mo + 1:mo + 2], scalar2=mods[:, c, mo:mo + 1], op0=ALU.mult, op1=ALU.add))
                          if cc % 2 else
                          (lambda e, cc=cc, c=c, x=x, h=h, W=W, mo=mo: e.activation(out=h[:, cc, 0:W], in_=x[:, cc, 0:W], func=AF.Identity, scale=mods[:, c, mo + 1:mo + 2], bias=mods[:, c, mo:mo + 1])),
                          r=[x, mods], w=[h])
                kb.dma("sp", hTv[:, c0:c0 + GA, t0:t0 + W], h[:, :, 0:W], r=[h], store=True)

    if upto == "A":
        return kb.finish()
    oi = [0]

    def evacB(j, t0, W, p):
        o = obs[oi[0] % 4]; oi[0] += 1
        if oi[0] % 2:
            kb.op("act", lambda e: e.activation(out=o[:, 0:W], in_=p[:, 0:W], func=AF.Copy), r=[p], w=[o])
        else:
            kb.op("dve", lambda e: e.tensor_copy(out=o[:, 0:W], in_=p[:, 0:W]), r=[p], w=[o])
        kb.dma("sp", P[j][:, t0:t0 + W], o[:, 0:W], r=[o], store=True)
    with kb.scope():
        obs = [kb.sb("ob%d" % i, [128, 512]) for i in range(4)]
        gemm_fm(kb, win, NCH, D, hT, NT, seq_tiles(TC, TL, 512), evacB, GS=7)

    if upto == "B":
        return kb.finish()
    cst = kb.sb("cst", [128, 20, 128]); kb.dma("sp", cst[:], cst_d[:], w=[cst])
    IDN, ML_S, MU_S, ML_I, MU_I, BONE = (cst[:, i, :] for i in range(6))
    mu = kb.sb("mu", [128, NCHR]); kb.dma("sp", mu[:], mu_d[:], w=[mu])
    omm = kb.sb("omm", [128, NCHR]); hmu = kb.sb("hmu", [128, NCHR])
    kb.op("dve", lambda e: e.tensor_scalar(out=omm[:], in0=mu[:], scalar1=-1.0, scalar2=1.0, op0=ALU.mult, op1=ALU.add), r=[mu], w=[omm])
    kb.op("dve", lambda e: e.tensor_scalar_mul(out=hmu[:], in0=mu[:], scalar1=0.5), r=[mu], w=[hmu])
    TW = 512
    ctiles = seq_tiles(TC, TL, TW)

    def load_shift(j, t0, W, lat, dst, raw, eng="dve"):
        s_lo, s_hi = (TC, NT) if lat else (0, TC)
        lo = t0 - 1 if t0 > s_lo else t0
        hi = t0 + W + 1 if t0 + W < s_hi else t0 + W
        if lo == t0:
            kb.op("pool", lambda e: e.memset(raw[:, 0:1], 0.0), w=[raw])
        if hi == t0 + W:
            kb.op("pool", lambda e: e.memset(raw[:, W + 1:W + 2], 0.0), w=[raw])
        kb.dma("sp", raw[:, 1 - (t0 - lo):1 - (t0 - lo) + (hi - lo)], P[j][:, lo:hi], w=[raw])
        kb.op(eng, lambda e: e.tensor_tensor(out=dst[:, 0:W], in0=raw[:, 0:W], in1=raw[:, 2:W + 2], op=ALU.add), r=[raw], w=[dst])
        kb.op(eng, lambda e: e.tensor_scalar_mul(out=dst[:, 0:W], in0=dst[:, 0:W], scalar1=hmu[:, j:j + 1]), r=[dst, hmu], w=[dst])
        kb.op(eng, lambda e: e.scalar_tensor_tensor(out=dst[:, 0:W], in0=raw[:, 1:W + 1], scalar=omm[:, j:j + 1], in1=dst[:, 0:W], op0=ALU.mult, op1=ALU.add), r=[raw, dst, omm], w=[dst])

    LR = kb.dram("LR", [6, 128, NT])
    with kb.scope():
        raw = [kb.sb("c0raw%d" % i, [128, TW + 2]) for i in range(2)]
        dst = [kb.sb("c0dst%d" % i, [128, TW]) for i in range(2)]
        n = 0
        for q in range(6):
            j = 3 * NBLK + q
            fn = AF.Tanh if q < 2 else (AF.Identity if q < 4 else AF.Sigmoid)
            for (t0, W, lat) in ctiles:
                rw_, ds_ = raw[n % 2], dst[n % 2]; n += 1
                load_shift(j, t0, W, lat, ds_, rw_)
                kb.op("act", lambda e, ds_=ds_, W=W, fn=fn: e.activation(out=ds_[:, 0:W], in_=ds_[:, 0:W], func=fn), r=[ds_], w=[ds_])
                kb.dma("sp", LR[q][:, t0:t0 + W], ds_[:, 0:W], r=[ds_], store=True)

    if upto == "C0":
        return kb.finish()
    with kb.scope():
        rvec = kb.sb("rvec", [128, NBLK, NVR]); kb.dma("sp", rvec[:], rvec_d[:], w=[rvec])
        omka = kb.sb("omka", [128, NBLK])
        kb.op("dve", lambda e: e.tensor_scalar(out=omka[:], in0=rvec[:, :, 5], scalar1=-1.0, scalar2=1.0, op0=ALU.mult, op1=ALU.add), r=[rvec], w=[omka])
        lnw = kb.sb("lnw", [128, RW]); lnb = kb.sb("lnb", [128, RW])
        kb.dma("sp", lnw[:], lnx_d[:, 0:RW].partition_broadcast(128), w=[lnw])
        kb.dma("sp", lnb[:], lnx_d[:, RW:2 * RW].partition_broadcast(128), w=[lnb])
        w2 = kb.sb("w2", [128, 2, RW]); a2 = kb.sb("a2", [128, 2, RW]); g2 = kb.sb("g2", [128, 2, RW])
        kb.dma("sp", w2[:], w2_d[:].rearrange("d k n -> k d n"), w=[w2])
        kb.dma("sp", a2[:], a2_d[:].rearrange("d k n -> k d n"), w=[a2])
        kb.dma("sp", g2[:], g2_d[:].rearrange("d k n -> k d n"), w=[g2])
        hsel = kb.sb("hsel", [128, 2])
        kb.op("dve", lambda e: e.tensor_copy(out=hsel[:, 0:1], in_=cst[:, 5, 0:1]), r=[cst], w=[hsel])
        kb.op("dve", lambda e: e.tensor_copy(out=hsel[:, 1:2], in_=cst[:, 5, 64:65]), r=[cst], w=[hsel])
        raw = kb.sb("raw", [128, TW + 2])
        Rs = kb.sb("Rs", [128, TW]); Ks = kb.sb("Ks", [128, TW]); Vs = kb.sb("Vs", [128, TW]); KKn = kb.sb("KKn", [128, TW])
        RK = kb.sb("RK", [128, TW]); LD = kb.sb("LD", [128, TW]); CL = kb.sb("CL", [128, TW]); ICL = kb.sb("ICL", [128, TW])
        KD = kb.sb("KD", [128, TW]); T0 = kb.sb("T0", [128, TW]); T1 = kb.sb("T1", [128, TW])
        O4 = [kb.sb("O4_%d" % i, [128, TW]) for i in range(4)]
        ones = kb.sb("ones", [128, 128]); kb.op("pool", lambda e: e.memset(ones[:], 1.0), w=[ones])
        lt = [kb.sb("lt%d" % i, [128, 512]) for i in range(2)]
        Vtm = kb.sb("Vtm", [128, NCK, 128]); Yd = [kb.sb("Yd%d" % d, [128, NCK, 128]) for d in range(2)]
        sbon = kb.sb("sbon", [128, NCK, 2]); EL = [kb.sb("EL%d" % d, [128, NCK]) for d in range(2)]
        ST = [[kb.sb("ST%d_%d" % (d, hh), [128, 64]) for hh in range(2)] for d in range(2)]
        PADS = [[[[kb.sb("pad%d_%d_%d_%d" % (d, q, hh, par), [128, 128]) for par in range(2)] for hh in range(2)] for q in range(3)] for d in range(2)]
        for d in range(2):
            for q in range(3):
                for hh in range(2):
                    for par in range(2):
                        kb.op("pool", lambda e: e.memset(PADS[d][q][hh][par][:], 0.0), w=[PADS[d][q][hh][par]])
        cst2 = kb.sb("cst2", [128, 15, 2, 128]); kb.dma("sp", cst2[:], cst2_d[:], w=[cst2])
        banks = [kb.ps("bank%d" % i, [128, 512]) for i in range(8)]
        pW = banks[0]
        pV = banks[1]
        OPB = [[[kb.sb("opb%d_%d_%d" % (d, b, q), [128, 128]) for q in range(4)] for b in range(2)] for d in range(2)]
        BKb = [[kb.sb("bkb%d_%d" % (d, q), [128, 128]) for q in range(2)] for d in range(2)]
        BKt = [[kb.sb("bkt%d_%d" % (d, q), [128, 128]) for q in range(2)] for d in range(2)]
        M3 = lambda nm: [kb.sb("%s%d" % (nm, d), [128, 2, 128]) for d in range(2)]
        Mf = M3("Mf"); MTf = M3("MTf"); Mm = M3("Mm"); MTm = M3("MTm"); T1s = M3("T1s"); T2s = M3("T2s")
        Xb = [[kb.sb("X%d_%d" % (d, b), [128, 2, 128]) for b in range(2)] for d in range(2)]
        XTb = [[kb.sb("XT%d_%d" % (d, b), [128, 2, 128]) for b in range(2)] for d in range(2)]
        Mak = [kb.sb("Mak%d" % d, [128, 2, 128]) for d in range(2)]
        Nrb = [kb.sb("Nrb%d" % d, [128, 2, 128]) for d in range(2)]
        Nrk = [kb.sb("Nrk%d" % d, [128, 2, 128]) for d in range(2)]
        Ub = [[kb.sb("U%d_%d" % (d, b), [128, 128]) for b in range(2)] for d in range(2)]
        bk = lambda i, a, b: Tl(banks[i].t[:, a:b], banks[i].key)
        pP = [bk(2 + 3 * d, 0, 256) for d in range(2)]
        pM = [bk(2 + 3 * d, 256, 512) for d in range(2)]
        pPT = [bk(3 + 3 * d, 0, 256) for d in range(2)]
        pT2 = [bk(3 + 3 * d, 256, 512) for d in range(2)]
        pU = [bk(4 + 3 * d, 0, 128) for d in range(2)]
        pY = [bk(4 + 3 * d, 128, 256) for d in range(2)]
        pS = [bk(4 + 3 * d, 256, 384) for d in range(2)]
        st1 = kb.sb("st1", [128, NCK * 2]); st2 = kb.sb("st2", [128, NCK * 2])
        gl = [kb.sb("gl%d" % i, [128, 2, 512]) for i in range(2)]
        order = [list(range(NCK)), list(range(CK_C - 1, -1, -1)) + list(range(NCK - 1, CK_C - 1, -1))]

        for bi in range(NBLK):
            jr, jk, jv = bi, NBLK + bi, 2 * NBLK + bi
            cs = slice(bi * 128, (bi + 1) * 128)
            V_ = lambda i: rvec[:, bi, i:i + 1]
            for (t0, W, lat) in ctiles:
                ck0 = t0 // 128; nck = W // 128
                load_shift(jr, t0, W, lat, Rs, raw)
                load_shift(jk, t0, W, lat, Ks, raw)
                load_shift(jv, t0, W, lat, Vs, raw)
                for cc in range(nck):
                    kb.op("pe", lambda e, cc=cc: e.transpose(pV[:, 0:128], Vs[:, cc * 128:(cc + 1) * 128], IDN), r=[Vs, cst], w=[pV])
                    kb.op("act", lambda e, cc=cc: e.activation(out=Vtm[:, ck0 + cc, :], in_=pV[:, 0:128], func=AF.Copy), r=[pV], w=[Vtm])
                kb.op("dve", lambda e: e.tensor_scalar_mul(out=KKn[:, 0:W], in0=Ks[:, 0:W], scalar1=V_(4)), r=[Ks, rvec], w=[KKn])
                kb.op("act", lambda e: e.activation(out=T0[:, 0:W], in_=KKn[:, 0:W], func=AF.Square), r=[KKn], w=[T0])
                for s0 in range(0, W, 512):
                    sw = min(512, W - s0)
                    kb.op("pe", lambda e, s0=s0, sw=sw: e.matmul(pW[:, 0:sw], lhsT=BONE, rhs=T0[:, s0:s0 + sw], start=True, stop=True), r=[T0, cst], w=[pW])
                    kb.op("dve", lambda e, s0=s0, sw=sw: e.tensor_scalar_max(out=T1[:, s0:s0 + sw], in0=pW[:, 0:sw], scalar1=1e-24), r=[pW], w=[T1])
                kb.op("act", lambda e: e.activation(out=T1[:, 0:W], in_=T1[:, 0:W], func=AF.Sqrt), r=[T1], w=[T1])
                kb.op("dve", lambda e: e.reciprocal(out=T1[:, 0:W], in_=T1[:, 0:W]), r=[T1], w=[T1])
                kb.op("dve", lambda e: e.tensor_tensor(out=KKn[:, 0:W], in0=KKn[:, 0:W], in1=T1[:, 0:W], op=ALU.mult), r=[KKn, T1], w=[KKn])
                for d in range(2):
                    for s0 in range(0, W, 512):
                        sw = min(512, W - s0)
                        for (q, wts, dstT, biasv) in ((d, w2, LD, V_(d)), (2 + d, a2, ICL, V_(2 + d))):
                            l = lt[(s0 // 512 + q) % 2]
                            kb.dma("sp", l[:, 0:sw], LR[q][:, t0 + s0:t0 + s0 + sw], w=[l])
                            kb.op("pe", lambda e, l=l, wts=wts, sw=sw: e.matmul(pW[:, 0:sw], lhsT=wts[:, d, cs], rhs=l[:, 0:sw], start=True, stop=True), r=[l, wts], w=[pW])
                            kb.op("act", lambda e, dstT=dstT, biasv=biasv, s0=s0, sw=sw: e.activation(out=dstT[:, s0:s0 + sw], in_=pW[:, 0:sw], func=AF.Sigmoid, bias=biasv, scale=1.0), r=[pW, rvec], w=[dstT])
                    kb.op("dve", lambda e: e.tensor_scalar_mul(out=LD[:, 0:W], in0=LD[:, 0:W], scalar1=-0.6065306597126334), r=[LD], w=[LD])
                    kb.op("dve", lambda e: e.tensor_scalar(out=KD[:, 0:W], in0=ICL[:, 0:W], scalar1=V_(5), scalar2=omka[:, bi:bi + 1], op0=ALU.mult, op1=ALU.add), r=[ICL, rvec, omka], w=[KD])
                    kb.op("dve", lambda e: e.tensor_tensor(out=KD[:, 0:W], in0=KD[:, 0:W], in1=Ks[:, 0:W], op=ALU.mult), r=[KD, Ks], w=[KD])
                    if d == 0:
                        kb.op("pool", lambda e: e.tensor_tensor(out=RK[:, 0:W], in0=Rs[:, 0:W], in1=KD[:, 0:W], op=ALU.mult), r=[Rs, KD], w=[RK])
                    else:
                        kb.op("pool", lambda e: e.tensor_tensor(out=T0[:, 0:W], in0=Rs[:, 0:W], in1=KD[:, 0:W], op=ALU.mult), r=[Rs, KD], w=[T0])
                        kb.op("pool", lambda e: e.tensor_tensor(out=RK[:, 0:W], in0=RK[:, 0:W], in1=T0[:, 0:W], op=ALU.add), r=[RK, T0], w=[RK])
                    for cc in range(nck):
                        sl = slice(cc * 128, (cc + 1) * 128)
                        if d == 0:
                            kb.op("dve", lambda e, sl=sl: e.tensor_tensor_scan(out=CL[:, sl], data0=ones[:], data1=LD[:, sl], initial=0.0, op0=ALU.mult, op1=ALU.add), r=[LD, ones], w=[CL])
                        else:
                            kb.op("dve", lambda e, sl=sl: e.tensor_tensor_scan(out=CL[:, sl][:, ::-1], data0=ones[:], data1=LD[:, sl][:, ::-1], initial=0.0, op0=ALU.mult, op1=ALU.add), r=[LD, ones], w=[CL])
                    CLv = CL[:, 0:W].rearrange("p (c l) -> p c l", l=128)
                    last = CLv[:, :, 127] if d == 0 else CLv[:, :, 0]
                    kb.op("act", lambda e, last=last: e.activation(out=EL[d][:, ck0:ck0 + nck], in_=last, func=AF.Exp), r=[CL], w=[EL[d]])
                    kb.op("act", lambda e: e.activation(out=T0[:, 0:W], in_=CL[:, 0:W], func=AF.Exp), r=[CL], w=[T0])
                    kb.op("dve", lambda e: e.tensor_tensor(out=O4[1][:, 0:W], in0=Rs[:, 0:W], in1=T0[:, 0:W], op=ALU.mult), r=[Rs, T0], w=[O4[1]])
                    kb.op("dve", lambda e: e.tensor_tensor(out=T1[:, 0:W], in0=CL[:, 0:W], in1=LD[:, 0:W], op=ALU.subtract), r=[CL, LD], w=[T1])
                    kb.op("act", lambda e: e.activation(out=T1[:, 0:W], in_=T1[:, 0:W], func=AF.Exp), r=[T1], w=[T1])
                    kb.op("dve", lambda e: e.scalar_tensor_tensor(out=O4[0][:, 0:W], in0=KKn[:, 0:W], scalar=-1.0, in1=T1[:, 0:W], op0=ALU.mult, op1=ALU.mult), r=[KKn, T1], w=[O4[0]])
                    kb.op("act", lambda e: e.activation(out=T0[:, 0:W], in_=CL[:, 0:W], func=AF.Exp, scale=-1.0), r=[CL], w=[T0])
                    kb.op("pool", lambda e: e.tensor_tensor(out=T1[:, 0:W], in0=KKn[:, 0:W], in1=ICL[:, 0:W], op=ALU.mult), r=[KKn, ICL], w=[T1])
                    kb.op("dve", lambda e: e.tensor_tensor(out=O4[2][:, 0:W], in0=T1[:, 0:W], in1=T0[:, 0:W], op=ALU.mult), r=[T1, T0], w=[O4[2]])
                    kb.op("dve", lambda e: e.tensor_tensor(out=O4[3][:, 0:W], in0=KD[:, 0:W], in1=T0[:, 0:W], op=ALU.mult), r=[KD, T0], w=[O4[3]])
                    for q in range(4):
                        kb.dma("sp", OPS[d, q][:, t0:t0 + W], O4[q][:, 0:W], r=[O4[q]], store=True)
                kb.op("dve", lambda e: e.tensor_scalar_mul(out=RK[:, 0:W], in0=RK[:, 0:W], scalar1=V_(6)), r=[RK, rvec], w=[RK])
                for cc in range(nck):
                    kb.op("pe", lambda e, cc=cc: e.matmul(pV[:, 256 + 2 * cc:258 + 2 * cc], lhsT=RK[:, cc * 128:(cc + 1) * 128], rhs=hsel[:], start=True, stop=True), r=[RK, hsel], w=[pV])
                kb.op("dve", lambda e: e.tensor_copy(out=sbon[:, ck0:ck0 + nck, :], in_=pV[:, 256:256 + 2 * nck].rearrange("p (c h) -> p c h", h=2)), r=[pV], w=[sbon])
            kb.barrier()
            if upto == "C":
                break
            for d in range(2):
                for hh in range(2):
                    kb.op("pool", lambda e, d=d, hh=hh: e.memset(ST[d][hh][:], 0.0), w=[ST[d][hh]])
            for step in range(NCK):
                cks = [order[0][step], order[1][step]]
                ob = [OPB[d][step % 2] for d in range(2)]
                for d in range(2):
                    c = cks[d]
                    for q in range(4):
                        kb.dma("sp" if q % 2 else "act", ob[d][q][:], OPS[d, q][:, c * 128:(c + 1) * 128], w=[ob[d][q]])
                AHd = [ob[d][0] for d in range(2)]; RHd = [ob[d][1] for d in range(2)]; BHd = [ob[d][2] for d in range(2)]; KHd = [ob[d][3] for d in range(2)]
                mS = [(ML_S, MU_S), (MU_S, ML_S)]
                mI = [MU_I, ML_I]
                HS = (slice(0, 64), slice(64, 128))

                def mm2(e, out_t, lhs, rhs):
                    ins = None
                    for hh in range(2):
                        ins = e.matmul(out_t[:, hh * 128:(hh + 1) * 128], lhsT=lhs[hh][:], rhs=rhs[:], start=True, stop=True)
                    return ins
                HSs = (slice(0, 64), slice(64, 128))
                for d in range(2):
                    c = cks[d]
                    for q, oq in ((0, 0), (1, 2), (2, 3)):
                        for hh in range(2):
                            pd = PADS[d][q][hh][step % 2]
                            kb.dma("sp" if (q + hh) % 2 else "act", pd[HSs[hh], :], OPS[d, oq][HSs[hh], c * 128:(c + 1) * 128], w=[pd])
                AP_ = [[PADS[d][0][hh][step % 2] for hh in range(2)] for d in range(2)]; BP_ = [[PADS[d][1][hh][step % 2] for hh in range(2)] for d in range(2)]; KP_ = [[PADS[d][2][hh][step % 2] for hh in range(2)] for d in range(2)]

                def evm(eng, dst, src, mask):
                    kb.op(eng, lambda e: e.tensor_tensor(out=dst[:], in0=src[:].rearrange("p (h s) -> p h s", h=2), in1=mask.unsqueeze(1).to_broadcast([128, 2, 128]), op=ALU.mult), r=[src, cst], w=[dst])
                for d in range(2):
                    kb.op("pe", lambda e, d=d: mm2(e, pP[d], AP_[d], BHd[d]), r=AP_[d] + [BHd[d]], w=[pP[d]])
                    kb.op("pe", lambda e, d=d: mm2(e, pPT[d], BP_[d], AHd[d]), r=BP_[d] + [AHd[d]], w=[pPT[d]])
                    kb.op("act", lambda e, d=d: e.activation(out=Mf[d][:].rearrange("p h s -> p (h s)"), in_=pP[d][:], func=AF.Copy), r=[pP[d]], w=[Mf[d]])
                    kb.op("act", lambda e, d=d: e.activation(out=MTf[d][:].rearrange("p h s -> p (h s)"), in_=pPT[d][:], func=AF.Copy), r=[pPT[d]], w=[MTf[d]])
                    kb.op("pe", lambda e, d=d: mm2(e, pM[d], KP_[d], AHd[d]), r=KP_[d] + [AHd[d]], w=[pM[d]])
                    evm("dve", Mak[d], pM[d], mS[d][1])
                    c = cks[d]
                    for q, src in ((0, BHd[d]), (1, KHd[d])):
                        kb.op("dve", lambda e, q=q, src=src, d=d, c=c: e.tensor_scalar_mul(out=BKb[d][q][:], in0=src[:], scalar1=EL[d][:, c:c + 1]), r=[src, EL[d]], w=[BKb[d][q]])
                        kb.op("pe", lambda e, q=q, d=d: e.transpose(pT2[d][:, q * 128:(q + 1) * 128], BKb[d][q][:], IDN), r=[BKb[d][q], cst], w=[pT2[d]])
                        kb.op("act", lambda e, q=q, d=d: e.activation(out=BKt[d][q][:], in_=pT2[d][:, q * 128:(q + 1) * 128], func=AF.Copy), r=[pT2[d]], w=[BKt[d][q]])
                for d in range(2):
                    c = cks[d]

                    def rhs0(e, d=d, c=c):
                        ins = None
                        for hh in range(2):
                            e.matmul(pU[d][:, hh * 64:(hh + 1) * 64], lhsT=AHd[d][:], rhs=ST[d][hh][:], start=True, stop=False)
                            ins = e.matmul(pU[d][:, hh * 64:(hh + 1) * 64], lhsT=Mak[d][:, hh, :], rhs=Vtm[:, c, hh * 64:(hh + 1) * 64], start=False, stop=True)
                        return ins
                    kb.op("pe", rhs0, r=[AHd[d], ST[d][0], ST[d][1], Mak[d], Vtm], w=[pU[d]])
                    kb.op("act", lambda e, d=d: e.activation(out=Ub[d][0][:], in_=pU[d][:], func=AF.Copy), r=[pU[d]], w=[Ub[d][0]])
                    if dbg and step == 1 and d == 0 and bi == 0:
                        kb.dump(Ub[0][0], "d_U0"); kb.dump(Mak[0], "d_Mak"); kb.dump(AHd[0], "d_AH1")
                    kb.op("pe", lambda e, d=d: mm2(e, pM[d], BP_[d], RHd[d]), r=BP_[d] + [RHd[d]], w=[pM[d]])
                    evm("dve", Nrb[d], pM[d], mI[d])
                    kb.op("pe", lambda e, d=d: mm2(e, pM[d], KP_[d], RHd[d]), r=KP_[d] + [RHd[d]], w=[pM[d]])
                    evm("dve", Nrk[d], pM[d], mI[d])
                MKa = lambda d, l: cst2[:, (0 if d == 0 else 7) + l, :, :]
                MKb = lambda d, l: cst2[:, (7 if d == 0 else 0) + l, :, :]
                ID2 = cst2[:, 14, :, :]

                def mmh(e, out_t, lhs, rhs):
                    ins = None
                    for hh in range(2):
                        ins = e.matmul(out_t[:, hh * 128:(hh + 1) * 128], lhsT=lhs[:, hh, :], rhs=rhs[:, hh, :], start=True, stop=True)
                    return ins
                flat = lambda t: t[:].rearrange("p h s -> p (h s)")
                for d in range(2):
                    kb.op("pool", lambda e, d=d: e.tensor_tensor(out=Xb[d][0][:], in0=Mf[d][:], in1=MKa(d, 0), op=ALU.mult), r=[Mf[d], cst2], w=[Xb[d][0]])
                    kb.op("pool", lambda e, d=d: e.tensor_tensor(out=Xb[d][0][:], in0=Xb[d][0][:], in1=ID2, op=ALU.add), r=[Xb[d][0], cst2], w=[Xb[d][0]])
                    kb.op("pool", lambda e, d=d: e.tensor_tensor(out=XTb[d][0][:], in0=MTf[d][:], in1=MKb(d, 0), op=ALU.mult), r=[MTf[d], cst2], w=[XTb[d][0]])
                    kb.op("pool", lambda e, d=d: e.tensor_tensor(out=XTb[d][0][:], in0=XTb[d][0][:], in1=ID2, op=ALU.add), r=[XTb[d][0], cst2], w=[XTb[d][0]])
                for l in range(1, 7):
                    a, b = (l - 1) % 2, l % 2
                    for d in range(2):
                        kb.op("pool", lambda e, d=d, l=l: e.tensor_tensor(out=Mm[d][:], in0=Mf[d][:], in1=MKa(d, l), op=ALU.mult), r=[Mf[d], cst2], w=[Mm[d]])
                        kb.op("pool", lambda e, d=d, l=l: e.tensor_tensor(out=MTm[d][:], in0=MTf[d][:], in1=MKb(d, l), op=ALU.mult), r=[MTf[d], cst2], w=[MTm[d]])
                        kb.op("pe", lambda e, d=d, a=a: mmh(e, pP[d], MTm[d], Xb[d][a]), r=[MTm[d], Xb[d][a]], w=[pP[d]])
                        kb.op("pe", lambda e, d=d, a=a: mmh(e, pPT[d], Mm[d], XTb[d][a]), r=[Mm[d], XTb[d][a]], w=[pPT[d]])
                        kb.op("act", lambda e, d=d: e.activation(out=flat(T1s[d]), in_=pP[d][:], func=AF.Copy), r=[pP[d]], w=[T1s[d]])
                        kb.op("dve", lambda e, d=d: e.tensor_copy(out=flat(T2s[d]), in_=pPT[d][:]), r=[pPT[d]], w=[T2s[d]])
                    for d in range(2):
                        if l < 6:
                            kb.op("pe", lambda e, d=d, a=a: mmh(e, pM[d], XTb[d][a], T1s[d]), r=[XTb[d][a], T1s[d]], w=[pM[d]])
                            kb.op("dve", lambda e, d=d, a=a, b=b: e.tensor_tensor(out=flat(Xb[d][b]), in0=pM[d][:], in1=flat(Xb[d][a]), op=ALU.add), r=[pM[d], Xb[d][a]], w=[Xb[d][b]])
                        kb.op("pe", lambda e, d=d, a=a: mmh(e, pT2[d], Xb[d][a], T2s[d]), r=[Xb[d][a], T2s[d]], w=[pT2[d]])
                        kb.op("dve", lambda e, d=d, a=a, b=b: e.tensor_tensor(out=flat(XTb[d][b]), in0=pT2[d][:], in1=flat(XTb[d][a]), op=ALU.add), r=[pT2[d], XTb[d][a]], w=[XTb[d][b]])
                for d in range(2):
                    def app(e, d=d):
                        ins = None
                        for hh in range(2):
                            ins = e.matmul(pU[d][:, hh * 64:(hh + 1) * 64], lhsT=XTb[d][0][:, hh, :], rhs=Ub[d][0][:, hh * 64:(hh + 1) * 64], start=True, stop=True)
                        return ins
                    kb.op("pe", app, r=[XTb[d][0], Ub[d][0]], w=[pU[d]])
                    kb.op("act", lambda e, d=d: e.activation(out=Ub[d][1][:], in_=pU[d][:], func=AF.Copy), r=[pU[d]], w=[Ub[d][1]])
                UF = 1
                for d in range(2):
                    c = cks[d]
                    U = Ub[d][UF]

                    def ymm(e, d=d, c=c, U=U):
                        ins = None
                        for hh in range(2):
                            o = pY[d][:, hh * 64:(hh + 1) * 64]
                            e.matmul(o, lhsT=RHd[d][:], rhs=ST[d][hh][:], start=True, stop=False)
                            e.matmul(o, lhsT=Nrb[d][:, hh, :], rhs=U[:, hh * 64:(hh + 1) * 64], start=False, stop=False)
                            ins = e.matmul(o, lhsT=Nrk[d][:, hh, :], rhs=Vtm[:, c, hh * 64:(hh + 1) * 64], start=False, stop=True)
                        return ins
                    kb.op("pe", ymm, r=[RHd[d], ST[d][0], ST[d][1], Nrb[d], Nrk[d], U, Vtm], w=[pY[d]])
                    kb.op("act", lambda e, d=d, c=c: e.activation(out=Yd[d][:, c, :], in_=pY[d][:], func=AF.Copy), r=[pY[d]], w=[Yd[d]])
                    if dbg and step == 1 and d == 0 and bi == 0:
                        kb.dump(U, "d_U1"); kb.dump(Nrb[0], "d_Nrb"); kb.dump(Nrk[0], "d_Nrk"); kb.dump(RHd[0], "d_RH1")

                    def smm(e, d=d, c=c, U=U):
                        e.matmul(pS[d][:], lhsT=BKt[d][0][:], rhs=U[:], start=True, stop=False)
                        return e.matmul(pS[d][:], lhsT=BKt[d][1][:], rhs=Vtm[:, c, :], start=False, stop=True)
                    kb.op("pe", smm, r=[BKt[d][0], BKt[d][1], U, Vtm], w=[pS[d]])
                    for hh in range(2):
                        kb.op("dve", lambda e, d=d, c=c, hh=hh: e.scalar_tensor_tensor(out=ST[d][hh][HS[hh], :], in0=ST[d][hh][HS[hh], :], scalar=EL[d][HS[hh], c:c + 1], in1=pS[d][HS[hh], hh * 64:(hh + 1) * 64], op0=ALU.mult, op1=ALU.add), r=[ST[d][hh], EL[d], pS[d]], w=[ST[d][hh]])
                    if dbg and step == 0 and d == 0 and bi == 0:
                        kb.dump(BKt[0][0], "d_BKt0"); kb.dump(BKt[0][1], "d_BKt1"); kb.dump(EL[0], "d_EL"); kb.dump(U, "d_U"); kb.dump(BKb[0][0], "d_BKb0")
            if upto == "D":
                break
            if dbg:
                dbgo = kb.dram("dbgo", [2, NT, 128], kind="ExternalOutput")
                for d in range(2):
                    kb.dma("sp", dbgo[d].rearrange("(c p) n -> p c n", p=128), Yd[d][:], r=[Yd[d]], store=True)
                kb.barrier()
            Y = Yd[0]
            Yv = lambda: Y[:].rearrange("p c (h v) -> p (c h) v", v=64)
            NG = NCK * 2
            kb.op("dve", lambda e: e.tensor_tensor(out=Y[:], in0=Yd[0][:], in1=Yd[1][:], op=ALU.add), r=[Yd[0], Yd[1]], w=[Y])
            kb.op("dve", lambda e: e.tensor_reduce(out=st1[:], in_=Yv(), axis=AX.X, op=ALU.add), r=[Y], w=[st1])
            kb.op("dve", lambda e: e.tensor_scalar_mul(out=st1[:], in0=st1[:], scalar1=1.0 / 64), r=[st1], w=[st1])
            kb.op("dve", lambda e: e.tensor_tensor(out=Yv(), in0=Yv(), in1=st1[:].unsqueeze(2).to_broadcast([128, NG, 64]), op=ALU.subtract), r=[Y, st1], w=[Y])
            Y2 = Yd[1]
            kb.op("dve", lambda e: e.tensor_tensor(out=Y2[:], in0=Y[:], in1=Y[:], op=ALU.mult), r=[Y], w=[Y2])
            kb.op("dve", lambda e: e.tensor_reduce(out=st2[:], in_=Y2[:].rearrange("p c (h v) -> p (c h) v", v=64), axis=AX.X, op=ALU.add), r=[Y2], w=[st2])
            kb.op("dve", lambda e: e.tensor_scalar(out=st2[:], in0=st2[:], scalar1=1.0 / 64, scalar2=64e-5, op0=ALU.mult, op1=ALU.add), r=[st2], w=[st2])
            kb.op("act", lambda e: e.activation(out=st2[:], in_=st2[:], func=AF.Sqrt), r=[st2], w=[st2])
            kb.op("dve", lambda e: e.reciprocal(out=st2[:], in_=st2[:]), r=[st2], w=[st2])
            kb.op("dve", lambda e: e.tensor_tensor(out=Yv(), in0=Yv(), in1=st2[:].unsqueeze(2).to_broadcast([128, NG, 64]), op=ALU.mult), r=[Y, st2], w=[Y])
            kb.op("dve", lambda e: e.tensor_tensor(out=Y[:], in0=Y[:], in1=lnw[:, cs].unsqueeze(1).to_broadcast([128, NCK, 128]), op=ALU.mult), r=[Y, lnw], w=[Y])
            kb.op("dve", lambda e: e.tensor_tensor(out=Y[:], in0=Y[:], in1=lnb[:, cs].unsqueeze(1).to_broadcast([128, NCK, 128]), op=ALU.add), r=[Y, lnb], w=[Y])
            kb.op("dve", lambda e: e.tensor_tensor(out=Y2[:].rearrange("p c (h v) -> p (c h) v", v=64), in0=Vtm[:].rearrange("p c (h v) -> p (c h) v", v=64), in1=sbon[:].rearrange("p c h -> p (c h)").unsqueeze(2).to_broadcast([128, NG, 64]), op=ALU.mult), r=[Vtm, sbon], w=[Y2])
            kb.op("dve", lambda e: e.tensor_tensor(out=Y[:], in0=Y[:], in1=Y2[:], op=ALU.add), r=[Y, Y2], w=[Y])
            for c4 in range(0, NCK, 4):
                n4 = min(4, NCK - c4)
                g = gl[(c4 // 4) % 2]
                kb.dma("sp", g[:, :, 0:n4 * 128], LR[4:6].rearrange("q p t -> p q t")[:, :, c4 * 128:(c4 + n4) * 128], w=[g])

                def gmm(e, g=g, n4=n4):
                    ins = None
                    for cc in range(n4):
                        for kq in range(2):
                            ins = e.matmul(pW[:, cc * 128:(cc + 1) * 128], lhsT=g[:, kq, cc * 128:(cc + 1) * 128], rhs=g2[:, kq, cs], start=(kq == 0), stop=(kq == 1))
                    return ins
                kb.op("pe", gmm, r=[g, g2], w=[pW])
                kb.op("dve", lambda e, c4=c4, n4=n4: e.tensor_tensor(out=Y[:, c4:c4 + n4, :], in0=Y[:, c4:c4 + n4, :], in1=pW[:, 0:n4 * 128].rearrange("p (c n) -> p c n", n=128), op=ALU.mult), r=[Y, pW], w=[Y])
            kb.dma("sp", ya[:, cs].rearrange("(c p) n -> p c n", p=128), Y[:], r=[Y], store=True)
            kb.barrier()
    if upto in ("C", "D", "E"):
        return kb.finish()
    with kb.scope():
        lvec = kb.sb("lvec", [128, NLB, NVL]); kb.dma("sp", lvec[:], lvec_d[:], w=[lvec])
        c8 = kb.sb("c8", [128, NLB, 2])
        kb.op("act", lambda e: e.activation(out=c8[:], in_=lvec[:, :, 9:11], func=AF.Exp, scale=-1.0), r=[lvec], w=[c8])
        kb.op("act", lambda e: e.activation(out=c8[:], in_=c8[:], func=AF.Ln, bias=1.0), r=[c8], w=[c8])
        kb.op("dve", lambda e: e.tensor_scalar_mul(out=c8[:], in0=c8[:], scalar1=-8.0), r=[c8], w=[c8])
        wa = kb.sb("wa", [128, 2, NLB, 128]); wx = kb.sb("wx", [128, 2, NLB, 128])
        kb.dma("sp", wa[:], lwa_d[:].rearrange("d n c o -> c d n o"), w=[wa])
        kb.dma("sp", wx[:], lwx_d[:].rearrange("d n c o -> c d n o"), w=[wx])
        raw = kb.sb("lraw", [128, TW + 3]); XC = kb.sb("XC", [128, TW]); GB = kb.sb("GB", [128, TW])
        A_ = kb.sb("A_", [128, TW]); GI = kb.sb("GI", [128, TW]); U_ = kb.sb("U_", [128, TW]); H = kb.sb("H", [128, TW]); TT = kb.sb("TT", [128, TW])
        HF = kb.sb("HF", [128, NT]); hst = kb.sb("hst", [128, 1])
        pA = kb.ps("lpA", [128, 512]); pB = kb.ps("lpB", [128, 512])
        C2 = 0.7978845608028654 * 2.0
        ctx_t = [t for t in ctiles if not t[2]]; lat_t = [t for t in ctiles if t[2]]
        for lb in range(NLB):
            jx = NCHR + lb; jg = NCHR + NLB + lb
            L_ = lambda i: lvec[:, lb, i:i + 1]
            for d in range(2):
                tl = ctiles if d == 0 else ctx_t[::-1] + lat_t[::-1]
                first = True
                for (t0, W, lat) in tl:
                    s_lo, s_hi = (TC, NT) if lat else (0, TC)
                    lo = max(s_lo, t0 - 1); hi = min(s_hi, t0 + W + 2)
                    kb.op("pool", lambda e: e.memset(raw[:], 0.0), w=[raw])
                    kb.dma("sp", raw[:, 1 - (t0 - lo):1 - (t0 - lo) + (hi - lo)], P[jx][:, lo:hi], w=[raw])
                    kb.op("dve", lambda e: e.tensor_scalar(out=XC[:, 0:W], in0=raw[:, 0:W], scalar1=L_(0), scalar2=L_(4), op0=ALU.mult, op1=ALU.add), r=[raw, lvec], w=[XC])
                    for j in range(1, 4):
                        kb.op("dve", lambda e: e.scalar_tensor_tensor(out=XC[:, 0:W], in0=raw[:, j:j + W], scalar=L_(j), in1=XC[:, 0:W], op0=ALU.mult, op1=ALU.add), r=[raw, lvec, XC], w=[XC])
                    for s0 in range(0, W, 512):
                        sw = min(512, W - s0)
                        kb.op("pe", lambda e: e.matmul(pA[:, 0:sw], lhsT=wa[:, d, lb, :], rhs=XC[:, s0:s0 + sw], start=True, stop=True), r=[wa, XC], w=[pA])
                        kb.op("act", lambda e: e.activation(out=A_[:, s0:s0 + sw], in_=pA[:, 0:sw], func=AF.Sigmoid, bias=L_(5 + d), scale=1.0), r=[pA, lvec], w=[A_])
                        kb.op("pe", lambda e: e.matmul(pB[:, 0:sw], lhsT=wx[:, d, lb, :], rhs=XC[:, s0:s0 + sw], start=True, stop=True), r=[wx, XC], w=[pB])
                        kb.op("act", lambda e: e.activation(out=GI[:, s0:s0 + sw], in_=pB[:, 0:sw], func=AF.Sigmoid, bias=L_(7 + d), scale=1.0), r=[pB, lvec], w=[GI])
                    kb.op("act", lambda e: e.activation(out=A_[:, 0:W], in_=A_[:, 0:W], func=AF.Exp, scale=c8[:, lb, d:d + 1]), r=[A_, c8], w=[A_])
                    kb.op("dve", lambda e: e.tensor_tensor(out=U_[:, 0:W], in0=XC[:, 0:W], in1=GI[:, 0:W], op=ALU.mult), r=[XC, GI], w=[U_])
                    kb.op("pool", lambda e: e.tensor_tensor(out=TT[:, 0:W], in0=A_[:, 0:W], in1=A_[:, 0:W], op=ALU.mult), r=[A_], w=[TT])
                    kb.op("dve", lambda e: e.tensor_scalar(out=TT[:, 0:W], in0=TT[:, 0:W], scalar1=-1.0, scalar2=1.0, op0=ALU.mult, op1=ALU.add), r=[TT], w=[TT])
                    kb.op("act", lambda e: e.activation(out=TT[:, 0:W], in_=TT[:, 0:W], func=AF.Sqrt), r=[TT], w=[TT])
                    kb.op("dve", lambda e: e.tensor_tensor(out=U_[:, 0:W], in0=U_[:, 0:W], in1=TT[:, 0:W], op=ALU.mult), r=[U_, TT], w=[U_])
                    init = 0.0 if first else hst[:, 0:1]
                    rr = [A_, U_] + ([] if first else [hst])
                    if d == 0:
                        kb.op("dve", lambda e: e.tensor_tensor_scan(out=H[:, 0:W], data0=A_[:, 0:W], data1=U_[:, 0:W], initial=init, op0=ALU.mult, op1=ALU.add), r=rr, w=[H])
                        kb.op("dve", lambda e: e.tensor_copy(out=hst[:], in_=H[:, W - 1:W]), r=[H], w=[hst])
                        kb.op("pool", lambda e: e.tensor_copy(out=HF[:, t0:t0 + W], in_=H[:, 0:W]), r=[H], w=[HF])
                    else:
                        kb.op("dve", lambda e: e.tensor_tensor_scan(out=H[:, 0:W][:, ::-1], data0=A_[:, 0:W][:, ::-1], data1=U_[:, 0:W][:, ::-1], initial=init, op0=ALU.mult, op1=ALU.add), r=rr, w=[H])
                        kb.op("dve", lambda e: e.tensor_copy(out=hst[:], in_=H[:, 0:1]), r=[H], w=[hst])
                        kb.dma("sp", GB[:, 0:W], P[jg][:, t0:t0 + W], w=[GB])
                        kb.op("act", lambda e: e.activation(out=TT[:, 0:W], in_=GB[:, 0:W], func=AF.Square), r=[GB], w=[TT])
                        kb.op("dve", lambda e: e.tensor_scalar(out=TT[:, 0:W], in0=TT[:, 0:W], scalar1=C2 * 0.044715, scalar2=C2, op0=ALU.mult, op1=ALU.add), r=[TT], w=[TT])
                        kb.op("dve", lambda e: e.tensor_tensor(out=TT[:, 0:W], in0=TT[:, 0:W], in1=GB[:, 0:W], op=ALU.mult), r=[TT, GB], w=[TT])
                        kb.op("act", lambda e: e.activation(out=TT[:, 0:W], in_=TT[:, 0:W], func=AF.Sigmoid), r=[TT], w=[TT])
                        kb.op("dve", lambda e: e.tensor_tensor(out=GB[:, 0:W], in0=GB[:, 0:W], in1=TT[:, 0:W], op=ALU.mult), r=[GB, TT], w=[GB])
                        kb.op("dve", lambda e: e.tensor_tensor(out=H[:, 0:W], in0=H[:, 0:W], in1=HF[:, t0:t0 + W], op=ALU.add), r=[H, HF], w=[H])
                        kb.op("dve", lambda e: e.tensor_tensor(out=GB[:, 0:W], in0=GB[:, 0:W], in1=H[:, 0:W], op=ALU.mult), r=[GB, H], w=[GB])
                        kb.dma("sp", yb[lb * 128:(lb + 1) * 128, t0:t0 + W], GB[:, 0:W], r=[GB], store=True)
                    first = False
    nc = kb.finish()
    return nc


def sincos_tab(TL, D):
    quarter = D // 4
    omega = (10000.0 ** (-np.arange(quarter, dtype=np.float32) / quarter)).astype(np.float32)
    idx = np.arange(64, dtype=np.float32)
    ang = idx[:, None] * omega[None, :]
    blk = np.concatenate([np.sin(ang), np.cos(ang)], -1).astype(np.float32)
    return np.concatenate([blk, blk], -1).T.copy()


def _pm(v, KC):
    return np.ascontiguousarray(v.reshape(KC, 128).T)


def p1_core_inputs(b, g, x, ctx, mods0, prm, NBLK, NLB):
    D = x.shape[2]; KC = D // 128; TL = x.shape[1]
    RW = NBLK * 128; LW = NLB * 128
    AW = prm["w0"].shape[-1]; BW = prm["lam"].shape[-1]
    DL = prm["w2"].shape[1]; GLo = prm["g2"].shape[0]
    w_in = prm["w_in"]; mu_full = prm["mu"]
    base = 3 * AW
    chunks = []
    for q in range(3):
        for bi in range(NBLK):
            c0 = q * AW + g * RW + bi * 128
            chunks.append(np.arange(c0, c0 + 128))
    for q in range(4):
        chunks.append(np.arange(base + q * DL, base + (q + 1) * DL))
    for q in range(2):
        chunks.append(np.arange(base + 4 * DL + q * 128, base + 4 * DL + (q + 1) * 128))
    NCHR = len(chunks)
    rc = 3 * AW + 4 * DL + GLo
    for q in range(2):
        for lb in range(NLB):
            c0 = rc + q * BW + g * LW + lb * 128
            chunks.append(np.arange(c0, c0 + 128))
    NCH = len(chunks)
    win = np.zeros((NCH, D, 128), np.float32)
    mu = np.zeros((128, NCHR), np.float32)
    for j, cols in enumerate(chunks):
        win[j, :, :len(cols)] = w_in[:, cols]
        if j < NCHR:
            mu[:len(cols), j] = mu_full[cols]
    sl = slice(g * RW, (g + 1) * RW)
    w2 = np.zeros((2, 128, RW), np.float32); w2[:, :DL] = prm["w2"][:, :, sl]
    a2 = np.zeros((2, 128, RW), np.float32); a2[:, :DL] = prm["a2"][:, :, sl]
    g2 = np.ascontiguousarray(prm["g2"][:, sl].reshape(2, 128, RW))
    vecs = [prm["w0"][0], prm["w0"][1], prm["a0"][0], prm["a0"][1], prm["k_k"], prm["k_a"], prm["r_k"].reshape(-1)]
    rvec = np.stack([v[sl].reshape(NBLK, 128).T for v in vecs], -1).astype(np.float32)
    lnx = np.concatenate([prm["lnx_w"][sl], prm["lnx_b"][sl]])[None, :].astype(np.float32)
    ls = slice(g * LW, (g + 1) * LW)
    lv = [prm["conv_w"][i] for i in range(4)] + [prm["conv_b"], prm["ba"][0], prm["ba"][1], prm["bx"][0], prm["bx"][1], prm["lam"][0], prm["lam"][1]]
    lvec = np.stack([v[ls].reshape(NLB, 128).T for v in lv], -1).astype(np.float32)
    lwa = np.ascontiguousarray(prm["wa"][:, g * NLB:(g + 1) * NLB]); lwx = np.ascontiguousarray(prm["wx"][:, g * NLB:(g + 1) * NLB])
    m = mods0
    mods = np.stack([_pm(m[b, 0:D], KC), _pm(m[b, D:2 * D], KC), _pm(m[2, 0:D], KC), _pm(m[2, D:2 * D], KC)], -1).astype(np.float32)
    tab = sincos_tab(TL, D)
    ptab = np.ascontiguousarray(tab.reshape(KC, 128, 64).transpose(1, 0, 2))
    return {"xT": np.ascontiguousarray(x[b].T), "cT": np.ascontiguousarray(ctx[b].T), "ptab": ptab, "mods": mods,
            "win": win, "mu": mu, "w2": w2, "a2": a2, "g2": g2, "rvec": np.ascontiguousarray(rvec), "lnx": lnx,
            "lvec": np.ascontiguousarray(lvec), "lwa": lwa, "lwx": lwx, "cst": make_consts(), "cst2": make_consts2()}


ALPHA_DN = 4.0 ** 0.25
LN_EPS_ = 1e-5


def build_post(D=4096, KM=4096, segs=((1024, 0), (64, 1)), mode="proj", router=True, hnext=True):
    kb = KB()
    KC = D // 128
    NTK = sum(n for n, _ in segs)
    nsets = max(sset for _, sset in segs) + 1
    NBK = (NTK + 127) // 128
    tiles = []
    t = 0
    for si, (n, sset) in enumerate(segs):
        for t0 in range(0, n, 512):
            tiles.append((t + t0, min(512, n - t0), sset, si == 0, t0))
        t += n
    tinfo = {tl[0]: (i, tl) for i, tl in enumerate(tiles)}
    I = lambda name, shape, dt=F32: kb.dram(name, shape, dt, kind="ExternalInput")
    resT = I("resT", [D, NTK]); modp_d = I("modp", [128, KC, nsets, 3]); lnp_d = I("lnp", [128, KC, 2])
    NR0 = segs[0][0] // 64
    prow_d = I("prow", [128, KC // 2, NR0]); pcol_d = I("pcol", [128, KC // 2, 64])
    if mode == "proj":
        yT = I("yT", [KM, NTK]); wout = I("wout", [KC, KM, 128])
    else:
        y1T = I("y1T", [D, NTK]); y2T = I("y2T", [D, NTK]); g12_d = I("g12", [2, NTK])
    if router:
        rw_d = I("rw", [128, KC, 16]); rb_d = I("rb", [1, 16]); cst_d = I("cst", [128, 20, 128])
        Gout = kb.dram("G", [NBK * 128, 16], kind="ExternalOutput")
    res_out = kb.dram("res_out", [D, NTK], kind="ExternalOutput")
    if hnext:
        h_out = kb.dram("h_out", [D, NTK], BF16, kind="ExternalOutput")
    zT = kb.dram("zT", [D, NTK])
    modp = kb.sb("modp", [128, KC, nsets, 3]); kb.dma("sp", modp[:], modp_d[:], w=[modp])
    lnp = kb.sb("lnp", [128, KC, 2]); kb.dma("sp", lnp[:], lnp_d[:], w=[lnp])
    prow = kb.sb("prow", [128, KC // 2, NR0]); kb.dma("sp", prow[:], prow_d[:], w=[prow])
    pcol = kb.sb("pcol", [128, KC // 2, 64]); kb.dma("sp", pcol[:], pcol_d[:], w=[pcol])
    if hnext:
        kb.op("dve", lambda e: e.tensor_scalar_add(out=modp[:, :, :, 2:3], in0=modp[:, :, :, 2:3], scalar1=1.0), r=[modp], w=[modp])
    acc1 = kb.sb("acc1", [128, NTK]); acc2 = kb.sb("acc2", [128, NTK])
    kb.op("pool", lambda e: e.memset(acc1[:], 0.0), w=[acc1]); kb.op("pool", lambda e: e.memset(acc2[:], 0.0), w=[acc2])
    ones = kb.sb("ones", [128, 128]); kb.op("pool", lambda e: e.memset(ones[:], 1.0), w=[ones])
    rts = [kb.sb("rt%d" % i, [128, 512]) for i in range(3)]
    zts = [kb.sb("zt%d" % i, [128, 512]) for i in range(3)]
    sqs = [kb.sb("sq%d" % i, [128, 512]) for i in range(2)]
    cnt = [0]

    def zstage(j, t0, W, o_ap, o_tl):
        _, (tt0, _, sset, seg0, off) = tinfo[t0]
        k = cnt[0]; cnt[0] += 1
        rt = rts[k % 3]; zt = zts[k % 3]; sq = sqs[k % 2]
        kb.dma("sp", rt[:, 0:W], resT[j * 128:(j + 1) * 128, t0:t0 + W], w=[rt])
        if seg0:
            nr = W // 64; r0 = off // 64
            rv = rt[:, 0:W].rearrange("p (r q) -> p r q", q=64)
            if j < KC // 2:
                pin = prow[:, j, r0:r0 + nr].unsqueeze(2).to_broadcast([128, nr, 64])
            else:
                pin = pcol[:, j - KC // 2, :].unsqueeze(1).to_broadcast([128, nr, 64])
            kb.op("dve", lambda e: e.tensor_tensor(out=rv, in0=rv, in1=pin, op=ALU.add), r=[rt, prow, pcol], w=[rt])
        kb.op("act", lambda e: e.activation(out=rt[:, 0:W], in_=rt[:, 0:W], func=AF.Copy, scale=ALPHA_DN), r=[rt], w=[rt])
        kb.op("dve", lambda e: e.scalar_tensor_tensor(out=zt[:, 0:W], in0=o_ap, scalar=modp[:, j, sset, 0:1], in1=rt[:, 0:W], op0=ALU.mult, op1=ALU.add), r=[o_tl, modp, rt], w=[zt])
        kb.op("pool", lambda e: e.tensor_tensor(out=acc1[:, t0:t0 + W], in0=acc1[:, t0:t0 + W], in1=zt[:, 0:W], op=ALU.add), r=[acc1, zt], w=[acc1])
        kb.op("act", lambda e: e.activation(out=sq[:, 0:W], in_=zt[:, 0:W], func=AF.Square), r=[zt], w=[sq])
        kb.op("pool", lambda e: e.tensor_tensor(out=acc2[:, t0:t0 + W], in0=acc2[:, t0:t0 + W], in1=sq[:, 0:W], op=ALU.add), r=[acc2, sq], w=[acc2])
        kb.dma("sp", zT[j * 128:(j + 1) * 128, t0:t0 + W], zt[:, 0:W], r=[zt], store=True)

    gtiles = [(t0, W, True) for (t0, W, _, _, _) in tiles]
    if mode == "proj":
        yTb = kb.dram("yTb", [KM, NTK], BF16)
        KMC = KM // 128
        with kb.scope():
            GA = min(8, KMC)
            xs = [kb.sb("cxs%d" % i, [128, GA, 512]) for i in range(2)]
            hb = [kb.sb("chb%d" % i, [128, GA, 512], BF16) for i in range(2)]
            yv = yT[:].rearrange("(c p) t -> p c t", p=128); ybv = yTb[:].rearrange("(c p) t -> p c t", p=128)
            it = 0
            for (t0, W, _) in gtiles:
                for g in range(KMC // GA):
                    x = xs[it % 2]; h = hb[it % 2]; it += 1
                    kb.dma("sp", x[:, :, 0:W], yv[:, g * GA:(g + 1) * GA, t0:t0 + W], w=[x])
                    kb.op("act" if it % 2 else "dve", (lambda e: e.activation(out=h[:, :, 0:W], in_=x[:, :, 0:W], func=AF.Copy)) if it % 2 else (lambda e: e.tensor_copy(out=h[:, :, 0:W], in_=x[:, :, 0:W])), r=[x], w=[h])
                    kb.dma("sp", ybv[:, g * GA:(g + 1) * GA, t0:t0 + W], h[:, :, 0:W], r=[h], store=True)
        gemm_fm(kb, wout, KC, KM, yTb, NTK, gtiles, lambda j, t0, W, p: zstage(j, t0, W, p[:, 0:W], p), dbuf=True)
    else:
        with kb.scope():
            G1 = kb.sb("G1b", [128, NTK]); G2 = kb.sb("G2b", [128, NTK])
            kb.dma("sp", G1[:], g12_d[0:1, :].partition_broadcast(128), w=[G1])
            kb.dma("sp", G2[:], g12_d[1:2, :].partition_broadcast(128), w=[G2])
            y1s = [kb.sb("y1s%d" % i, [128, 512]) for i in range(2)]; y2s = [kb.sb("y2s%d" % i, [128, 512]) for i in range(2)]
            n = 0
            for (t0, W, _) in gtiles:
                for j in range(KC):
                    a = y1s[n % 2]; b = y2s[n % 2]; n += 1
                    kb.dma("sp", a[:, 0:W], y1T[j * 128:(j + 1) * 128, t0:t0 + W], w=[a])
                    kb.dma("act", b[:, 0:W], y2T[j * 128:(j + 1) * 128, t0:t0 + W], w=[b])
                    kb.op("dve", lambda e: e.tensor_tensor(out=a[:, 0:W], in0=a[:, 0:W], in1=G1[:, t0:t0 + W], op=ALU.mult), r=[a, G1], w=[a])
                    kb.op("pool", lambda e: e.tensor_tensor(out=b[:, 0:W], in0=b[:, 0:W], in1=G2[:, t0:t0 + W], op=ALU.mult), r=[b, G2], w=[b])
                    kb.op("dve", lambda e: e.tensor_tensor(out=a[:, 0:W], in0=a[:, 0:W], in1=b[:, 0:W], op=ALU.add), r=[a, b], w=[a])
                    zstage(j, t0, W, a[:, 0:W], a)
    kb.barrier()
    with kb.scope():
        pst = kb.ps("pst", [128, 512]); pr = kb.ps("pr", [128, 512]); ptr = kb.ps("ptr", [128, 512])
        mean = kb.sb("mean", [128, NTK]); rstd = kb.sb("rstd", [128, NTK]); tmp = kb.sb("tmpst", [128, 512])
        if router:
            rw = kb.sb("rw", [128, KC, 16]); kb.dma("sp", rw[:], rw_d[:], w=[rw])
            rbb = kb.sb("rbb", [128, 16]); kb.dma("sp", rbb[:], rb_d[:].partition_broadcast(128), w=[rbb])
            cst = kb.sb("cstp", [128, 128]); kb.dma("sp", cst[:], cst_d[:, 0, :], w=[cst])
            lg = kb.sb("lg", [16, 512])
            A = kb.sb("Aaff", [128, NBK, 16]); kb.op("pool", lambda e: e.memset(A[:], 0.0), w=[A])
        zl = [kb.sb("zl%d" % i, [128, 512]) for i in range(3)]
        hf = [kb.sb("hf%d" % i, [128, 512]) for i in range(2)]
        hbf = [kb.sb("hbf%d" % i, [128, 512], BF16) for i in range(2)]
        n = 0
        for (t0, W, sset, seg0, off) in tiles:
            kb.op("pe", lambda e: e.matmul(pst[:, 0:W], lhsT=ones[:], rhs=acc1[:, t0:t0 + W], start=True, stop=True), r=[ones, acc1], w=[pst])
            kb.op("act", lambda e: e.activation(out=mean[:, t0:t0 + W], in_=pst[:, 0:W], func=AF.Copy, scale=1.0 / D), r=[pst], w=[mean])
            kb.op("pe", lambda e: e.matmul(pst[:, 0:W], lhsT=ones[:], rhs=acc2[:, t0:t0 + W], start=True, stop=True), r=[ones, acc2], w=[pst])
            kb.op("dve", lambda e: e.tensor_tensor(out=tmp[:, 0:W], in0=mean[:, t0:t0 + W], in1=mean[:, t0:t0 + W], op=ALU.mult), r=[mean], w=[tmp])
            kb.op("dve", lambda e: e.scalar_tensor_tensor(out=rstd[:, t0:t0 + W], in0=pst[:, 0:W], scalar=1.0 / D, in1=tmp[:, 0:W], op0=ALU.mult, op1=ALU.subtract), r=[pst, tmp], w=[rstd])
            kb.op("dve", lambda e: e.tensor_scalar_add(out=rstd[:, t0:t0 + W], in0=rstd[:, t0:t0 + W], scalar1=LN_EPS_), r=[rstd], w=[rstd])
            kb.op("act", lambda e: e.activation(out=rstd[:, t0:t0 + W], in_=rstd[:, t0:t0 + W], func=AF.Sqrt), r=[rstd], w=[rstd])
            kb.op("dve", lambda e: e.reciprocal(out=rstd[:, t0:t0 + W], in_=rstd[:, t0:t0 + W]), r=[rstd], w=[rstd])
            for j in range(KC):
                z = zl[n % 3]; h = hf[n % 2]; hb_ = hbf[n % 2]; n += 1
                kb.dma("sp", z[:, 0:W], zT[j * 128:(j + 1) * 128, t0:t0 + W], w=[z])
                kb.op("dve", lambda e: e.tensor_tensor(out=z[:, 0:W], in0=z[:, 0:W], in1=mean[:, t0:t0 + W], op=ALU.subtract), r=[z, mean], w=[z])
                kb.op("pool", lambda e: e.tensor_tensor(out=z[:, 0:W], in0=z[:, 0:W], in1=rstd[:, t0:t0 + W], op=ALU.mult), r=[z, rstd], w=[z])
                kb.op("act", lambda e: e.activation(out=z[:, 0:W], in_=z[:, 0:W], func=AF.Identity, scale=lnp[:, j, 0:1], bias=lnp[:, j, 1:2]), r=[z, lnp], w=[z])
                kb.dma("sp", res_out[j * 128:(j + 1) * 128, t0:t0 + W], z[:, 0:W], r=[z], store=True)
                if hnext:
                    kb.op("dve", lambda e: e.tensor_scalar(out=h[:, 0:W], in0=z[:, 0:W], scalar1=modp[:, j, sset, 2:3], scalar2=modp[:, j, sset, 1:2], op0=ALU.mult, op1=ALU.add), r=[z, modp], w=[h])
                    kb.op("act", lambda e: e.activation(out=hb_[:, 0:W], in_=h[:, 0:W], func=AF.Copy), r=[h], w=[hb_])
                    kb.dma("act", h_out[j * 128:(j + 1) * 128, t0:t0 + W], hb_[:, 0:W], r=[hb_], store=True)
                    if router:
                        kb.op("pe", lambda e: e.matmul(pr[0:16, 0:W], lhsT=rw[:, j, :], rhs=h[:, 0:W], start=(j == 0), stop=(j == KC - 1)), r=[rw, h], w=[pr])
            if router:
                kb.op("dve", lambda e: e.tensor_copy(out=lg[:, 0:W], in_=pr[0:16, 0:W]), r=[pr], w=[lg])
                for b0 in range(0, W, 128):
                    bw = min(128, W - b0); blk = (t0 + b0) // 128
                    kb.op("pe", lambda e: e.transpose(ptr[0:bw, 0:16], lg[:, b0:b0 + bw], cst[0:16, 0:16]), r=[lg, cst], w=[ptr])
                    kb.op("act", lambda e: e.activation(out=A[0:bw, blk, :], in_=ptr[0:bw, 0:16], func=AF.Sigmoid), r=[ptr], w=[A])
        if router:
            T = lambda nm, shp: kb.sb(nm, shp)
            Bz = T("Bz", [128, NBK, 16]); gs = T("gs", [128, NBK, 4]); ps_ = T("pairs", [128, NBK, 4]); gm = T("gm", [128, NBK]); ing = T("ing", [128, NBK, 4])
            mb = T("mb", [128, NBK, 16]); mbias = T("mbias", [128, NBK, 4]); m1 = T("m1", [128, NBK]); s1 = T("s1", [128, NBK, 16]); s2 = T("s2", [128, NBK, 16]); gsum = T("gsum", [128, NBK])
            kb.op("dve", lambda e: e.tensor_tensor(out=Bz[:], in0=A[:], in1=rbb[:].unsqueeze(1).to_broadcast([128, NBK, 16]), op=ALU.add), r=[A, rbb], w=[Bz])
            B4 = Bz[:].rearrange("p b (g m) -> p b g m", m=4)
            first = True
            for i in range(4):
                for jx in range(i + 1, 4):
                    dst = gs if first else ps_
                    kb.op("dve", lambda e: e.tensor_tensor(out=dst[:], in0=B4[:, :, :, i], in1=B4[:, :, :, jx], op=ALU.add), r=[Bz], w=[dst])
                    if not first:
                        kb.op("dve", lambda e: e.tensor_tensor(out=gs[:], in0=gs[:], in1=ps_[:], op=ALU.max), r=[gs, ps_], w=[gs])
                    first = False
            kb.op("dve", lambda e: e.tensor_reduce(out=gm[:], in_=gs[:], axis=AX.X, op=ALU.max), r=[gs], w=[gm])
            kb.op("dve", lambda e: e.tensor_tensor(out=ing[:], in0=gs[:], in1=gm[:].unsqueeze(2).to_broadcast([128, NBK, 4]), op=ALU.is_equal), r=[gs, gm], w=[ing])
            kb.op("dve", lambda e: e.tensor_scalar(out=mbias[:], in0=ing[:], scalar1=1e30, scalar2=-1e30, op0=ALU.mult, op1=ALU.add), r=[ing], w=[mbias])
            M4 = mb[:].rearrange("p b (g m) -> p b g m", m=4)
            kb.op("dve", lambda e: e.tensor_tensor(out=M4, in0=B4, in1=ing[:].unsqueeze(3).to_broadcast([128, NBK, 4, 4]), op=ALU.mult), r=[Bz, ing], w=[mb])
            kb.op("dve", lambda e: e.tensor_tensor(out=M4, in0=M4, in1=mbias[:].unsqueeze(3).to_broadcast([128, NBK, 4, 4]), op=ALU.add), r=[mb, mbias], w=[mb])
            bc16 = lambda t_: t_[:].unsqueeze(2).to_broadcast([128, NBK, 16])
            kb.op("dve", lambda e: e.tensor_reduce(out=m1[:], in_=mb[:], axis=AX.X, op=ALU.max), r=[mb], w=[m1])
            kb.op("dve", lambda e: e.tensor_tensor(out=s1[:], in0=mb[:], in1=bc16(m1), op=ALU.is_equal), r=[mb, m1], w=[s1])
            kb.op("dve", lambda e: e.scalar_tensor_tensor(out=mb[:], in0=s1[:], scalar=-1e30, in1=mb[:], op0=ALU.mult, op1=ALU.add), r=[s1, mb], w=[mb])
            kb.op("dve", lambda e: e.tensor_reduce(out=m1[:], in_=mb[:], axis=AX.X, op=ALU.max), r=[mb], w=[m1])
            kb.op("dve", lambda e: e.tensor_tensor(out=s2[:], in0=mb[:], in1=bc16(m1), op=ALU.is_equal), r=[mb, m1], w=[s2])
            kb.op("dve", lambda e: e.tensor_tensor(out=s1[:], in0=s1[:], in1=s2[:], op=ALU.add), r=[s1, s2], w=[s1])
            kb.op("dve", lambda e: e.tensor_tensor(out=s1[:], in0=s1[:], in1=A[:], op=ALU.mult), r=[s1, A], w=[s1])
            kb.op("dve", lambda e: e.tensor_reduce(out=gsum[:], in_=s1[:], axis=AX.X, op=ALU.add), r=[s1], w=[gsum])
            kb.op("dve", lambda e: e.tensor_scalar_max(out=gsum[:], in0=gsum[:], scalar1=1e-30), r=[gsum], w=[gsum])
            kb.op("dve", lambda e: e.reciprocal(out=gsum[:], in_=gsum[:]), r=[gsum], w=[gsum])
            kb.op("dve", lambda e: e.tensor_tensor(out=s1[:], in0=s1[:], in1=bc16(gsum), op=ALU.mult), r=[s1, gsum], w=[s1])
            kb.dma("sp", Gout[:].rearrange("(b p) e -> p b e", p=128), s1[:], r=[s1], store=True)
    return kb.finish()


def build_ffn(D=4096, DE=1024, R=1536):
    kb = KB()
    I = lambda name, shape, dt=F32: kb.dram(name, shape, dt, kind="ExternalInput")
    XT = I("XT", [D, R], BF16)
    wgu = I("wgu", [2 * DE // 128, D, 128])
    wd = I("wd", [D // 128, DE, 128])
    YT = kb.dram("YT", [D, R], kind="ExternalOutput")
    HT = kb.dram("HT", [DE, R], BF16)
    tiles = [(t0, min(512, R - t0), True) for t0 in range(0, R, 512)]
    with kb.scope():
        sg = [kb.sb("sg%d" % i, [128, 512]) for i in range(2)]
        hb = [kb.sb("hbb%d" % i, [128, 512], BF16) for i in range(2)]

        def ev1(j, t0, W, p):
            q = j // 2
            if j % 2 == 0:
                kb.op("act", lambda e: e.activation(out=sg[q % 2][:, 0:W], in_=p[:, 0:W], func=AF.Silu), r=[p], w=[sg[q % 2]])
            else:
                kb.op("dve", lambda e: e.tensor_tensor(out=hb[q % 2][:, 0:W], in0=p[:, 0:W], in1=sg[q % 2][:, 0:W], op=ALU.mult), r=[p, sg[q % 2]], w=[hb[q % 2]])
                kb.dma("sp", HT[q * 128:(q + 1) * 128, t0:t0 + W], hb[q % 2][:, 0:W], r=[hb[q % 2]], store=True)
        gemm_fm(kb, wgu, 2 * DE // 128, D, XT, R, tiles, ev1, dbuf=True)
    with kb.scope():
        ob = [kb.sb("fob%d" % i, [128, 512]) for i in range(4)]
        n = [0]

        def ev2(j, t0, W, p):
            o = ob[n[0] % 4]; n[0] += 1
            if n[0] % 2:
                kb.op("act", lambda e: e.activation(out=o[:, 0:W], in_=p[:, 0:W], func=AF.Copy), r=[p], w=[o])
            else:
                kb.op("dve", lambda e: e.tensor_copy(out=o[:, 0:W], in_=p[:, 0:W]), r=[p], w=[o])
            kb.dma("sp", YT[j * 128:(j + 1) * 128, t0:t0 + W], o[:, 0:W], r=[o], store=True)
        gemm_fm(kb, wd, D // 128, DE, HT, R, tiles, ev2, dbuf=True)
    return kb.finish()


def build_p5(D=4096, TC=256, TL=4096, NB=2):
    kb = KB()
    NT = TC + TL
    NTB = NB * NT
    NCK = NT // 128
    CKC = TC // 128
    NCH = 13
    I = lambda name, shape, dt=F32: kb.dram(name, shape, dt, kind="ExternalInput")
    hT = I("hT", [D, NTB], BF16); win = I("win", [NCH, D, 128]); cst_d = I("cst", [128, 20, 128])
    gb_d = I("gbias", [1, 4]); nw_d = I("nw", [1, 1024])
    Y = kb.dram("Y", [NB * TL, 512], kind="ExternalOutput")
    P = kb.dram("P5P", [NCH, 128, NTB]); PB = kb.dram("P5PB", [4, 128, NTB], BF16)
    KTM = kb.dram("KTM", [NB * NCK, 128, 256], BF16); VTM = kb.dram("VTM", [NB * NCK, 128, 512], BF16)
    OTM = kb.dram("OTM", [NB * NCK, 128, 512]); HF = kb.dram("HFs", [NB * NCK, 128, 512])
    tiles = []
    for b in range(NB):
        tiles += [(b * NT + t0, W, lat) for (t0, W, lat) in seq_tiles(TC, TL, 512)]
    with kb.scope():
        obs = [kb.sb("ob%d" % i, [128, 512]) for i in range(4)]
        obb = [kb.sb("obb%d" % i, [128, 512], BF16) for i in range(2)]
        n = [0]

        def ev(j, t0, W, p):
            o = obs[n[0] % 4]; n[0] += 1
            kb.op("act" if n[0] % 2 else "dve", (lambda e: e.activation(out=o[:, 0:W], in_=p[:, 0:W], func=AF.Copy)) if n[0] % 2 else (lambda e: e.tensor_copy(out=o[:, 0:W], in_=p[:, 0:W])), r=[p], w=[o])
            kb.dma("sp", P[j][:, t0:t0 + W], o[:, 0:W], r=[o], store=True)
            if j < 4:
                ob_ = obb[n[0] % 2]
                kb.op("pool", lambda e: e.tensor_copy(out=ob_[:, 0:W], in_=o[:, 0:W]), r=[o], w=[ob_])
                kb.dma("sp", PB[j][:, t0:t0 + W], ob_[:, 0:W], r=[ob_], store=True)
        gemm_fm(kb, win, NCH, D, hT, NTB, tiles, ev, GS=7)
    cst = kb.sb("cst", [128, 20, 128]); kb.dma("sp", cst[:], cst_d[:], w=[cst])
    IDN, ML_I, MU_I = cst[:, 0, :], cst[:, 3, :], cst[:, 4, :]
    TRI = [MU_I, ML_I]
    capm = [kb.sb("cap%d" % d, [128, 128]) for d in range(2)]
    for d in range(2):
        kb.op("dve", lambda e: e.tensor_scalar(out=capm[d][:], in0=TRI[d], scalar1=2e30, scalar2=-1e30, op0=ALU.mult, op1=ALU.add), r=[cst], w=[capm[d]])
    ones = kb.sb("ones", [128, 128]); kb.op("pool", lambda e: e.memset(ones[:], 1.0), w=[ones])
    onesb = kb.sb("onesb", [128, 1], BF16); kb.op("pool", lambda e: e.memset(onesb[:], 1.0), w=[onesb])
    G = kb.sb("Gtm", [128, NB * NCK, 4])
    banks = [kb.ps("bank%d" % i, [128, 512]) for i in range(8)]
    bk = lambda i, a, b_: Tl(banks[i].t[:, a:b_], banks[i].key)
    with kb.scope():
        ld = [kb.sb("tld%d" % i, [128, 11, 128]) for i in range(2)]
        kt = [kb.sb("tkt%d" % i, [128, 256], BF16) for i in range(2)]
        vt = [kb.sb("tvt%d" % i, [128, 512], BF16) for i in range(2)]
        ot = [kb.sb("tot%d" % i, [128, 512]) for i in range(2)]
        for cg in range(NB * NCK):
            l = ld[cg % 2]; k_ = kt[cg % 2]; v_ = vt[cg % 2]; o_ = ot[cg % 2]
            is_lat = (cg % NCK) >= CKC
            kb.dma("sp", l[:], P[2:13].rearrange("j p t -> p j t")[:, :, cg * 128:(cg + 1) * 128], w=[l])
            pk = bk(0, 0, 256); pv = banks[1]; po = banks[2]; pg = bk(3, 0, 128)

            def tr(e, dst, j0, nj):
                ins = None
                for q in range(nj):
                    ins = e.transpose(dst[:, q * 128:(q + 1) * 128], l[:, j0 + q, :], IDN)
                return ins
            kb.op("pe", lambda e: tr(e, pk, 0, 2), r=[l, cst], w=[pk])
            kb.op("act", lambda e: e.activation(out=k_[:], in_=pk[:], func=AF.Copy), r=[pk], w=[k_])
            kb.dma("sp", KTM[cg], k_[:], r=[k_], store=True)
            kb.op("pe", lambda e: tr(e, pv, 2, 4), r=[l, cst], w=[pv])
            kb.op("dve", lambda e: e.tensor_copy(out=v_[:], in_=pv[:]), r=[pv], w=[v_])
            kb.dma("sp", VTM[cg], v_[:], r=[v_], store=True)
            if is_lat:
                kb.op("pe", lambda e: tr(e, po, 6, 4), r=[l, cst], w=[po])
                kb.op("act", lambda e: e.activation(out=o_[:], in_=po[:], func=AF.Sigmoid), r=[po], w=[o_])
                kb.dma("sp", OTM[cg], o_[:], r=[o_], store=True)
            kb.op("pe", lambda e: tr(e, pg, 10, 1), r=[l, cst], w=[pg])
            kb.op("dve", lambda e: e.tensor_copy(out=G[:, cg, :], in_=pg[:, 0:4]), r=[pg], w=[G])
    gbb = kb.sb("gbb", [128, 4]); kb.dma("sp", gbb[:], gb_d[:].partition_broadcast(128), w=[gbb])
    SC = kb.sb("SC", [128, NB * NCK, 4]); NLF = kb.sb("NLF", [128, NB * NCK, 4])
    kb.op("dve", lambda e: e.tensor_tensor(out=SC[:], in0=G[:], in1=gbb[:].unsqueeze(1).to_broadcast([128, NB * NCK, 4]), op=ALU.add), r=[G, gbb], w=[SC])
    kb.op("act", lambda e: e.activation(out=SC[:], in_=SC[:], func=AF.Tanh, scale=1.0 / 15.0), r=[SC], w=[SC])
    kb.op("dve", lambda e: e.tensor_scalar_mul(out=SC[:], in0=SC[:], scalar1=15.0), r=[SC], w=[SC])
    kb.op("act", lambda e: e.activation(out=NLF[:], in_=SC[:], func=AF.Exp, scale=-1.0), r=[SC], w=[NLF])
    kb.op("act", lambda e: e.activation(out=NLF[:], in_=NLF[:], func=AF.Ln, bias=1.0), r=[NLF], w=[NLF])
    nwb = kb.sb("nwb", [128, 1024]); kb.dma("sp", nwb[:], nw_d[:].partition_broadcast(128), w=[nwb])
    kb.barrier()
    C = kb.sb("Cst", [128, 2, 512]); Cb = kb.sb("Cbf", [128, 2, 512], BF16); nst = kb.sb("nst", [128, 2]); nb16 = kb.sb("nb16", [128, 2], BF16)
    qk = [kb.sb("qk%d" % i, [128, 4, 128], BF16) for i in range(2)]
    ktm = [kb.sb("ktm%d" % i, [128, 256], BF16) for i in range(2)]
    vtm = [kb.sb("vtm%d" % i, [128, 512], BF16) for i in range(2)]
    kw = kb.sb("kw", [128, 256], BF16)
    rows = kb.sb("rows", [1, 256]); cols = kb.sb("cols", [128, 2]); ew = kb.sb("ew", [128, 1]); eb = kb.sb("eb", [128, 1]); ebL = kb.sb("ebL", [128, 1])
    Dm = kb.sb("Dm", [128, 128]); SD = kb.sb("SD", [128, 128], BF16)
    tq = kb.sb("tq", [128, 512]); Hc = kb.sb("Hc", [128, 512]); hf = kb.sb("hfl", [128, 512]); ol = kb.sb("ol", [128, 512]); sqj = kb.sb("sqj", [128, 512])
    dn = kb.sb("dn", [128, 2]); st = kb.sb("stt", [128, 4])
    pR = bk(0, 0, 256); pCo = bk(0, 256, 258); pD = bk(1, 0, 128); pSc = bk(1, 128, 256)
    pN = banks[2]; pQ = banks[3]; pDen = bk(4, 0, 2); pNn = bk(4, 2, 4); pC0 = banks[5]; pC1 = banks[6]
    order = [list(range(NCK)), list(range(CKC - 1, -1, -1)) + list(range(NCK - 1, CKC - 1, -1))]
    it = 0
    for b in range(NB):
        for d in range(2):
            kb.op("pool", lambda e: e.memset(C[:], 0.0), w=[C]); kb.op("pool", lambda e: e.memset(Cb[:], 0.0), w=[Cb])
            kb.op("pool", lambda e: e.memset(nst[:], 0.0), w=[nst]); kb.op("pool", lambda e: e.memset(nb16[:], 0.0), w=[nb16])
            for c in order[d]:
                cg = b * NCK + c
                lat = c >= CKC
                q_ = qk[it % 2]; k_ = ktm[it % 2]; v_ = vtm[it % 2]; it += 1
                kb.dma("sp", q_[:], PB[:].rearrange("j p t -> p j t")[:, :, cg * 128:(cg + 1) * 128], w=[q_])
                kb.dma("act", k_[:], KTM[cg], w=[k_]); kb.dma("sp", v_[:], VTM[cg], w=[v_])
                igc = SC[:, cg, 2 * d:2 * d + 1]; nlf = NLF[:, cg, 2 * d + 1:2 * d + 2]

                def small(e):
                    e.matmul(pR[0:1, 0:128], lhsT=nlf, rhs=TRI[d], start=True, stop=True)
                    e.matmul(pR[0:1, 128:256], lhsT=igc, rhs=IDN, start=True, stop=False)
                    e.matmul(pR[0:1, 128:256], lhsT=nlf, rhs=TRI[d], start=False, stop=True)
                    e.matmul(pCo[:, 0:1], lhsT=TRI[d], rhs=nlf, start=True, stop=True)
                    return e.matmul(pCo[:, 1:2], lhsT=ones[:], rhs=nlf, start=True, stop=True)
                kb.op("pe", small, r=[NLF, SC, cst, ones], w=[pR])
                kb.op("act", lambda e: e.activation(out=rows[0:1, 0:128], in_=pR[0:1, 0:128], func=AF.Copy, scale=-1.0), r=[pR], w=[rows])
                kb.op("act", lambda e: e.activation(out=rows[0:1, 128:256], in_=pR[0:1, 128:256], func=AF.Copy), r=[pR], w=[rows])
                kb.op("dve", lambda e: e.tensor_copy(out=cols[:], in_=pCo[:, 0:2]), r=[pR], w=[cols])
                kb.op("dve", lambda e: e.scalar_tensor_tensor(out=ew[:], in0=cols[:, 0:1], scalar=igc, in1=cols[:, 1:2], op0=ALU.add, op1=ALU.subtract), r=[cols, SC], w=[ew])
                kb.op("act", lambda e: e.activation(out=ew[:], in_=ew[:], func=AF.Exp), r=[ew], w=[ew])
                kb.op("act", lambda e: e.activation(out=ebL[:], in_=cols[:, 1:2], func=AF.Exp, scale=-1.0), r=[cols], w=[ebL])
                if lat:
                    kb.op("act", lambda e: e.activation(out=eb[:], in_=cols[:, 0:1], func=AF.Exp, scale=-1.0), r=[cols], w=[eb])
                    kb.op("dve", lambda e: e.tensor_scalar_mul(out=eb[:], in0=eb[:], scalar1=1.0 / 16.0), r=[eb], w=[eb])

                    def dlog(e):
                        e.matmul(pD[:], lhsT=ones[0:1, :], rhs=rows[0:1, 0:128], start=True, stop=False)
                        return e.matmul(pD[:], lhsT=rows[0:1, 128:256], rhs=ones[0:1, :], start=False, stop=True)
                    kb.op("pe", dlog, r=[rows, ones], w=[pD])
                    kb.op("dve", lambda e: e.tensor_tensor(out=Dm[:], in0=pD[:], in1=capm[d][:], op=ALU.min), r=[pD, capm[d]], w=[Dm])
                    kb.op("act", lambda e: e.activation(out=Dm[:], in_=Dm[:], func=AF.Exp), r=[Dm], w=[Dm])

                    def scm(e):
                        e.matmul(pSc[:], lhsT=q_[:, 2, :], rhs=q_[:, 0, :], start=True, stop=False)
                        return e.matmul(pSc[:], lhsT=q_[:, 3, :], rhs=q_[:, 1, :], start=False, stop=True)
                    kb.op("pe", scm, r=[q_], w=[pD])
                    kb.op("dve", lambda e: e.scalar_tensor_tensor(out=SD[:], in0=pSc[:], scalar=1.0 / 16.0, in1=Dm[:], op0=ALU.mult, op1=ALU.mult), r=[pD, Dm], w=[SD])
                    kb.op("pe", lambda e: e.matmul(pN[:], lhsT=SD[:], rhs=v_[:], start=True, stop=True), r=[SD, v_], w=[pN])

                    def qc(e):
                        e.matmul(pQ[:], lhsT=q_[:, 0, :], rhs=Cb[:, 0, :], start=True, stop=False)
                        return e.matmul(pQ[:], lhsT=q_[:, 1, :], rhs=Cb[:, 1, :], start=False, stop=True)
                    kb.op("pe", qc, r=[q_, Cb], w=[pQ])

                    def den(e):
                        e.matmul(pDen[:, 0:1], lhsT=SD[:], rhs=onesb[:], start=True, stop=True)
                        e.matmul(pDen[:, 1:2], lhsT=q_[:, 0, :], rhs=nb16[:, 0:1], start=True, stop=False)
                        return e.matmul(pDen[:, 1:2], lhsT=q_[:, 1, :], rhs=nb16[:, 1:2], start=False, stop=True)
                    kb.op("pe", den, r=[SD, onesb, q_, nb16], w=[pDen])
                    kb.op("act", lambda e: e.activation(out=tq[:], in_=pQ[:], func=AF.Identity, scale=eb[:, 0:1]), r=[pQ, eb], w=[tq])
                    kb.op("dve", lambda e: e.tensor_tensor(out=Hc[:], in0=pN[:], in1=tq[:], op=ALU.add), r=[pN, tq], w=[Hc])
                    kb.op("dve", lambda e: e.tensor_copy(out=dn[:], in_=pDen[:, 0:2]), r=[pDen], w=[dn])
                    kb.op("dve", lambda e: e.scalar_tensor_tensor(out=dn[:, 0:1], in0=dn[:, 1:2], scalar=eb[:, 0:1], in1=dn[:, 0:1], op0=ALU.mult, op1=ALU.add), r=[dn, eb], w=[dn])
                    kb.op("dve", lambda e: e.tensor_scalar_mul(out=dn[:, 1:2], in0=dn[:, 0:1], scalar1=-1.0), r=[dn], w=[dn])
                    kb.op("dve", lambda e: e.tensor_tensor(out=dn[:, 0:1], in0=dn[:, 0:1], in1=dn[:, 1:2], op=ALU.max), r=[dn], w=[dn])
                    kb.op("dve", lambda e: e.tensor_scalar_max(out=dn[:, 0:1], in0=dn[:, 0:1], scalar1=1.0), r=[dn], w=[dn])
                    kb.op("dve", lambda e: e.reciprocal(out=dn[:, 0:1], in_=dn[:, 0:1]), r=[dn], w=[dn])
                    kb.op("dve", lambda e: e.tensor_scalar_mul(out=Hc[:], in0=Hc[:], scalar1=dn[:, 0:1]), r=[Hc, dn], w=[Hc])
                    if d == 0:
                        kb.dma("sp", HF[cg], Hc[:], r=[Hc], store=True)
                    else:
                        kb.dma("sp", hf[:], HF[cg], w=[hf]); kb.dma("act", ol[:], OTM[cg], w=[ol])
                        kb.op("dve", lambda e: e.tensor_tensor(out=Hc[:], in0=Hc[:], in1=hf[:], op=ALU.add), r=[Hc, hf], w=[Hc])
                        kb.op("dve", lambda e: e.tensor_reduce(out=st[:, 0:1], in_=Hc[:], axis=AX.X, op=ALU.add), r=[Hc], w=[st])
                        kb.op("dve", lambda e: e.tensor_scalar_mul(out=st[:, 0:1], in0=st[:, 0:1], scalar1=1.0 / 512), r=[st], w=[st])
                        kb.op("dve", lambda e: e.tensor_scalar_sub(out=Hc[:], in0=Hc[:], scalar1=st[:, 0:1]), r=[Hc, st], w=[Hc])
                        kb.op("dve", lambda e: e.tensor_tensor(out=sqj[:], in0=Hc[:], in1=Hc[:], op=ALU.mult), r=[Hc], w=[sqj])
                        kb.op("dve", lambda e: e.tensor_reduce(out=st[:, 1:2], in_=sqj[:], axis=AX.X, op=ALU.add), r=[sqj], w=[st])
                        kb.op("dve", lambda e: e.tensor_scalar(out=st[:, 1:2], in0=st[:, 1:2], scalar1=1.0 / 512, scalar2=1e-6, op0=ALU.mult, op1=ALU.add), r=[st], w=[st])
                        kb.op("act", lambda e: e.activation(out=st[:, 1:2], in_=st[:, 1:2], func=AF.Sqrt), r=[st], w=[st])
                        kb.op("dve", lambda e: e.reciprocal(out=st[:, 1:2], in_=st[:, 1:2]), r=[st], w=[st])
                        kb.op("dve", lambda e: e.scalar_tensor_tensor(out=Hc[:], in0=Hc[:], scalar=st[:, 1:2], in1=nwb[:, 0:512], op0=ALU.mult, op1=ALU.mult), r=[Hc, st, nwb], w=[Hc])
                        kb.op("dve", lambda e: e.tensor_tensor(out=Hc[:], in0=Hc[:], in1=nwb[:, 512:1024], op=ALU.add), r=[Hc, nwb], w=[Hc])
                        kb.op("dve", lambda e: e.tensor_tensor(out=Hc[:], in0=Hc[:], in1=ol[:], op=ALU.mult), r=[Hc, ol], w=[Hc])
                        r0 = b * TL + (c - CKC) * 128
                        kb.dma("sp", Y[r0:r0 + 128, :], Hc[:], r=[Hc], store=True)
                kb.op("dve", lambda e: e.tensor_scalar_mul(out=kw[:], in0=k_[:], scalar1=ew[:, 0:1]), r=[k_, ew], w=[kw])
                kb.op("pe", lambda e: e.matmul(pC0[:], lhsT=kw[:, 0:128], rhs=v_[:], start=True, stop=True), r=[kw, v_], w=[pC0])
                kb.op("pe", lambda e: e.matmul(pC1[:], lhsT=kw[:, 128:256], rhs=v_[:], start=True, stop=True), r=[kw, v_], w=[pC1])

                def nmm(e):
                    e.matmul(pNn[:, 0:1], lhsT=kw[:, 0:128], rhs=onesb[:], start=True, stop=True)
                    return e.matmul(pNn[:, 1:2], lhsT=kw[:, 128:256], rhs=onesb[:], start=True, stop=True)
                kb.op("pe", nmm, r=[kw, onesb], w=[pDen])
                kb.op("dve", lambda e: e.scalar_tensor_tensor(out=C[:, 0, :], in0=C[:, 0, :], scalar=ebL[:, 0:1], in1=pC0[:], op0=ALU.mult, op1=ALU.add), r=[C, ebL, pC0], w=[C])
                kb.op("dve", lambda e: e.scalar_tensor_tensor(out=C[:, 1, :], in0=C[:, 1, :], scalar=ebL[:, 0:1], in1=pC1[:], op0=ALU.mult, op1=ALU.add), r=[C, ebL, pC1], w=[C])
                kb.op("act", lambda e: e.activation(out=Cb[:], in_=C[:], func=AF.Copy), r=[C], w=[Cb])
                kb.op("dve", lambda e: e.scalar_tensor_tensor(out=nst[:], in0=nst[:], scalar=ebL[:, 0:1], in1=pNn[:, 0:2], op0=ALU.mult, op1=ALU.add), r=[nst, ebL, pDen], w=[nst])
                kb.op("dve", lambda e: e.tensor_copy(out=nb16[:], in_=nst[:]), r=[nst], w=[nb16])
            kb.barrier()
    return kb.finish()


def p5_core_inputs(hd, hT_full, prm):
    w = prm["w_in"]; D = w.shape[0]
    H = prm["ig_b"].shape[1]
    QW = H * 256; VW = H * 512; QO = QW + VW
    cols = []
    for q in range(2): cols.append(np.arange(hd * 256 + q * 128, hd * 256 + (q + 1) * 128))
    for q in range(2): cols.append(np.arange(QO + hd * 256 + q * 128, QO + hd * 256 + (q + 1) * 128))
    for q in range(4): cols.append(np.arange(QO + QW + hd * 512 + q * 128, QO + QW + hd * 512 + (q + 1) * 128))
    for q in range(4): cols.append(np.arange(QW + hd * 512 + q * 128, QW + hd * 512 + (q + 1) * 128))
    gbase = QO + QW + VW
    cols.append(np.array([gbase + d * 2 * H + io * H + hd for d in range(2) for io in range(2)]))
    win = np.zeros((13, D, 128), np.float32)
    for j, cc in enumerate(cols):
        win[j, :, :len(cc)] = w[:, cc]
    gb = np.array([[prm["ig_b"][0, hd], prm["fg_b"][0, hd], prm["ig_b"][1, hd], prm["fg_b"][1, hd]]], np.float32)
    nw = np.concatenate([prm["norm_w"][hd * 512:(hd + 1) * 512], prm["norm_b"][hd * 512:(hd + 1) * 512]])[None].astype(np.float32)
    return {"hT": hT_full, "win": win, "cst": make_consts(), "gbias": gb, "nw": nw}


_HOOK = None


def _hook(name, arr):
    if _HOOK is not None:
        _HOOK(name, arr)


def run_p1(x, ctx, mods0, prm):
    B, TL, D = x.shape
    TC = ctx.shape[1]
    AW = prm["w0"].shape[-1]; BW = prm["lam"].shape[-1]
    G = 8 // B
    NBLK = AW // 128 // G; NLB = BW // 128 // G
    nc = build_p1(D=D, TC=TC, TL=TL, NBLK=NBLK, NLB=NLB)
    cores = [(b, g) for b in range(B) for g in range(G)]
    maps = [p1_core_inputs(b, g, x, ctx, mods0, prm, NBLK, NLB) for (b, g) in cores]
    res = _run(nc, maps)
    y = np.zeros((B, TC + TL, AW + BW), np.float32)
    for i, (b, g) in enumerate(cores):
        y[b, :, g * NBLK * 128:(g + 1) * NBLK * 128] = res[i]["ya"]
        y[b, :, AW + g * NLB * 128:AW + (g + 1) * NLB * 128] = res[i]["yb"].T
    return y


def _modp(vsets, KC):
    return np.ascontiguousarray(np.stack([np.stack([_pm(v, KC) for v in vs], -1) for vs in vsets], 2).astype(np.float32))


def _chunk_cols(w):
    K_, N = w.shape
    return np.ascontiguousarray(w.reshape(K_, N // 128, 128).transpose(1, 0, 2))


def moe_capacity(G_all):
    cnt = int((G_all > 0).sum(0).max())
    return int(min(3072, max(1024, -(-cnt // 512) * 512)))


def run_moe(ffn_nc, HT_all, G_all, wg, wu, wd, R):
    D, N = HT_all.shape
    ex = np.argsort(-G_all, axis=1, kind="stable")[:, :2]
    ex.sort(axis=1)
    gv = np.take_along_axis(G_all, ex, 1).astype(np.float32)
    items = []
    for e in range(G_all.shape[1]):
        idx = np.nonzero((ex == e).any(1))[0]
        for s0 in range(0, len(idx), R):
            items.append((e, idx[s0:s0 + R]))
    y1T = np.zeros((D, N), np.float32); y2T = np.zeros((D, N), np.float32)
    cache = {}

    def wl(e):
        if e not in cache:
            cache.clear()
            a = np.empty((2 * wg.shape[2] // 128, D, 128), np.float32)
            a[0::2] = _chunk_cols(wg[e]); a[1::2] = _chunk_cols(wu[e])
            cache[e] = (a, _chunk_cols(wd[e]))
        return cache[e]
    for l0 in range(0, len(items), 8):
        batch = items[l0:l0 + 8]
        full = batch + [(batch[0][0], np.zeros((0,), np.int64))] * (8 - len(batch))
        maps = []
        for (e, idx) in full:
            XT = np.zeros((D, R), HT_all.dtype)
            XT[:, :len(idx)] = HT_all[:, idx]
            a, b_ = wl(e)
            maps.append({"XT": XT, "wgu": a, "wd": b_})
        res = _run(ffn_nc, maps)
        for (e, idx), r in zip(batch, res):
            YT = r["YT"][:, :len(idx)]
            m0 = ex[idx, 0] == e
            y1T[:, idx[m0]] = YT[:, m0]
            y2T[:, idx[~m0]] = YT[:, ~m0]
    return y1T, y2T, np.ascontiguousarray(gv.T)


def kernel(x, c, ctx, c_ctx, ada_w, ada_b, norm_g, norm_b,
           ev_w_in, ev_w_out, rwkv_mu, rwkv_w0, rwkv_w2, rwkv_a0, rwkv_a2, rwkv_g2,
           rwkv_k_k, rwkv_k_a, rwkv_r_k, rwkv_lnx_w, rwkv_lnx_b,
           lru_conv_w, lru_conv_b, lru_wa, lru_ba, lru_wx, lru_bx, lru_lam,
           od_w_in, od_w_out, mlstm_ig_b, mlstm_fg_b, mlstm_norm_w, mlstm_norm_b,
           router_w, router_b, moe_w_gate, moe_w_up, moe_w_down):
    f = lambda a: np.asarray(a, dtype=np.float32)
    x = f(x); ctx = f(ctx)
    B, TL, D = x.shape
    TC = ctx.shape[1]
    KC = D // 128
    NQ = 8 // B
    LT = TL // NQ; CT = TC // NQ
    cores = [(b, q) for b in range(B) for q in range(NQ)]
    mods = run_p0(f(c), f(c_ctx), f(ada_w), f(ada_b))
    _hook("mods", mods)
    M = lambda l, v, k: mods[l, v, k * D:(k + 1) * D]
    prm = dict(w_in=f(ev_w_in[0]), mu=f(rwkv_mu[0]), w0=f(rwkv_w0[0]), w2=f(rwkv_w2[0]), a0=f(rwkv_a0[0]), a2=f(rwkv_a2[0]),
               g2=f(rwkv_g2[0]), k_k=f(rwkv_k_k[0]), k_a=f(rwkv_k_a[0]), r_k=f(rwkv_r_k[0]), lnx_w=f(rwkv_lnx_w[0]), lnx_b=f(rwkv_lnx_b[0]),
               conv_w=f(lru_conv_w[0]), conv_b=f(lru_conv_b[0]), wa=f(lru_wa[0]), ba=f(lru_ba[0]), wx=f(lru_wx[0]), bx=f(lru_bx[0]), lam=f(lru_lam[0]))
    y0 = run_p1(x, ctx, mods[0], prm)
    _hook("y0", y0)
    tab = sincos_tab(TL, D)
    rw_l = np.ascontiguousarray(f(router_w).reshape(KC, 128, 16).transpose(1, 0, 2)); rb_l = f(router_b)[None]
    cstc = make_consts()
    zrow = np.zeros((128, KC // 2, LT // 64), np.float32); zcol = np.zeros((128, KC // 2, 64), np.float32)
    ffn_cache = {}

    def get_ffn(R):
        if R not in ffn_cache:
            ffn_cache[R] = build_ffn(D=D, DE=moe_w_gate.shape[3], R=R)
        return ffn_cache[R]

    nc_a = build_post(D=D, KM=y0.shape[2], segs=((LT, 0), (CT, 1)), mode="proj", router=True, hnext=True)
    wout0 = _chunk_cols(f(ev_w_out[0]))
    lnp = lambda l, i: np.ascontiguousarray(np.stack([_pm(f(norm_g[l, i]), KC), _pm(f(norm_b[l, i]), KC)], -1))
    maps = []
    for (b, q) in cores:
        ls = slice(q * LT, (q + 1) * LT); cs_ = slice(q * CT, (q + 1) * CT)
        maps.append({"resT": np.ascontiguousarray(np.concatenate([x[b, ls], ctx[b, cs_]], 0).T),
                     "yT": np.ascontiguousarray(np.concatenate([y0[b, TC + q * LT:TC + (q + 1) * LT], y0[b, cs_]], 0).T),
                     "modp": _modp([(M(0, b, 2), M(0, b, 3), M(0, b, 4)), (M(0, 2, 2), M(0, 2, 3), M(0, 2, 4))], KC),
                     "lnp": lnp(0, 0), "wout": wout0, "rw": rw_l, "rb": rb_l, "cst": cstc,
                     "prow": np.ascontiguousarray(tab[:D // 2, q * (LT // 64):(q + 1) * (LT // 64)].reshape(KC // 2, 128, LT // 64).transpose(1, 0, 2)),
                     "pcol": np.ascontiguousarray(tab[D // 2:, :].reshape(KC // 2, 128, 64).transpose(1, 0, 2))})
    ra = _run(nc_a, maps)
    del maps, y0
    NTK = LT + CT
    resA = [r["res_out"] for r in ra]
    HT_all = np.concatenate([r["h_out"] for r in ra], 1)
    G_all = np.concatenate([r["G"][:NTK] for r in ra], 0)
    _hook("resA0", resA); _hook("G0", G_all)
    R_CAP = moe_capacity(G_all)
    y1T, y2T, g12 = run_moe(get_ffn(R_CAP), HT_all, G_all, f(moe_w_gate[0]), f(moe_w_up[0]), f(moe_w_down[0]), R_CAP)
    _hook("moe0", (y1T, y2T, g12))
    nc_b = build_post(D=D, segs=((LT, 0), (CT, 1)), mode="comb", router=False, hnext=True)
    maps = []
    for i, (b, q) in enumerate(cores):
        sl = slice(i * NTK, (i + 1) * NTK)
        maps.append({"resT": resA[i], "y1T": np.ascontiguousarray(y1T[:, sl]), "y2T": np.ascontiguousarray(y2T[:, sl]), "g12": np.ascontiguousarray(g12[:, sl]),
                     "modp": _modp([(M(0, b, 5), M(1, b, 0), M(1, b, 1)), (M(0, 2, 5), M(1, 2, 0), M(1, 2, 1))], KC),
                     "lnp": lnp(0, 1), "prow": zrow, "pcol": zcol})
    rb = _run(nc_b, maps)
    del maps, y1T, y2T
    resB = [r["res_out"] for r in rb]
    _hook("resB0", resB)
    NT = TC + TL
    hT_full = np.zeros((D, B * NT), rb[0]["h_out"].dtype)
    for i, (b, q) in enumerate(cores):
        h = rb[i]["h_out"]
        hT_full[:, b * NT + TC + q * LT:b * NT + TC + (q + 1) * LT] = h[:, 0:LT]
        hT_full[:, b * NT + q * CT:b * NT + (q + 1) * CT] = h[:, LT:LT + CT]
    prm5 = dict(w_in=f(od_w_in[0]), ig_b=f(mlstm_ig_b[0]), fg_b=f(mlstm_fg_b[0]), norm_w=f(mlstm_norm_w[0]), norm_b=f(mlstm_norm_b[0]))
    nc5 = build_p5(D=D, TC=TC, TL=TL, NB=B)
    r5 = _run(nc5, [p5_core_inputs(hd, hT_full, prm5) for hd in range(8)])
    y1 = np.zeros((B, TL, 8 * 512), np.float32)
    for hd in range(8):
        y1[:, :, hd * 512:(hd + 1) * 512] = r5[hd]["Y"].reshape(B, TL, 512)
    del hT_full, r5
    _hook("y1", y1)
    nc_c = build_post(D=D, KM=y1.shape[2], segs=((LT, 0),), mode="proj", router=True, hnext=True)
    wout1 = _chunk_cols(f(od_w_out[0]))
    maps = []
    for i, (b, q) in enumerate(cores):
        maps.append({"resT": np.ascontiguousarray(resB[i][:, 0:LT]), "yT": np.ascontiguousarray(y1[b, q * LT:(q + 1) * LT].T),
                     "modp": _modp([(M(1, b, 2), M(1, b, 3), M(1, b, 4))], KC), "lnp": lnp(1, 0), "wout": wout1,
                     "rw": rw_l, "rb": rb_l, "cst": cstc, "prow": zrow, "pcol": zcol})
    rc = _run(nc_c, maps)
    del maps, y1
    resC = [r["res_out"] for r in rc]
    HT_all = np.concatenate([r["h_out"] for r in rc], 1)
    G_all = np.concatenate([r["G"][:LT] for r in rc], 0)
    _hook("resC1", resC)
    R_CAP = moe_capacity(G_all)
    y1T, y2T, g12 = run_moe(get_ffn(R_CAP), HT_all, G_all, f(moe_w_gate[1]), f(moe_w_up[1]), f(moe_w_down[1]), R_CAP)
    nc_d = build_post(D=D, segs=((LT, 0),), mode="comb", router=False, hnext=False)
    maps = []
    for i, (b, q) in enumerate(cores):
        sl = slice(i * LT, (i + 1) * LT)
        maps.append({"resT": resC[i], "y1T": np.ascontiguousarray(y1T[:, sl]), "y2T": np.ascontiguousarray(y2T[:, sl]), "g12": np.ascontiguousarray(g12[:, sl]),
                     "modp": _modp([(M(1, b, 5), M(1, b, 5), M(1, b, 5))], KC), "lnp": lnp(1, 1), "prow": zrow, "pcol": zcol})
    rd = _run(nc_d, maps)
    out = np.zeros((B, TL, D), np.float32)
    for i, (b, q) in enumerate(cores):
        out[b, q * LT:(q + 1) * LT] = rd[i]["res_out"].T
    return out
```

```python
import contextlib
import numpy as np
import concourse.bass as bass
import concourse.mybir as mybir
from concourse.bass_utils import run_bass_kernel_spmd

F32 = mybir.dt.float32
BF16 = mybir.dt.bfloat16
AF = mybir.ActivationFunctionType
ALU = mybir.AluOpType
AX = mybir.AxisListType


class Tl:
    __slots__ = ("t", "key")

    def __init__(self, t, key):
        self.t = t
        self.key = key

    def __getitem__(self, idx):
        return self.t[idx]


class KB:
    ENG = ("pe", "act", "dve", "pool", "sp")

    def __init__(self):
        self.nc = bass.Bass("TRN2", target_bir_lowering=False)
        self.es = contextlib.ExitStack()
        self.es.enter_context(self.nc.allow_low_precision("bf16 matmul operands, fp32 accumulation"))
        nc = self.nc
        self.eng = {"pe": nc.tensor, "act": nc.scalar, "dve": nc.vector, "pool": nc.gpsimd, "sp": nc.sync}
        self.sem = {}
        self.cnt = {}
        for e in self.ENG:
            self.sem[e] = self.es.enter_context(nc.semaphore("s_" + e))
            self.cnt[e] = 0
        self.seen = {e: {} for e in self.ENG}
        self.dep = {}
        self.dsem = {}
        self.semobj = {}
        self.nuniq = 0
        self.stores = []
        self.free_dsem = []
        self.es_perm = self.es

    def sb(self, name, shape, dtype=F32):
        self.nalloc = getattr(self, "nalloc", 0) + 1
        name = "sb%d_%s" % (self.nalloc, name)
        t = self.es.enter_context(self.nc.sbuf_tensor(name, list(shape), dtype))
        return Tl(t, name)

    def ps(self, name, shape, dtype=F32):
        self.nalloc = getattr(self, "nalloc", 0) + 1
        name = "ps%d_%s" % (self.nalloc, name)
        t = self.es.enter_context(self.nc.psum_tensor(name, list(shape), dtype))
        return Tl(t, name)

    def dram(self, name, shape, dtype=F32, kind="Internal"):
        t = self.nc.dram_tensor(name, list(shape), dtype, kind=kind)
        return Tl(t.ap(), name)

    def _needs(self, e, r, w):
        waits = {}

        def need(tok, same_ok):
            if tok is None:
                return
            sname, val = tok
            if same_ok and sname == "s_" + e:
                return
            if self.seen[e].get(sname, 0) >= val:
                return
            if waits.get(sname, 0) < val:
                waits[sname] = val

        for t in r:
            d = self.dep.get(t.key)
            if d:
                need(d["w"], False)
        for t in w:
            d = self.dep.get(t.key)
            if d:
                need(d["w"], False)
                for sname, val in d["r"].items():
                    need((sname, val), False)
        for sname, val in waits.items():
            self.eng[e].wait_ge(self.semobj[sname], val)
            self.seen[e][sname] = val

    def _commit(self, tok, r, w):
        for t in r:
            d = self.dep.setdefault(t.key, {"w": None, "r": {}})
            if d["r"].get(tok[0], 0) < tok[1]:
                d["r"][tok[0]] = tok[1]
        for t in w:
            self.dep[t.key] = {"w": tok, "r": {}}

    def op(self, e, fn, r=(), w=()):
        self.semobj.setdefault("s_" + e, self.sem[e])
        self._needs(e, r, w)
        ins = fn(self.eng[e])
        self.cnt[e] += 1
        ins.then_inc(self.sem[e], 1)
        self._commit(("s_" + e, self.cnt[e]), r, w)

    def dma(self, q, out, in_, r=(), w=(), semkey=None, store=False):
        if semkey is None:
            semkey = (r[0].key if store else w[0].key)
        if semkey not in self.dsem:
            if self.free_dsem:
                self.dsem[semkey] = self.free_dsem.pop()
            else:
                nm = "d%d" % self.nuniq
                self.nuniq += 1
                s = self.es_perm.enter_context(self.nc.semaphore(nm))
                self.dsem[semkey] = [nm, 0]
                self.semobj[nm] = s
        ent = self.dsem[semkey]
        self._needs(q, r, w)
        pairs = out if isinstance(out, list) else [(out, in_)]
        for o, i in pairs:
            self.eng[q].dma_start(out=o, in_=i).then_inc(self.semobj[ent[0]], 16)
            ent[1] += 16
        tok = (ent[0], ent[1])
        self._commit(tok, r, w)
        if store:
            self.stores.append(tok)

    def dump(self, tl, name):
        shp = list(tl.t.shape)
        d = self.dram(name, shp, tl.t.dtype, kind="ExternalOutput")
        self.dma("sp", d[:], tl[:], r=[tl], store=True)

    def barrier(self):
        for e in self.ENG:
            for e2 in self.ENG:
                if e2 != e and self.cnt[e2] > self.seen[e].get("s_" + e2, 0):
                    self.semobj.setdefault("s_" + e2, self.sem[e2])
                    self.eng[e].wait_ge(self.sem[e2], self.cnt[e2])
                    self.seen[e]["s_" + e2] = self.cnt[e2]
            for key, (nm, val) in self.dsem.items():
                if val > self.seen[e].get(nm, 0):
                    self.eng[e].wait_ge(self.semobj[nm], val)
                    self.seen[e][nm] = val
        self.dep = {}

    @contextlib.contextmanager
    def scope(self):
        saved = self.es
        self.es = contextlib.ExitStack()
        keys_before = set(self.dsem.keys())
        try:
            yield
        finally:
            self.barrier()
            for k in list(self.dsem.keys()):
                if k not in keys_before:
                    self.free_dsem.append(self.dsem.pop(k))
            self.es.close()
            self.es = saved

    def finish(self):
        last = {}
        for sname, val in self.stores:
            last[sname] = max(last.get(sname, 0), val)
        for sname, val in last.items():
            self.eng["sp"].wait_ge(self.semobj[sname], val)
        self.es.close()
        return self.nc


_TRACE = False
_TIMES = []


def _run(nc, in_maps):
    if _TRACE:
        res = run_bass_kernel_spmd(nc, in_maps, core_ids=list(range(len(in_maps))), trace=True)
        _TIMES.append(res.exec_time_ns)
    else:
        res = run_bass_kernel_spmd(nc, in_maps, core_ids=list(range(len(in_maps))))
    return res.results


def build_p0(D=4096, NCOL=3072, NV=3, CW=512):
    kb = KB()
    nc = kb.nc
    KC = D // 128
    cvT = kb.dram("cvT", [128, KC, NV], kind="ExternalInput")
    W = kb.dram("w", [D, NCOL], kind="ExternalInput")
    bias = kb.dram("b", [1, NCOL], kind="ExternalInput")
    out = kb.dram("out", [NV, NCOL], kind="ExternalOutput")
    cv = kb.sb("cv", [128, KC, NV])
    cs = kb.sb("cs", [128, KC, NV])
    bt = kb.sb("bt", [NV, NCOL])
    ot = kb.sb("ot", [NV, NCOL])
    NB = 3
    wt = [kb.sb("wt%d" % i, [128, 8, CW]) for i in range(NB)]
    pss = [kb.ps("ps%d" % i, [128, CW]) for i in range(2)]
    kb.dma("sp", cv[:], cvT[:], w=[cv])
    kb.dma("sp", [(bt[v:v + 1, :], bias[:]) for v in range(NV)], None, w=[bt])
    kb.op("act", lambda e: e.activation(out=cs[:], in_=cv[:], func=AF.Silu), r=[cv], w=[cs])
    Wv = W[:].rearrange("(c p) n -> p c n", p=128)
    nblk = NCOL // CW
    li = 0
    for j in range(nblk):
        p = pss[j % 2]
        for c8 in range(KC // 8):
            wb = wt[li % NB]
            li += 1
            kb.dma("sp" if li % 2 else "pool", wb[:], Wv[:, c8 * 8:(c8 + 1) * 8, j * CW:(j + 1) * CW], w=[wb])

            def mm(e, c8=c8, wb=wb, p=p):
                ins = None
                for cc in range(8):
                    c = c8 * 8 + cc
                    ins = e.matmul(p[0:NV, :], lhsT=cs[:, c, :], rhs=wb[:, cc, :], start=(c == 0), stop=(c == KC - 1))
                return ins
            kb.op("pe", mm, r=[cs, wb], w=[p])
        kb.op("dve", lambda e, p=p, j=j: e.tensor_tensor(out=ot[:, j * CW:(j + 1) * CW], in0=p[0:NV, :],
                                                        in1=bt[:, j * CW:(j + 1) * CW], op=ALU.add),
              r=[p, bt], w=[ot])
    kb.dma("sp", out[:], ot[:], r=[ot], store=True)
    return kb.finish()


def run_p0(c, c_ctx, ada_w, ada_b):
    D = c.shape[1]
    L = ada_w.shape[0]
    ncol = ada_w.shape[2]
    cvs = np.concatenate([c, c_ctx[None, :]], 0).astype(np.float32)
    cvT = np.ascontiguousarray(cvs.T.reshape(D // 128, 128, 3).transpose(1, 0, 2))
    per = ncol * L // 8
    nc = build_p0(D=D, NCOL=per)
    maps = []
    for j in range(8):
        l = (j * per) // ncol
        c0 = (j * per) % ncol
        maps.append({"cvT": cvT, "w": np.ascontiguousarray(ada_w[l][:, c0:c0 + per]),
                     "b": np.ascontiguousarray(ada_b[l][None, c0:c0 + per])})
    res = _run(nc, maps)
    flat = np.concatenate([r["out"] for r in res], axis=1)
    return flat.reshape(3, L, ncol).transpose(1, 0, 2)


def seq_tiles(TC, TL, W):
    out = []
    for t0 in range(0, TC, W):
        out.append((t0, min(W, TC - t0), False))
    for t0 in range(0, TL, W):
        out.append((TC + t0, min(W, TL - t0), True))
    return out


def make_consts():
    c = np.zeros((128, 20, 128), np.float32)
    i = np.arange(128)
    c[:, 0, :] = np.eye(128)
    c[:, 1, :] = (i[:, None] > i[None, :])
    c[:, 2, :] = (i[:, None] < i[None, :])
    c[:, 3, :] = (i[:, None] >= i[None, :])
    c[:, 4, :] = (i[:, None] <= i[None, :])
    c[:, 5, :] = ((i[:, None] // 64) == (i[None, :] // 64))
    for l in range(7):
        t, s_ = i[:, None], i[None, :]
        m = ((t >> (l + 1)) == (s_ >> (l + 1))) & (((t >> l) & 1) == 1) & (((s_ >> l) & 1) == 0)
        c[:, 6 + l, :] = m
        c[:, 13 + l, :] = m.T
    return c


def make_consts2():
    c = make_consts()
    o = np.zeros((128, 15, 2, 128), np.float32)
    for l in range(7):
        o[:, l, :, :] = c[:, 6 + l, None, :]
        o[:, 7 + l, :, :] = c[:, 13 + l, None, :]
    o[:, 14, :, :] = c[:, 0, None, :]
    return o


def gemm_fm(kb, Wd, NCH, K, XT, NT, tiles, evac, GS=4, xdt=BF16, dbuf=False):
    KC = K // 128
    TW = max(w for _, w, _ in tiles)
    NS = 2 if dbuf else 1
    with kb.scope():
        wst = [kb.sb("g_wst%d" % i, [128, KC, 128]) for i in range(2)]
        wb = [[kb.sb("g_wb%d_%d" % (s_, i), [128, KC, 128], BF16) for i in range(GS)] for s_ in range(NS)]
        xt = [kb.sb("g_xt%d" % i, [128, KC, TW], xdt) for i in range(2)]
        pss = [kb.ps("g_ps%d" % i, [128, 512]) for i in range(GS)]
        XTv = XT[:].rearrange("(c p) t -> p c t", p=128)
        groups = [list(range(g0, min(NCH, g0 + GS))) for g0 in range(0, NCH, GS)]
        li = [0]

        def load_group(gi):
            for jj, j in enumerate(groups[gi]):
                st = wst[li[0] % 2]
                li[0] += 1
                kb.dma("sp", st[:], Wd[j].rearrange("(c p) n -> p c n", p=128), w=[st])
                dst = wb[gi % NS][jj]
                kb.op("pool", lambda e: e.tensor_copy(out=dst[:], in_=st[:]), r=[st], w=[dst])
        xi = 0
        load_group(0)
        for gi, js in enumerate(groups):
            if dbuf and gi + 1 < len(groups):
                load_group(gi + 1)
            wset = wb[gi % NS]
            for (t0, W, _) in tiles:
                x = xt[xi % 2]
                xi += 1
                kb.dma("act", x[:, :, 0:W], XTv[:, :, t0:t0 + W], w=[x])
                for jj, j in enumerate(js):
                    p = pss[jj]

                    def mm(e):
                        ins = None
                        for c in range(KC):
                            ins = e.matmul(p[:, 0:W], lhsT=wset[jj][:, c, :], rhs=x[:, c, 0:W], start=(c == 0), stop=(c == KC - 1))
                        return ins
                    kb.op("pe", mm, r=[wset[jj], x], w=[p])
                    evac(j, t0, W, p)
            if not dbuf and gi + 1 < len(groups):
                load_group(gi + 1)


NVR = 7
NVL = 11


def build_p1(D=4096, TC=256, TL=4096, NBLK=4, NLB=4, dbg=False, upto=None):
    kb = KB()
    KC = D // 128
    NT = TC + TL
    NCK = NT // 128
    RW = NBLK * 128
    NCHR = 3 * NBLK + 6
    NCH = NCHR + 2 * NLB
    CK_C = TC // 128
    I = lambda name, shape, dt=F32: kb.dram(name, shape, dt, kind="ExternalInput")
    xT = I("xT", [D, TL]); cT = I("cT", [D, TC]); ptab_d = I("ptab", [128, KC, 64]); mods_d = I("mods", [128, KC, 4])
    win = I("win", [NCH, D, 128]); mu_d = I("mu", [128, NCHR])
    w2_d = I("w2", [2, 128, RW]); a2_d = I("a2", [2, 128, RW]); g2_d = I("g2", [2, 128, RW])
    rvec_d = I("rvec", [128, NBLK, NVR]); lnx_d = I("lnx", [1, 2 * RW])
    lvec_d = I("lvec", [128, NLB, NVL]); lwa_d = I("lwa", [2, NLB, 128, 128]); lwx_d = I("lwx", [2, NLB, 128, 128])
    cst_d = I("cst", [128, 20, 128]); cst2_d = I("cst2", [128, 15, 2, 128])
    ya = kb.dram("ya", [NT, RW], kind="ExternalOutput")
    yb = kb.dram("yb", [NLB * 128, NT], kind="ExternalOutput")
    hT = kb.dram("hT", [D, NT], BF16)
    P = kb.dram("P", [NCH, 128, NT])
    OPS = kb.dram("OPS", [2, 4, 128, NT])

    with kb.scope():
        ptab = kb.sb("ptab", [128, KC, 64]); mods = kb.sb("mods", [128, KC, 4])
        kb.dma("sp", ptab[:], ptab_d[:], w=[ptab]); kb.dma("sp", mods[:], mods_d[:], w=[mods])
        kb.op("dve", lambda e: e.tensor_scalar_add(out=mods[:, :, 1:2], in0=mods[:, :, 1:2], scalar1=1.0), r=[mods], w=[mods])
        kb.op("dve", lambda e: e.tensor_scalar_add(out=mods[:, :, 3:4], in0=mods[:, :, 3:4], scalar1=1.0), r=[mods], w=[mods])
        GA = min(8, KC // 2)
        xs = [kb.sb("xs%d" % i, [128, GA, 512]) for i in range(2)]
        hb = [kb.sb("hb%d" % i, [128, GA, 512], BF16) for i in range(2)]
        hTv = hT[:].rearrange("(c p) t -> p c t", p=128)
        it = 0
        for (t0, W, lat) in seq_tiles(TC, TL, 512):
            src, s0 = (xT, t0 - TC) if lat else (cT, t0)
            srcv = src[:].rearrange("(c p) t -> p c t", p=128)
            for g in range(KC // GA):
                x = xs[it % 2]; h = hb[it % 2]; it += 1
                c0 = g * GA
                kb.dma("sp", x[:, :, 0:W], srcv[:, c0:c0 + GA, s0:s0 + W], w=[x])
                if lat:
                    nr = W // 64; r0 = s0 // 64
                    xv = x[:, :, 0:W].rearrange("p c (r q) -> p c r q", q=64)
                    if c0 < KC // 2:
                        pin = ptab[:, c0:c0 + GA, r0:r0 + nr].unsqueeze(3).to_broadcast([128, GA, nr, 64])
                    else:
                        pin = ptab[:, c0:c0 + GA, 0:64].unsqueeze(2).to_broadcast([128, GA, nr, 64])
                    kb.op("pool", lambda e, xv=xv, pin=pin: e.tensor_tensor(out=xv, in0=xv, in1=pin, op=ALU.add), r=[x, ptab], w=[x])
                mo = 0 if lat else 2
                for cc in range(GA):
                    c = c0 + cc
                    kb.op("dve" if cc % 2 else "act",
                          (lambda e, cc=cc, c=c, x=x, h=h, W=W, mo=mo: e.tensor_scalar(out=h[:, cc, 0:W], in0=x[:, cc, 0:W], scalar1=mods[:, c, mo + 1:mo + 2], scalar2=mods[:, c, mo:mo + 1], op0=ALU.mult, op1=ALU.add))
                          if cc % 2 else
                          (lambda e, cc=cc, c=c, x=x, h=h, W=W, mo=mo: e.activation(out=h[:, cc, 0:W], in_=x[:, cc, 0:W], func=AF.Identity, scale=mods[:, c, mo + 1:mo + 2], bias=mods[:, c, mo:mo + 1])),
                          r=[x, mods], w=[h])
                kb.dma("sp", hTv[:, c0:c0 + GA, t0:t0 + W], h[:, :, 0:W], r=[h], store=True)

    if upto == "A":
        return kb.finish()
    oi = [0]

    def evacB(j, t0, W, p):
        o = obs[oi[0] % 4]; oi[0] += 1
        if oi[0] % 2:
            kb.op("act", lambda e: e.activation(out=o[:, 0:W], in_=p[:, 0:W], func=AF.Copy), r=[p], w=[o])
        else:
            kb.op("dve", lambda e: e.tensor_copy(out=o[:, 0:W], in_=p[:, 0:W]), r=[p], w=[o])
        kb.dma("sp", P[j][:, t0:t0 + W], o[:, 0:W], r=[o], store=True)
    with kb.scope():
        obs = [kb.sb("ob%d" % i, [128, 512]) for i in range(4)]
        gemm_fm(kb, win, NCH, D, hT, NT, seq_tiles(TC, TL, 512), evacB, GS=7)

    if upto == "B":
        return kb.finish()
    cst = kb.sb("cst", [128, 20, 128]); kb.dma("sp", cst[:], cst_d[:], w=[cst])
    IDN, ML_S, MU_S, ML_I, MU_I, BONE = (cst[:, i, :] for i in range(6))
    mu = kb.sb("mu", [128, NCHR]); kb.dma("sp", mu[:], mu_d[:], w=[mu])
    omm = kb.sb("omm", [128, NCHR]); hmu = kb.sb("hmu", [128, NCHR])
    kb.op("dve", lambda e: e.tensor_scalar(out=omm[:], in0=mu[:], scalar1=-1.0, scalar2=1.0, op0=ALU.mult, op1=ALU.add), r=[mu], w=[omm])
    kb.op("dve", lambda e: e.tensor_scalar_mul(out=hmu[:], in0=mu[:], scalar1=0.5), r=[mu], w=[hmu])
    TW = 512
    ctiles = seq_tiles(TC, TL, TW)

    def load_shift(j, t0, W, lat, dst, raw, eng="dve"):
        s_lo, s_hi = (TC, NT) if lat else (0, TC)
        lo = t0 - 1 if t0 > s_lo else t0
        hi = t0 + W + 1 if t0 + W < s_hi else t0 + W
        if lo == t0:
            kb.op("pool", lambda e: e.memset(raw[:, 0:1], 0.0), w=[raw])
        if hi == t0 + W:
            kb.op("pool", lambda e: e.memset(raw[:, W + 1:W + 2], 0.0), w=[raw])
        kb.dma("sp", raw[:, 1 - (t0 - lo):1 - (t0 - lo) + (hi - lo)], P[j][:, lo:hi], w=[raw])
        kb.op(eng, lambda e: e.tensor_tensor(out=dst[:, 0:W], in0=raw[:, 0:W], in1=raw[:, 2:W + 2], op=ALU.add), r=[raw], w=[dst])
        kb.op(eng, lambda e: e.tensor_scalar_mul(out=dst[:, 0:W], in0=dst[:, 0:W], scalar1=hmu[:, j:j + 1]), r=[dst, hmu], w=[dst])
        kb.op(eng, lambda e: e.scalar_tensor_tensor(out=dst[:, 0:W], in0=raw[:, 1:W + 1], scalar=omm[:, j:j + 1], in1=dst[:, 0:W], op0=ALU.mult, op1=ALU.add), r=[raw, dst, omm], w=[dst])

    LR = kb.dram("LR", [6, 128, NT])
    with kb.scope():
        raw = [kb.sb("c0raw%d" % i, [128, TW + 2]) for i in range(2)]
        dst = [kb.sb("c0dst%d" % i, [128, TW]) for i in range(2)]
        n = 0
        for q in range(6):
            j = 3 * NBLK + q
            fn = AF.Tanh if q < 2 else (AF.Identity if q < 4 else AF.Sigmoid)
            for (t0, W, lat) in ctiles:
                rw_, ds_ = raw[n % 2], dst[n % 2]; n += 1
                load_shift(j, t0, W, lat, ds_, rw_)
                kb.op("act", lambda e, ds_=ds_, W=W, fn=fn: e.activation(out=ds_[:, 0:W], in_=ds_[:, 0:W], func=fn), r=[ds_], w=[ds_])
                kb.dma("sp", LR[q][:, t0:t0 + W], ds_[:, 0:W], r=[ds_], store=True)

    if upto == "C0":
        return kb.finish()
    with kb.scope():
        rvec = kb.sb("rvec", [128, NBLK, NVR]); kb.dma("sp", rvec[:], rvec_d[:], w=[rvec])
        omka = kb.sb("omka", [128, NBLK])
        kb.op("dve", lambda e: e.tensor_scalar(out=omka[:], in0=rvec[:, :, 5], scalar1=-1.0, scalar2=1.0, op0=ALU.mult, op1=ALU.add), r=[rvec], w=[omka])
        lnw = kb.sb("lnw", [128, RW]); lnb = kb.sb("lnb", [128, RW])
        kb.dma("sp", lnw[:], lnx_d[:, 0:RW].partition_broadcast(128), w=[lnw])
        kb.dma("sp", lnb[:], lnx_d[:, RW:2 * RW].partition_broadcast(128), w=[lnb])
        w2 = kb.sb("w2", [128, 2, RW]); a2 = kb.sb("a2", [128, 2, RW]); g2 = kb.sb("g2", [128, 2, RW])
        kb.dma("sp", w2[:], w2_d[:].rearrange("d k n -> k d n"), w=[w2])
        kb.dma("sp", a2[:], a2_d[:].rearrange("d k n -> k d n"), w=[a2])
        kb.dma("sp", g2[:], g2_d[:].rearrange("d k n -> k d n"), w=[g2])
        hsel = kb.sb("hsel", [128, 2])
        kb.op("dve", lambda e: e.tensor_copy(out=hsel[:, 0:1], in_=cst[:, 5, 0:1]), r=[cst], w=[hsel])
        kb.op("dve", lambda e: e.tensor_copy(out=hsel[:, 1:2], in_=cst[:, 5, 64:65]), r=[cst], w=[hsel])
        raw = kb.sb("raw", [128, TW + 2])
        Rs = kb.sb("Rs", [128, TW]); Ks = kb.sb("Ks", [128, TW]); Vs = kb.sb("Vs", [128, TW]); KKn = kb.sb("KKn", [128, TW])
        RK = kb.sb("RK", [128, TW]); LD = kb.sb("LD", [128, TW]); CL = kb.sb("CL", [128, TW]); ICL = kb.sb("ICL", [128, TW])
        KD = kb.sb("KD", [128, TW]); T0 = kb.sb("T0", [128, TW]); T1 = kb.sb("T1", [128, TW])
        O4 = [kb.sb("O4_%d" % i, [128, TW]) for i in range(4)]
        ones = kb.sb("ones", [128, 128]); kb.op("pool", lambda e: e.memset(ones[:], 1.0), w=[ones])
        lt = [kb.sb("lt%d" % i, [128, 512]) for i in range(2)]
        Vtm = kb.sb("Vtm", [128, NCK, 128]); Yd = [kb.sb("Yd%d" % d, [128, NCK, 128]) for d in range(2)]
        sbon = kb.sb("sbon", [128, NCK, 2]); EL = [kb.sb("EL%d" % d, [128, NCK]) for d in range(2)]
        ST = [[kb.sb("ST%d_%d" % (d, hh), [128, 64]) for hh in range(2)] for d in range(2)]
        PADS = [[[[kb.sb("pad%d_%d_%d_%d" % (d, q, hh, par), [128, 128]) for par in range(2)] for hh in range(2)] for q in range(3)] for d in range(2)]
        for d in range(2):
            for q in range(3):
                for hh in range(2):
                    for par in range(2):
                        kb.op("pool", lambda e: e.memset(PADS[d][q][hh][par][:], 0.0), w=[PADS[d][q][hh][par]])
        cst2 = kb.sb("cst2", [128, 15, 2, 128]); kb.dma("sp", cst2[:], cst2_d[:], w=[cst2])
        banks = [kb.ps("bank%d" % i, [128, 512]) for i in range(8)]
        pW = banks[0]
        pV = banks[1]
        OPB = [[[kb.sb("opb%d_%d_%d" % (d, b, q), [128, 128]) for q in range(4)] for b in range(2)] for d in range(2)]
        BKb = [[kb.sb("bkb%d_%d" % (d, q), [128, 128]) for q in range(2)] for d in range(2)]
        BKt = [[kb.sb("bkt%d_%d" % (d, q), [128, 128]) for q in range(2)] for d in range(2)]
        M3 = lambda nm: [kb.sb("%s%d" % (nm, d), [128, 2, 128]) for d in range(2)]
        Mf = M3("Mf"); MTf = M3("MTf"); Mm = M3("Mm"); MTm = M3("MTm"); T1s = M3("T1s"); T2s = M3("T2s")
        Xb = [[kb.sb("X%d_%d" % (d, b), [128, 2, 128]) for b in range(2)] for d in range(2)]
        XTb = [[kb.sb("XT%d_%d" % (d, b), [128, 2, 128]) for b in range(2)] for d in range(2)]
        Mak = [kb.sb("Mak%d" % d, [128, 2, 128]) for d in range(2)]
        Nrb = [kb.sb("Nrb%d" % d, [128, 2, 128]) for d in range(2)]
        Nrk = [kb.sb("Nrk%d" % d, [128, 2, 128]) for d in range(2)]
        Ub = [[kb.sb("U%d_%d" % (d, b), [128, 128]) for b in range(2)] for d in range(2)]
        bk = lambda i, a, b: Tl(banks[i].t[:, a:b], banks[i].key)
        pP = [bk(2 + 3 * d, 0, 256) for d in range(2)]
        pM = [bk(2 + 3 * d, 256, 512) for d in range(2)]
        pPT = [bk(3 + 3 * d, 0, 256) for d in range(2)]
        pT2 = [bk(3 + 3 * d, 256, 512) for d in range(2)]
        pU = [bk(4 + 3 * d, 0, 128) for d in range(2)]
        pY = [bk(4 + 3 * d, 128, 256) for d in range(2)]
        pS = [bk(4 + 3 * d, 256, 384) for d in range(2)]
        st1 = kb.sb("st1", [128, NCK * 2]); st2 = kb.sb("st2", [128, NCK * 2])
        gl = [kb.sb("gl%d" % i, [128, 2, 512]) for i in range(2)]
        order = [list(range(NCK)), list(range(CK_C - 1, -1, -1)) + list(range(NCK - 1, CK_C - 1, -1))]

        for bi in range(NBLK):
            jr, jk, jv = bi, NBLK + bi, 2 * NBLK + bi
            cs = slice(bi * 128, (bi + 1) * 128)
            V_ = lambda i: rvec[:, bi, i:i + 1]
            for (t0, W, lat) in ctiles:
                ck0 = t0 // 128; nck = W // 128
                load_shift(jr, t0, W, lat, Rs, raw)
                load_shift(jk, t0, W, lat, Ks, raw)
                load_shift(jv, t0, W, lat, Vs, raw)
                for cc in range(nck):
                    kb.op("pe", lambda e, cc=cc: e.transpose(pV[:, 0:128], Vs[:, cc * 128:(cc + 1) * 128], IDN), r=[Vs, cst], w=[pV])
                    kb.op("act", lambda e, cc=cc: e.activation(out=Vtm[:, ck0 + cc, :], in_=pV[:, 0:128], func=AF.Copy), r=[pV], w=[Vtm])
                kb.op("dve", lambda e: e.tensor_scalar_mul(out=KKn[:, 0:W], in0=Ks[:, 0:W], scalar1=V_(4)), r=[Ks, rvec], w=[KKn])
                kb.op("act", lambda e: e.activation(out=T0[:, 0:W], in_=KKn[:, 0:W], func=AF.Square), r=[KKn], w=[T0])
                for s0 in range(0, W, 512):
                    sw = min(512, W - s0)
                    kb.op("pe", lambda e, s0=s0, sw=sw: e.matmul(pW[:, 0:sw], lhsT=BONE, rhs=T0[:, s0:s0 + sw], start=True, stop=True), r=[T0, cst], w=[pW])
                    kb.op("dve", lambda e, s0=s0, sw=sw: e.tensor_scalar_max(out=T1[:, s0:s0 + sw], in0=pW[:, 0:sw], scalar1=1e-24), r=[pW], w=[T1])
                kb.op("act", lambda e: e.activation(out=T1[:, 0:W], in_=T1[:, 0:W], func=AF.Sqrt), r=[T1], w=[T1])
                kb.op("dve", lambda e: e.reciprocal(out=T1[:, 0:W], in_=T1[:, 0:W]), r=[T1], w=[T1])
                kb.op("dve", lambda e: e.tensor_tensor(out=KKn[:, 0:W], in0=KKn[:, 0:W], in1=T1[:, 0:W], op=ALU.mult), r=[KKn, T1], w=[KKn])
                for d in range(2):
                    for s0 in range(0, W, 512):
                        sw = min(512, W - s0)
                        for (q, wts, dstT, biasv) in ((d, w2, LD, V_(d)), (2 + d, a2, ICL, V_(2 + d))):
                            l = lt[(s0 // 512 + q) % 2]
                            kb.dma("sp", l[:, 0:sw], LR[q][:, t0 + s0:t0 + s0 + sw], w=[l])
                            kb.op("pe", lambda e, l=l, wts=wts, sw=sw: e.matmul(pW[:, 0:sw], lhsT=wts[:, d, cs], rhs=l[:, 0:sw], start=True, stop=True), r=[l, wts], w=[pW])
                            kb.op("act", lambda e, dstT=dstT, biasv=biasv, s0=s0, sw=sw: e.activation(out=dstT[:, s0:s0 + sw], in_=pW[:, 0:sw], func=AF.Sigmoid, bias=biasv, scale=1.0), r=[pW, rvec], w=[dstT])
                    kb.op("dve", lambda e: e.tensor_scalar_mul(out=LD[:, 0:W], in0=LD[:, 0:W], scalar1=-0.6065306597126334), r=[LD], w=[LD])
                    kb.op("dve", lambda e: e.tensor_scalar(out=KD[:, 0:W], in0=ICL[:, 0:W], scalar1=V_(5), scalar2=omka[:, bi:bi + 1], op0=ALU.mult, op1=ALU.add), r=[ICL, rvec, omka], w=[KD])
                    kb.op("dve", lambda e: e.tensor_tensor(out=KD[:, 0:W], in0=KD[:, 0:W], in1=Ks[:, 0:W], op=ALU.mult), r=[KD, Ks], w=[KD])
                    if d == 0:
                        kb.op("pool", lambda e: e.tensor_tensor(out=RK[:, 0:W], in0=Rs[:, 0:W], in1=KD[:, 0:W], op=ALU.mult), r=[Rs, KD], w=[RK])
                    else:
                        kb.op("pool", lambda e: e.tensor_tensor(out=T0[:, 0:W], in0=Rs[:, 0:W], in1=KD[:, 0:W], op=ALU.mult), r=[Rs, KD], w=[T0])
                        kb.op("pool", lambda e: e.tensor_tensor(out=RK[:, 0:W], in0=RK[:, 0:W], in1=T0[:, 0:W], op=ALU.add), r=[RK, T0], w=[RK])
                    for cc in range(nck):
                        sl = slice(cc * 128, (cc + 1) * 128)
                        if d == 0:
                            kb.op("dve", lambda e, sl=sl: e.tensor_tensor_scan(out=CL[:, sl], data0=ones[:], data1=LD[:, sl], initial=0.0, op0=ALU.mult, op1=ALU.add), r=[LD, ones], w=[CL])
                        else:
                            kb.op("dve", lambda e, sl=sl: e.tensor_tensor_scan(out=CL[:, sl][:, ::-1], data0=ones[:], data1=LD[:, sl][:, ::-1], initial=0.0, op0=ALU.mult, op1=ALU.add), r=[LD, ones], w=[CL])
                    CLv = CL[:, 0:W].rearrange("p (c l) -> p c l", l=128)
                    last = CLv[:, :, 127] if d == 0 else CLv[:, :, 0]
                    kb.op("act", lambda e, last=last: e.activation(out=EL[d][:, ck0:ck0 + nck], in_=last, func=AF.Exp), r=[CL], w=[EL[d]])
                    kb.op("act", lambda e: e.activation(out=T0[:, 0:W], in_=CL[:, 0:W], func=AF.Exp), r=[CL], w=[T0])
                    kb.op("dve", lambda e: e.tensor_tensor(out=O4[1][:, 0:W], in0=Rs[:, 0:W], in1=T0[:, 0:W], op=ALU.mult), r=[Rs, T0], w=[O4[1]])
                    kb.op("dve", lambda e: e.tensor_tensor(out=T1[:, 0:W], in0=CL[:, 0:W], in1=LD[:, 0:W], op=ALU.subtract), r=[CL, LD], w=[T1])
                    kb.op("act", lambda e: e.activation(out=T1[:, 0:W], in_=T1[:, 0:W], func=AF.Exp), r=[T1], w=[T1])
                    kb.op("dve", lambda e: e.scalar_tensor_tensor(out=O4[0][:, 0:W], in0=KKn[:, 0:W], scalar=-1.0, in1=T1[:, 0:W], op0=ALU.mult, op1=ALU.mult), r=[KKn, T1], w=[O4[0]])
                    kb.op("act", lambda e: e.activation(out=T0[:, 0:W], in_=CL[:, 0:W], func=AF.Exp, scale=-1.0), r=[CL], w=[T0])
                    kb.op("pool", lambda e: e.tensor_tensor(out=T1[:, 0:W], in0=KKn[:, 0:W], in1=ICL[:, 0:W], op=ALU.mult), r=[KKn, ICL], w=[T1])
                    kb.op("dve", lambda e: e.tensor_tensor(out=O4[2][:, 0:W], in0=T1[:, 0:W], in1=T0[:, 0:W], op=ALU.mult), r=[T1, T0], w=[O4[2]])
                    kb.op("dve", lambda e: e.tensor_tensor(out=O4[3][:, 0:W], in0=KD[:, 0:W], in1=T0[:, 0:W], op=ALU.mult), r=[KD, T0], w=[O4[3]])
                    for q in range(4):
                        kb.dma("sp", OPS[d, q][:, t0:t0 + W], O4[q][:, 0:W], r=[O4[q]], store=True)
                kb.op("dve", lambda e: e.tensor_scalar_mul(out=RK[:, 0:W], in0=RK[:, 0:W], scalar1=V_(6)), r=[RK, rvec], w=[RK])
                for cc in range(nck):
                    kb.op("pe", lambda e, cc=cc: e.matmul(pV[:, 256 + 2 * cc:258 + 2 * cc], lhsT=RK[:, cc * 128:(cc + 1) * 128], rhs=hsel[:], start=True, stop=True), r=[RK, hsel], w=[pV])
                kb.op("dve", lambda e: e.tensor_copy(out=sbon[:, ck0:ck0 + nck, :], in_=pV[:, 256:256 + 2 * nck].rearrange("p (c h) -> p c h", h=2)), r=[pV], w=[sbon])
            kb.barrier()
            if upto == "C":
                break
            for d in range(2):
                for hh in range(2):
                    kb.op("pool", lambda e, d=d, hh=hh: e.memset(ST[d][hh][:], 0.0), w=[ST[d][hh]])
            for step in range(NCK):
                cks = [order[0][step], order[1][step]]
                ob = [OPB[d][step % 2] for d in range(2)]
                for d in range(2):
                    c = cks[d]
                    for q in range(4):
                        kb.dma("sp" if q % 2 else "act", ob[d][q][:], OPS[d, q][:, c * 128:(c + 1) * 128], w=[ob[d][q]])
                AHd = [ob[d][0] for d in range(2)]; RHd = [ob[d][1] for d in range(2)]; BHd = [ob[d][2] for d in range(2)]; KHd = [ob[d][3] for d in range(2)]
                mS = [(ML_S, MU_S), (MU_S, ML_S)]
                mI = [MU_I, ML_I]
                HS = (slice(0, 64), slice(64, 128))

                def mm2(e, out_t, lhs, rhs):
                    ins = None
                    for hh in range(2):
                        ins = e.matmul(out_t[:, hh * 128:(hh + 1) * 128], lhsT=lhs[hh][:], rhs=rhs[:], start=True, stop=True)
                    return ins
                HSs = (slice(0, 64), slice(64, 128))
                for d in range(2):
                    c = cks[d]
                    for q, oq in ((0, 0), (1, 2), (2, 3)):
                        for hh in range(2):
                            pd = PADS[d][q][hh][step % 2]
                            kb.dma("sp" if (q + hh) % 2 else "act", pd[HSs[hh], :], OPS[d, oq][HSs[hh], c * 128:(c + 1) * 128], w=[pd])
                AP_ = [[PADS[d][0][hh][step % 2] for hh in range(2)] for d in range(2)]; BP_ = [[PADS[d][1][hh][step % 2] for hh in range(2)] for d in range(2)]; KP_ = [[PADS[d][2][hh][step % 2] for hh in range(2)] for d in range(2)]

                def evm(eng, dst, src, mask):
                    kb.op(eng, lambda e: e.tensor_tensor(out=dst[:], in0=src[:].rearrange("p (h s) -> p h s", h=2), in1=mask.unsqueeze(1).to_broadcast([128, 2, 128]), op=ALU.mult), r=[src, cst], w=[dst])
                for d in range(2):
                    kb.op("pe", lambda e, d=d: mm2(e, pP[d], AP_[d], BHd[d]), r=AP_[d] + [BHd[d]], w=[pP[d]])
                    kb.op("pe", lambda e, d=d: mm2(e, pPT[d], BP_[d], AHd[d]), r=BP_[d] + [AHd[d]], w=[pPT[d]])
                    kb.op("act", lambda e, d=d: e.activation(out=Mf[d][:].rearrange("p h s -> p (h s)"), in_=pP[d][:], func=AF.Copy), r=[pP[d]], w=[Mf[d]])
                    kb.op("act", lambda e, d=d: e.activation(out=MTf[d][:].rearrange("p h s -> p (h s)"), in_=pPT[d][:], func=AF.Copy), r=[pPT[d]], w=[MTf[d]])
                    kb.op("pe", lambda e, d=d: mm2(e, pM[d], KP_[d], AHd[d]), r=KP_[d] + [AHd[d]], w=[pM[d]])
                    evm("dve", Mak[d], pM[d], mS[d][1])
                    c = cks[d]
                    for q, src in ((0, BHd[d]), (1, KHd[d])):
                        kb.op("dve", lambda e, q=q, src=src, d=d, c=c: e.tensor_scalar_mul(out=BKb[d][q][:], in0=src[:], scalar1=EL[d][:, c:c + 1]), r=[src, EL[d]], w=[BKb[d][q]])
                        kb.op("pe", lambda e, q=q, d=d: e.transpose(pT2[d][:, q * 128:(q + 1) * 128], BKb[d][q][:], IDN), r=[BKb[d][q], cst], w=[pT2[d]])
                        kb.op("act", lambda e, q=q, d=d: e.activation(out=BKt[d][q][:], in_=pT2[d][:, q * 128:(q + 1) * 128], func=AF.Copy), r=[pT2[d]], w=[BKt[d][q]])
                for d in range(2):
                    c = cks[d]

                    def rhs0(e, d=d, c=c):
                        ins = None
                        for hh in range(2):
                            e.matmul(pU[d][:, hh * 64:(hh + 1) * 64], lhsT=AHd[d][:], rhs=ST[d][hh][:], start=True, stop=False)
                            ins = e.matmul(pU[d][:, hh * 64:(hh + 1) * 64], lhsT=Mak[d][:, hh, :], rhs=Vtm[:, c, hh * 64:(hh + 1) * 64], start=False, stop=True)
                        return ins
                    kb.op("pe", rhs0, r=[AHd[d], ST[d][0], ST[d][1], Mak[d], Vtm], w=[pU[d]])
                    kb.op("act", lambda e, d=d: e.activation(out=Ub[d][0][:], in_=pU[d][:], func=AF.Copy), r=[pU[d]], w=[Ub[d][0]])
                    if dbg and step == 1 and d == 0 and bi == 0:
                        kb.dump(Ub[0][0], "d_U0"); kb.dump(Mak[0], "d_Mak"); kb.dump(AHd[0], "d_AH1")
                    kb.op("pe", lambda e, d=d: mm2(e, pM[d], BP_[d], RHd[d]), r=BP_[d] + [RHd[d]], w=[pM[d]])
                    evm("dve", Nrb[d], pM[d], mI[d])
                    kb.op("pe", lambda e, d=d: mm2(e, pM[d], KP_[d], RHd[d]), r=KP_[d] + [RHd[d]], w=[pM[d]])
                    evm("dve", Nrk[d], pM[d], mI[d])
                MKa = lambda d, l: cst2[:, (0 if d == 0 else 7) + l, :, :]
                MKb = lambda d, l: cst2[:, (7 if d == 0 else 0) + l, :, :]
                ID2 = cst2[:, 14, :, :]

                def mmh(e, out_t, lhs, rhs):
                    ins = None
                    for hh in range(2):
                        ins = e.matmul(out_t[:, hh * 128:(hh + 1) * 128], lhsT=lhs[:, hh, :], rhs=rhs[:, hh, :], start=True, stop=True)
                    return ins
                flat = lambda t: t[:].rearrange("p h s -> p (h s)")
                for d in range(2):
                    kb.op("pool", lambda e, d=d: e.tensor_tensor(out=Xb[d][0][:], in0=Mf[d][:], in1=MKa(d, 0), op=ALU.mult), r=[Mf[d], cst2], w=[Xb[d][0]])
                    kb.op("pool", lambda e, d=d: e.tensor_tensor(out=Xb[d][0][:], in0=Xb[d][0][:], in1=ID2, op=ALU.add), r=[Xb[d][0], cst2], w=[Xb[d][0]])
                    kb.op("pool", lambda e, d=d: e.tensor_tensor(out=XTb[d][0][:], in0=MTf[d][:], in1=MKb(d, 0), op=ALU.mult), r=[MTf[d], cst2], w=[XTb[d][0]])
                    kb.op("pool", lambda e, d=d: e.tensor_tensor(out=XTb[d][0][:], in0=XTb[d][0][:], in1=ID2, op=ALU.add), r=[XTb[d][0], cst2], w=[XTb[d][0]])
                for l in range(1, 7):
                    a, b = (l - 1) % 2, l % 2
                    for d in range(2):
                        kb.op("pool", lambda e, d=d, l=l: e.tensor_tensor(out=Mm[d][:], in0=Mf[d][:], in1=MKa(d, l), op=ALU.mult), r=[Mf[d], cst2], w=[Mm[d]])
                        kb.op("pool", lambda e, d=d, l=l: e.tensor_tensor(out=MTm[d][:], in0=MTf[d][:], in1=MKb(d, l), op=ALU.mult), r=[MTf[d], cst2], w=[MTm[d]])
                        kb.op("pe", lambda e, d=d, a=a: mmh(e, pP[d], MTm[d], Xb[d][a]), r=[MTm[d], Xb[d][a]], w=[pP[d]])
                        kb.op("pe", lambda e, d=d, a=a: mmh(e, pPT[d], Mm[d], XTb[d][a]), r=[Mm[d], XTb[d][a]], w=[pPT[d]])
                        kb.op("act", lambda e, d=d: e.activation(out=flat(T1s[d]), in_=pP[d][:], func=AF.Copy), r=[pP[d]], w=[T1s[d]])
                        kb.op("dve", lambda e, d=d: e.tensor_copy(out=flat(T2s[d]), in_=pPT[d][:]), r=[pPT[d]], w=[T2s[d]])
                    for d in range(2):
                        if l < 6:
                            kb.op("pe", lambda e, d=d, a=a: mmh(e, pM[d], XTb[d][a], T1s[d]), r=[XTb[d][a], T1s[d]], w=[pM[d]])
                            kb.op("dve", lambda e, d=d, a=a, b=b: e.tensor_tensor(out=flat(Xb[d][b]), in0=pM[d][:], in1=flat(Xb[d][a]), op=ALU.add), r=[pM[d], Xb[d][a]], w=[Xb[d][b]])
                        kb.op("pe", lambda e, d=d, a=a: mmh(e, pT2[d], Xb[d][a], T2s[d]), r=[Xb[d][a], T2s[d]], w=[pT2[d]])
                        kb.op("dve", lambda e, d=d, a=a, b=b: e.tensor_tensor(out=flat(XTb[d][b]), in0=pT2[d][:], in1=flat(XTb[d][a]), op=ALU.add), r=[pT2[d], XTb[d][a]], w=[XTb[d][b]])
                for d in range(2):
                    def app(e, d=d):
                        ins = None
                        for hh in range(2):
                            ins = e.matmul(pU[d][:, hh * 64:(hh + 1) * 64], lhsT=XTb[d][0][:, hh, :], rhs=Ub[d][0][:, hh * 64:(hh + 1) * 64], start=True, stop=True)
                        return ins
                    kb.op("pe", app, r=[XTb[d][0], Ub[d][0]], w=[pU[d]])
                    kb.op("act", lambda e, d=d: e.activation(out=Ub[d][1][:], in_=pU[d][:], func=AF.Copy), r=[pU[d]], w=[Ub[d][1]])
                UF = 1
                for d in range(2):
                    c = cks[d]
                    U = Ub[d][UF]

                    def ymm(e, d=d, c=c, U=U):
                        ins = None
                        for hh in range(2):
                            o = pY[d][:, hh * 64:(hh + 1) * 64]
                            e.matmul(o, lhsT=RHd[d][:], rhs=ST[d][hh][:], start=True, stop=False)
                            e.matmul(o, lhsT=Nrb[d][:, hh, :], rhs=U[:, hh * 64:(hh + 1) * 64], start=False, stop=False)
                            ins = e.matmul(o, lhsT=Nrk[d][:, hh, :], rhs=Vtm[:, c, hh * 64:(hh + 1) * 64], start=False, stop=True)
                        return ins
                    kb.op("pe", ymm, r=[RHd[d], ST[d][0], ST[d][1], Nrb[d], Nrk[d], U, Vtm], w=[pY[d]])
                    kb.op("act", lambda e, d=d, c=c: e.activation(out=Yd[d][:, c, :], in_=pY[d][:], func=AF.Copy), r=[pY[d]], w=[Yd[d]])
                    if dbg and step == 1 and d == 0 and bi == 0:
                        kb.dump(U, "d_U1"); kb.dump(Nrb[0], "d_Nrb"); kb.dump(Nrk[0], "d_Nrk"); kb.dump(RHd[0], "d_RH1")

                    def smm(e, d=d, c=c, U=U):
                        e.matmul(pS[d][:], lhsT=BKt[d][0][:], rhs=U[:], start=True, stop=False)
                        return e.matmul(pS[d][:], lhsT=BKt[d][1][:], rhs=Vtm[:, c, :], start=False, stop=True)
                    kb.op("pe", smm, r=[BKt[d][0], BKt[d][1], U, Vtm], w=[pS[d]])
                    for hh in range(2):
                        kb.op("dve", lambda e, d=d, c=c, hh=hh: e.scalar_tensor_tensor(out=ST[d][hh][HS[hh], :], in0=ST[d][hh][HS[hh], :], scalar=EL[d][HS[hh], c:c + 1], in1=pS[d][HS[hh], hh * 64:(hh + 1) * 64], op0=ALU.mult, op1=ALU.add), r=[ST[d][hh], EL[d], pS[d]], w=[ST[d][hh]])
                    if dbg and step == 0 and d == 0 and bi == 0:
                        kb.dump(BKt[0][0], "d_BKt0"); kb.dump(BKt[0][1], "d_BKt1"); kb.dump(EL[0], "d_EL"); kb.dump(U, "d_U"); kb.dump(BKb[0][0], "d_BKb0")
            if upto == "D":
                break
            if dbg:
                dbgo = kb.dram("dbgo", [2, NT, 128], kind="ExternalOutput")
                for d in range(2):
                    kb.dma("sp", dbgo[d].rearrange("(c p) n -> p c n", p=128), Yd[d][:], r=[Yd[d]], store=True)
                kb.barrier()
            Y = Yd[0]
            Yv = lambda: Y[:].rearrange("p c (h v) -> p (c h) v", v=64)
            NG = NCK * 2
            kb.op("dve", lambda e: e.tensor_tensor(out=Y[:], in0=Yd[0][:], in1=Yd[1][:], op=ALU.add), r=[Yd[0], Yd[1]], w=[Y])
            kb.op("dve", lambda e: e.tensor_reduce(out=st1[:], in_=Yv(), axis=AX.X, op=ALU.add), r=[Y], w=[st1])
            kb.op("dve", lambda e: e.tensor_scalar_mul(out=st1[:], in0=st1[:], scalar1=1.0 / 64), r=[st1], w=[st1])
            kb.op("dve", lambda e: e.tensor_tensor(out=Yv(), in0=Yv(), in1=st1[:].unsqueeze(2).to_broadcast([128, NG, 64]), op=ALU.subtract), r=[Y, st1], w=[Y])
            Y2 = Yd[1]
            kb.op("dve", lambda e: e.tensor_tensor(out=Y2[:], in0=Y[:], in1=Y[:], op=ALU.mult), r=[Y], w=[Y2])
            kb.op("dve", lambda e: e.tensor_reduce(out=st2[:], in_=Y2[:].rearrange("p c (h v) -> p (c h) v", v=64), axis=AX.X, op=ALU.add), r=[Y2], w=[st2])
            kb.op("dve", lambda e: e.tensor_scalar(out=st2[:], in0=st2[:], scalar1=1.0 / 64, scalar2=64e-5, op0=ALU.mult, op1=ALU.add), r=[st2], w=[st2])
            kb.op("act", lambda e: e.activation(out=st2[:], in_=st2[:], func=AF.Sqrt), r=[st2], w=[st2])
            kb.op("dve", lambda e: e.reciprocal(out=st2[:], in_=st2[:]), r=[st2], w=[st2])
            kb.op("dve", lambda e: e.tensor_tensor(out=Yv(), in0=Yv(), in1=st2[:].unsqueeze(2).to_broadcast([128, NG, 64]), op=ALU.mult), r=[Y, st2], w=[Y])
            kb.op("dve", lambda e: e.tensor_tensor(out=Y[:], in0=Y[:], in1=lnw[:, cs].unsqueeze(1).to_broadcast([128, NCK, 128]), op=ALU.mult), r=[Y, lnw], w=[Y])
            kb.op("dve", lambda e: e.tensor_tensor(out=Y[:], in0=Y[:], in1=lnb[:, cs].unsqueeze(1).to_broadcast([128, NCK, 128]), op=ALU.add), r=[Y, lnb], w=[Y])
            kb.op("dve", lambda e: e.tensor_tensor(out=Y2[:].rearrange("p c (h v) -> p (c h) v", v=64), in0=Vtm[:].rearrange("p c (h v) -> p (c h) v", v=64), in1=sbon[:].rearrange("p c h -> p (c h)").unsqueeze(2).to_broadcast([128, NG, 64]), op=ALU.mult), r=[Vtm, sbon], w=[Y2])
            kb.op("dve", lambda e: e.tensor_tensor(out=Y[:], in0=Y[:], in1=Y2[:], op=ALU.add), r=[Y, Y2], w=[Y])
            for c4 in range(0, NCK, 4):
                n4 = min(4, NCK - c4)
                g = gl[(c4 // 4) % 2]
                kb.dma("sp", g[:, :, 0:n4 * 128], LR[4:6].rearrange("q p t -> p q t")[:, :, c4 * 128:(c4 + n4) * 128], w=[g])

                def gmm(e, g=g, n4=n4):
                    ins = None
                    for cc in range(n4):
                        for kq in range(2):
                            ins = e.matmul(pW[:, cc * 128:(cc + 1) * 128], lhsT=g[:, kq, cc * 128:(cc + 1) * 128], rhs=g2[:, kq, cs], start=(kq == 0), stop=(kq == 1))
                    return ins
                kb.op("pe", gmm, r=[g, g2], w=[pW])
                kb.op("dve", lambda e, c4=c4, n4=n4: e.tensor_tensor(out=Y[:, c4:c4 + n4, :], in0=Y[:, c4:c4 + n4, :], in1=pW[:, 0:n4 * 128].rearrange("p (c n) -> p c n", n=128), op=ALU.mult), r=[Y, pW], w=[Y])
            kb.dma("sp", ya[:, cs].rearrange("(c p) n -> p c n", p=128), Y[:], r=[Y], store=True)
            kb.barrier()
    if upto in ("C", "D", "E"):
        return kb.finish()
    with kb.scope():
        lvec = kb.sb("lvec", [128, NLB, NVL]); kb.dma("sp", lvec[:], lvec_d[:], w=[lvec])
        c8 = kb.sb("c8", [128, NLB, 2])
        kb.op("act", lambda e: e.activation(out=c8[:], in_=lvec[:, :, 9:11], func=AF.Exp, scale=-1.0), r=[lvec], w=[c8])
        kb.op("act", lambda e: e.activation(out=c8[:], in_=c8[:], func=AF.Ln, bias=1.0), r=[c8], w=[c8])
        kb.op("dve", lambda e: e.tensor_scalar_mul(out=c8[:], in0=c8[:], scalar1=-8.0), r=[c8], w=[c8])
        wa = kb.sb("wa", [128, 2, NLB, 128]); wx = kb.sb("wx", [128, 2, NLB, 128])
        kb.dma("sp", wa[:], lwa_d[:].rearrange("d n c o -> c d n o"), w=[wa])
        kb.dma("sp", wx[:], lwx_d[:].rearrange("d n c o -> c d n o"), w=[wx])
        raw = kb.sb("lraw", [128, TW + 3]); XC = kb.sb("XC", [128, TW]); GB = kb.sb("GB", [128, TW])
        A_ = kb.sb("A_", [128, TW]); GI = kb.sb("GI", [128, TW]); U_ = kb.sb("U_", [128, TW]); H = kb.sb("H", [128, TW]); TT = kb.sb("TT", [128, TW])
        HF = kb.sb("HF", [128, NT]); hst = kb.sb("hst", [128, 1])
        pA = kb.ps("lpA", [128, 512]); pB = kb.ps("lpB", [128, 512])
        C2 = 0.7978845608028654 * 2.0
        ctx_t = [t for t in ctiles if not t[2]]; lat_t = [t for t in ctiles if t[2]]
        for lb in range(NLB):
            jx = NCHR + lb; jg = NCHR + NLB + lb
            L_ = lambda i: lvec[:, lb, i:i + 1]
            for d in range(2):
                tl = ctiles if d == 0 else ctx_t[::-1] + lat_t[::-1]
                first = True
                for (t0, W, lat) in tl:
                    s_lo, s_hi = (TC, NT) if lat else (0, TC)
                    lo = max(s_lo, t0 - 1); hi = min(s_hi, t0 + W + 2)
                    kb.op("pool", lambda e: e.memset(raw[:], 0.0), w=[raw])
                    kb.dma("sp", raw[:, 1 - (t0 - lo):1 - (t0 - lo) + (hi - lo)], P[jx][:, lo:hi], w=[raw])
                    kb.op("dve", lambda e: e.tensor_scalar(out=XC[:, 0:W], in0=raw[:, 0:W], scalar1=L_(0), scalar2=L_(4), op0=ALU.mult, op1=ALU.add), r=[raw, lvec], w=[XC])
                    for j in range(1, 4):
                        kb.op("dve", lambda e: e.scalar_tensor_tensor(out=XC[:, 0:W], in0=raw[:, j:j + W], scalar=L_(j), in1=XC[:, 0:W], op0=ALU.mult, op1=ALU.add), r=[raw, lvec, XC], w=[XC])
                    for s0 in range(0, W, 512):
                        sw = min(512, W - s0)
                        kb.op("pe", lambda e: e.matmul(pA[:, 0:sw], lhsT=wa[:, d, lb, :], rhs=XC[:, s0:s0 + sw], start=True, stop=True), r=[wa, XC], w=[pA])
                        kb.op("act", lambda e: e.activation(out=A_[:, s0:s0 + sw], in_=pA[:, 0:sw], func=AF.Sigmoid, bias=L_(5 + d), scale=1.0), r=[pA, lvec], w=[A_])
                        kb.op("pe", lambda e: e.matmul(pB[:, 0:sw], lhsT=wx[:, d, lb, :], rhs=XC[:, s0:s0 + sw], start=True, stop=True), r=[wx, XC], w=[pB])
                        kb.op("act", lambda e: e.activation(out=GI[:, s0:s0 + sw], in_=pB[:, 0:sw], func=AF.Sigmoid, bias=L_(7 + d), scale=1.0), r=[pB, lvec], w=[GI])
                    kb.op("act", lambda e: e.activation(out=A_[:, 0:W], in_=A_[:, 0:W], func=AF.Exp, scale=c8[:, lb, d:d + 1]), r=[A_, c8], w=[A_])
                    kb.op("dve", lambda e: e.tensor_tensor(out=U_[:, 0:W], in0=XC[:, 0:W], in1=GI[:, 0:W], op=ALU.mult), r=[XC, GI], w=[U_])
                    kb.op("pool", lambda e: e.tensor_tensor(out=TT[:, 0:W], in0=A_[:, 0:W], in1=A_[:, 0:W], op=ALU.mult), r=[A_], w=[TT])
                    kb.op("dve", lambda e: e.tensor_scalar(out=TT[:, 0:W], in0=TT[:, 0:W], scalar1=-1.0, scalar2=1.0, op0=ALU.mult, op1=ALU.add), r=[TT], w=[TT])
                    kb.op("act", lambda e: e.activation(out=TT[:, 0:W], in_=TT[:, 0:W], func=AF.Sqrt), r=[TT], w=[TT])
                    kb.op("dve", lambda e: e.tensor_tensor(out=U_[:, 0:W], in0=U_[:, 0:W], in1=TT[:, 0:W], op=ALU.mult), r=[U_, TT], w=[U_])
                    init = 0.0 if first else hst[:, 0:1]
                    rr = [A_, U_] + ([] if first else [hst])
                    if d == 0:
                        kb.op("dve", lambda e: e.tensor_tensor_scan(out=H[:, 0:W], data0=A_[:, 0:W], data1=U_[:, 0:W], initial=init, op0=ALU.mult, op1=ALU.add), r=rr, w=[H])
                        kb.op("dve", lambda e: e.tensor_copy(out=hst[:], in_=H[:, W - 1:W]), r=[H], w=[hst])
                        kb.op("pool", lambda e: e.tensor_copy(out=HF[:, t0:t0 + W], in_=H[:, 0:W]), r=[H], w=[HF])
                    else:
                        kb.op("dve", lambda e: e.tensor_tensor_scan(out=H[:, 0:W][:, ::-1], data0=A_[:, 0:W][:, ::-1], data1=U_[:, 0:W][:, ::-1], initial=init, op0=ALU.mult, op1=ALU.add), r=rr, w=[H])
                        kb.op("dve", lambda e: e.tensor_copy(out=hst[:], in_=H[:, 0:1]), r=[H], w=[hst])
                        kb.dma("sp", GB[:, 0:W], P[jg][:, t0:t0 + W], w=[GB])
                        kb.op("act", lambda e: e.activation(out=TT[:, 0:W], in_=GB[:, 0:W], func=AF.Square), r=[GB], w=[TT])
                        kb.op("dve", lambda e: e.tensor_scalar(out=TT[:, 0:W], in0=TT[:, 0:W], scalar1=C2 * 0.044715, scalar2=C2, op0=ALU.mult, op1=ALU.add), r=[TT], w=[TT])
                        kb.op("dve", lambda e: e.tensor_tensor(out=TT[:, 0:W], in0=TT[:, 0:W], in1=GB[:, 0:W], op=ALU.mult), r=[TT, GB], w=[TT])
                        kb.op("act", lambda e: e.activation(out=TT[:, 0:W], in_=TT[:, 0:W], func=AF.Sigmoid), r=[TT], w=[TT])
                        kb.op("dve", lambda e: e.tensor_tensor(out=GB[:, 0:W], in0=GB[:, 0:W], in1=TT[:, 0:W], op=ALU.mult), r=[GB, TT], w=[GB])
                        kb.op("dve", lambda e: e.tensor_tensor(out=H[:, 0:W], in0=H[:, 0:W], in1=HF[:, t0:t0 + W], op=ALU.add), r=[H, HF], w=[H])
                        kb.op("dve", lambda e: e.tensor_tensor(out=GB[:, 0:W], in0=GB[:, 0:W], in1=H[:, 0:W], op=ALU.mult), r=[GB, H], w=[GB])
                        kb.dma("sp", yb[lb * 128:(lb + 1) * 128, t0:t0 + W], GB[:, 0:W], r=[GB], store=True)
                    first = False
    nc = kb.finish()
    return nc


def sincos_tab(TL, D):
    quarter = D // 4
    omega = (10000.0 ** (-np.arange(quarter, dtype=np.float32) / quarter)).astype(np.float32)
    idx = np.arange(64, dtype=np.float32)
    ang = idx[:, None] * omega[None, :]
    blk = np.concatenate([np.sin(ang), np.cos(ang)], -1).astype(np.float32)
    return np.concatenate([blk, blk], -1).T.copy()


def _pm(v, KC):
    return np.ascontiguousarray(v.reshape(KC, 128).T)


def p1_core_inputs(b, g, x, ctx, mods0, prm, NBLK, NLB):
    D = x.shape[2]; KC = D // 128; TL = x.shape[1]
    RW = NBLK * 128; LW = NLB * 128
    AW = prm["w0"].shape[-1]; BW = prm["lam"].shape[-1]
    DL = prm["w2"].shape[1]; GLo = prm["g2"].shape[0]
    w_in = prm["w_in"]; mu_full = prm["mu"]
    base = 3 * AW
    chunks = []
    for q in range(3):
        for bi in range(NBLK):
            c0 = q * AW + g * RW + bi * 128
            chunks.append(np.arange(c0, c0 + 128))
    for q in range(4):
        chunks.append(np.arange(base + q * DL, base + (q + 1) * DL))
    for q in range(2):
        chunks.append(np.arange(base + 4 * DL + q * 128, base + 4 * DL + (q + 1) * 128))
    NCHR = len(chunks)
    rc = 3 * AW + 4 * DL + GLo
    for q in range(2):
        for lb in range(NLB):
            c0 = rc + q * BW + g * LW + lb * 128
            chunks.append(np.arange(c0, c0 + 128))
    NCH = len(chunks)
    win = np.zeros((NCH, D, 128), np.float32)
    mu = np.zeros((128, NCHR), np.float32)
    for j, cols in enumerate(chunks):
        win[j, :, :len(cols)] = w_in[:, cols]
        if j < NCHR:
            mu[:len(cols), j] = mu_full[cols]
    sl = slice(g * RW, (g + 1) * RW)
    w2 = np.zeros((2, 128, RW), np.float32); w2[:, :DL] = prm["w2"][:, :, sl]
    a2 = np.zeros((2, 128, RW), np.float32); a2[:, :DL] = prm["a2"][:, :, sl]
    g2 = np.ascontiguousarray(prm["g2"][:, sl].reshape(2, 128, RW))
    vecs = [prm["w0"][0], prm["w0"][1], prm["a0"][0], prm["a0"][1], prm["k_k"], prm["k_a"], prm["r_k"].reshape(-1)]
    rvec = np.stack([v[sl].reshape(NBLK, 128).T for v in vecs], -1).astype(np.float32)
    lnx = np.concatenate([prm["lnx_w"][sl], prm["lnx_b"][sl]])[None, :].astype(np.float32)
    ls = slice(g * LW, (g + 1) * LW)
    lv = [prm["conv_w"][i] for i in range(4)] + [prm["conv_b"], prm["ba"][0], prm["ba"][1], prm["bx"][0], prm["bx"][1], prm["lam"][0], prm["lam"][1]]
    lvec = np.stack([v[ls].reshape(NLB, 128).T for v in lv], -1).astype(np.float32)
    lwa = np.ascontiguousarray(prm["wa"][:, g * NLB:(g + 1) * NLB]); lwx = np.ascontiguousarray(prm["wx"][:, g * NLB:(g + 1) * NLB])
    m = mods0
    mods = np.stack([_pm(m[b, 0:D], KC), _pm(m[b, D:2 * D], KC), _pm(m[2, 0:D], KC), _pm(m[2, D:2 * D], KC)], -1).astype(np.float32)
    tab = sincos_tab(TL, D)
    ptab = np.ascontiguousarray(tab.reshape(KC, 128, 64).transpose(1, 0, 2))
    return {"xT": np.ascontiguousarray(x[b].T), "cT": np.ascontiguousarray(ctx[b].T), "ptab": ptab, "mods": mods,
            "win": win, "mu": mu, "w2": w2, "a2": a2, "g2": g2, "rvec": np.ascontiguousarray(rvec), "lnx": lnx,
            "lvec": np.ascontiguousarray(lvec), "lwa": lwa, "lwx": lwx, "cst": make_consts(), "cst2": make_consts2()}


ALPHA_DN = 4.0 ** 0.25
LN_EPS_ = 1e-5


def build_post(D=4096, KM=4096, segs=((1024, 0), (64, 1)), mode="proj", router=True, hnext=True):
    kb = KB()
    KC = D // 128
    NTK = sum(n for n, _ in segs)
    nsets = max(sset for _, sset in segs) + 1
    NBK = (NTK + 127) // 128
    tiles = []
    t = 0
    for si, (n, sset) in enumerate(segs):
        for t0 in range(0, n, 512):
            tiles.append((t + t0, min(512, n - t0), sset, si == 0, t0))
        t += n
    tinfo = {tl[0]: (i, tl) for i, tl in enumerate(tiles)}
    I = lambda name, shape, dt=F32: kb.dram(name, shape, dt, kind="ExternalInput")
    resT = I("resT", [D, NTK]); modp_d = I("modp", [128, KC, nsets, 3]); lnp_d = I("lnp", [128, KC, 2])
    NR0 = segs[0][0] // 64
    prow_d = I("prow", [128, KC // 2, NR0]); pcol_d = I("pcol", [128, KC // 2, 64])
    if mode == "proj":
        yT = I("yT", [KM, NTK]); wout = I("wout", [KC, KM, 128])
    else:
        y1T = I("y1T", [D, NTK]); y2T = I("y2T", [D, NTK]); g12_d = I("g12", [2, NTK])
    if router:
        rw_d = I("rw", [128, KC, 16]); rb_d = I("rb", [1, 16]); cst_d = I("cst", [128, 20, 128])
        Gout = kb.dram("G", [NBK * 128, 16], kind="ExternalOutput")
    res_out = kb.dram("res_out", [D, NTK], kind="ExternalOutput")
    if hnext:
        h_out = kb.dram("h_out", [D, NTK], BF16, kind="ExternalOutput")
    zT = kb.dram("zT", [D, NTK])
    modp = kb.sb("modp", [128, KC, nsets, 3]); kb.dma("sp", modp[:], modp_d[:], w=[modp])
    lnp = kb.sb("lnp", [128, KC, 2]); kb.dma("sp", lnp[:], lnp_d[:], w=[lnp])
    prow = kb.sb("prow", [128, KC // 2, NR0]); kb.dma("sp", prow[:], prow_d[:], w=[prow])
    pcol = kb.sb("pcol", [128, KC // 2, 64]); kb.dma("sp", pcol[:], pcol_d[:], w=[pcol])
    if hnext:
        kb.op("dve", lambda e: e.tensor_scalar_add(out=modp[:, :, :, 2:3], in0=modp[:, :, :, 2:3], scalar1=1.0), r=[modp], w=[modp])
    acc1 = kb.sb("acc1", [128, NTK]); acc2 = kb.sb("acc2", [128, NTK])
    kb.op("pool", lambda e: e.memset(acc1[:], 0.0), w=[acc1]); kb.op("pool", lambda e: e.memset(acc2[:], 0.0), w=[acc2])
    ones = kb.sb("ones", [128, 128]); kb.op("pool", lambda e: e.memset(ones[:], 1.0), w=[ones])
    rts = [kb.sb("rt%d" % i, [128, 512]) for i in range(3)]
    zts = [kb.sb("zt%d" % i, [128, 512]) for i in range(3)]
    sqs = [kb.sb("sq%d" % i, [128, 512]) for i in range(2)]
    cnt = [0]

    def zstage(j, t0, W, o_ap, o_tl):
        _, (tt0, _, sset, seg0, off) = tinfo[t0]
        k = cnt[0]; cnt[0] += 1
        rt = rts[k % 3]; zt = zts[k % 3]; sq = sqs[k % 2]
        kb.dma("sp", rt[:, 0:W], resT[j * 128:(j + 1) * 128, t0:t0 + W], w=[rt])
        if seg0:
            nr = W // 64; r0 = off // 64
            rv = rt[:, 0:W].rearrange("p (r q) -> p r q", q=64)
            if j < KC // 2:
                pin = prow[:, j, r0:r0 + nr].unsqueeze(2).to_broadcast([128, nr, 64])
            else:
                pin = pcol[:, j - KC // 2, :].unsqueeze(1).to_broadcast([128, nr, 64])
            kb.op("dve", lambda e: e.tensor_tensor(out=rv, in0=rv, in1=pin, op=ALU.add), r=[rt, prow, pcol], w=[rt])
        kb.op("act", lambda e: e.activation(out=rt[:, 0:W], in_=rt[:, 0:W], func=AF.Copy, scale=ALPHA_DN), r=[rt], w=[rt])
        kb.op("dve", lambda e: e.scalar_tensor_tensor(out=zt[:, 0:W], in0=o_ap, scalar=modp[:, j, sset, 0:1], in1=rt[:, 0:W], op0=ALU.mult, op1=ALU.add), r=[o_tl, modp, rt], w=[zt])
        kb.op("pool", lambda e: e.tensor_tensor(out=acc1[:, t0:t0 + W], in0=acc1[:, t0:t0 + W], in1=zt[:, 0:W], op=ALU.add), r=[acc1, zt], w=[acc1])
        kb.op("act", lambda e: e.activation(out=sq[:, 0:W], in_=zt[:, 0:W], func=AF.Square), r=[zt], w=[sq])
        kb.op("pool", lambda e: e.tensor_tensor(out=acc2[:, t0:t0 + W], in0=acc2[:, t0:t0 + W], in1=sq[:, 0:W], op=ALU.add), r=[acc2, sq], w=[acc2])
        kb.dma("sp", zT[j * 128:(j + 1) * 128, t0:t0 + W], zt[:, 0:W], r=[zt], store=True)

    gtiles = [(t0, W, True) for (t0, W, _, _, _) in tiles]
    if mode == "proj":
        yTb = kb.dram("yTb", [KM, NTK], BF16)
        KMC = KM // 128
        with kb.scope():
            GA = min(8, KMC)
            xs = [kb.sb("cxs%d" % i, [128, GA, 512]) for i in range(2)]
            hb = [kb.sb("chb%d" % i, [128, GA, 512], BF16) for i in range(2)]
            yv = yT[:].rearrange("(c p) t -> p c t", p=128); ybv = yTb[:].rearrange("(c p) t -> p c t", p=128)
            it = 0
            for (t0, W, _) in gtiles:
                for g in range(KMC // GA):
                    x = xs[it % 2]; h = hb[it % 2]; it += 1
                    kb.dma("sp", x[:, :, 0:W], yv[:, g * GA:(g + 1) * GA, t0:t0 + W], w=[x])
                    kb.op("act" if it % 2 else "dve", (lambda e: e.activation(out=h[:, :, 0:W], in_=x[:, :, 0:W], func=AF.Copy)) if it % 2 else (lambda e: e.tensor_copy(out=h[:, :, 0:W], in_=x[:, :, 0:W])), r=[x], w=[h])
                    kb.dma("sp", ybv[:, g * GA:(g + 1) * GA, t0:t0 + W], h[:, :, 0:W], r=[h], store=True)
        gemm_fm(kb, wout, KC, KM, yTb, NTK, gtiles, lambda j, t0, W, p: zstage(j, t0, W, p[:, 0:W], p), dbuf=True)
    else:
        with kb.scope():
            G1 = kb.sb("G1b", [128, NTK]); G2 = kb.sb("G2b", [128, NTK])
            kb.dma("sp", G1[:], g12_d[0:1, :].partition_broadcast(128), w=[G1])
            kb.dma("sp", G2[:], g12_d[1:2, :].partition_broadcast(128), w=[G2])
            y1s = [kb.sb("y1s%d" % i, [128, 512]) for i in range(2)]; y2s = [kb.sb("y2s%d" % i, [128, 512]) for i in range(2)]
            n = 0
            for (t0, W, _) in gtiles:
                for j in range(KC):
                    a = y1s[n % 2]; b = y2s[n % 2]; n += 1
                    kb.dma("sp", a[:, 0:W], y1T[j * 128:(j + 1) * 128, t0:t0 + W], w=[a])
                    kb.dma("act", b[:, 0:W], y2T[j * 128:(j + 1) * 128, t0:t0 + W], w=[b])
                    kb.op("dve", lambda e: e.tensor_tensor(out=a[:, 0:W], in0=a[:, 0:W], in1=G1[:, t0:t0 + W], op=ALU.mult), r=[a, G1], w=[a])
                    kb.op("pool", lambda e: e.tensor_tensor(out=b[:, 0:W], in0=b[:, 0:W], in1=G2[:, t0:t0 + W], op=ALU.mult), r=[b, G2], w=[b])
                    kb.op("dve", lambda e: e.tensor_tensor(out=a[:, 0:W], in0=a[:, 0:W], in1=b[:, 0:W], op=ALU.add), r=[a, b], w=[a])
                    zstage(j, t0, W, a[:, 0:W], a)
    kb.barrier()
    with kb.scope():
        pst = kb.ps("pst", [128, 512]); pr = kb.ps("pr", [128, 512]); ptr = kb.ps("ptr", [128, 512])
        mean = kb.sb("mean", [128, NTK]); rstd = kb.sb("rstd", [128, NTK]); tmp = kb.sb("tmpst", [128, 512])
        if router:
            rw = kb.sb("rw", [128, KC, 16]); kb.dma("sp", rw[:], rw_d[:], w=[rw])
            rbb = kb.sb("rbb", [128, 16]); kb.dma("sp", rbb[:], rb_d[:].partition_broadcast(128), w=[rbb])
            cst = kb.sb("cstp", [128, 128]); kb.dma("sp", cst[:], cst_d[:, 0, :], w=[cst])
            lg = kb.sb("lg", [16, 512])
            A = kb.sb("Aaff", [128, NBK, 16]); kb.op("pool", lambda e: e.memset(A[:], 0.0), w=[A])
        zl = [kb.sb("zl%d" % i, [128, 512]) for i in range(3)]
        hf = [kb.sb("hf%d" % i, [128, 512]) for i in range(2)]
        hbf = [kb.sb("hbf%d" % i, [128, 512], BF16) for i in range(2)]
        n = 0
        for (t0, W, sset, seg0, off) in tiles:
            kb.op("pe", lambda e: e.matmul(pst[:, 0:W], lhsT=ones[:], rhs=acc1[:, t0:t0 + W], start=True, stop=True), r=[ones, acc1], w=[pst])
            kb.op("act", lambda e: e.activation(out=mean[:, t0:t0 + W], in_=pst[:, 0:W], func=AF.Copy, scale=1.0 / D), r=[pst], w=[mean])
            kb.op("pe", lambda e: e.matmul(pst[:, 0:W], lhsT=ones[:], rhs=acc2[:, t0:t0 + W], start=True, stop=True), r=[ones, acc2], w=[pst])
            kb.op("dve", lambda e: e.tensor_tensor(out=tmp[:, 0:W], in0=mean[:, t0:t0 + W], in1=mean[:, t0:t0 + W], op=ALU.mult), r=[mean], w=[tmp])
            kb.op("dve", lambda e: e.scalar_tensor_tensor(out=rstd[:, t0:t0 + W], in0=pst[:, 0:W], scalar=1.0 / D, in1=tmp[:, 0:W], op0=ALU.mult, op1=ALU.subtract), r=[pst, tmp], w=[rstd])
            kb.op("dve", lambda e: e.tensor_scalar_add(out=rstd[:, t0:t0 + W], in0=rstd[:, t0:t0 + W], scalar1=LN_EPS_), r=[rstd], w=[rstd])
            kb.op("act", lambda e: e.activation(out=rstd[:, t0:t0 + W], in_=rstd[:, t0:t0 + W], func=AF.Sqrt), r=[rstd], w=[rstd])
            kb.op("dve", lambda e: e.reciprocal(out=rstd[:, t0:t0 + W], in_=rstd[:, t0:t0 + W]), r=[rstd], w=[rstd])
            for j in range(KC):
                z = zl[n % 3]; h = hf[n % 2]; hb_ = hbf[n % 2]; n += 1
                kb.dma("sp", z[:, 0:W], zT[j * 128:(j + 1) * 128, t0:t0 + W], w=[z])
                kb.op("dve", lambda e: e.tensor_tensor(out=z[:, 0:W], in0=z[:, 0:W], in1=mean[:, t0:t0 + W], op=ALU.subtract), r=[z, mean], w=[z])
                kb.op("pool", lambda e: e.tensor_tensor(out=z[:, 0:W], in0=z[:, 0:W], in1=rstd[:, t0:t0 + W], op=ALU.mult), r=[z, rstd], w=[z])
                kb.op("act", lambda e: e.activation(out=z[:, 0:W], in_=z[:, 0:W], func=AF.Identity, scale=lnp[:, j, 0:1], bias=lnp[:, j, 1:2]), r=[z, lnp], w=[z])
                kb.dma("sp", res_out[j * 128:(j + 1) * 128, t0:t0 + W], z[:, 0:W], r=[z], store=True)
                if hnext:
                    kb.op("dve", lambda e: e.tensor_scalar(out=h[:, 0:W], in0=z[:, 0:W], scalar1=modp[:, j, sset, 2:3], scalar2=modp[:, j, sset, 1:2], op0=ALU.mult, op1=ALU.add), r=[z, modp], w=[h])
                    kb.op("act", lambda e: e.activation(out=hb_[:, 0:W], in_=h[:, 0:W], func=AF.Copy), r=[h], w=[hb_])
                    kb.dma("act", h_out[j * 128:(j + 1) * 128, t0:t0 + W], hb_[:, 0:W], r=[hb_], store=True)
                    if router:
                        kb.op("pe", lambda e: e.matmul(pr[0:16, 0:W], lhsT=rw[:, j, :], rhs=h[:, 0:W], start=(j == 0), stop=(j == KC - 1)), r=[rw, h], w=[pr])
            if router:
                kb.op("dve", lambda e: e.tensor_copy(out=lg[:, 0:W], in_=pr[0:16, 0:W]), r=[pr], w=[lg])
                for b0 in range(0, W, 128):
                    bw = min(128, W - b0); blk = (t0 + b0) // 128
                    kb.op("pe", lambda e: e.transpose(ptr[0:bw, 0:16], lg[:, b0:b0 + bw], cst[0:16, 0:16]), r=[lg, cst], w=[ptr])
                    kb.op("act", lambda e: e.activation(out=A[0:bw, blk, :], in_=ptr[0:bw, 0:16], func=AF.Sigmoid), r=[ptr], w=[A])
        if router:
            T = lambda nm, shp: kb.sb(nm, shp)
            Bz = T("Bz", [128, NBK, 16]); gs = T("gs", [128, NBK, 4]); ps_ = T("pairs", [128, NBK, 4]); gm = T("gm", [128, NBK]); ing = T("ing", [128, NBK, 4])
            mb = T("mb", [128, NBK, 16]); mbias = T("mbias", [128, NBK, 4]); m1 = T("m1", [128, NBK]); s1 = T("s1", [128, NBK, 16]); s2 = T("s2", [128, NBK, 16]); gsum = T("gsum", [128, NBK])
            kb.op("dve", lambda e: e.tensor_tensor(out=Bz[:], in0=A[:], in1=rbb[:].unsqueeze(1).to_broadcast([128, NBK, 16]), op=ALU.add), r=[A, rbb], w=[Bz])
            B4 = Bz[:].rearrange("p b (g m) -> p b g m", m=4)
            first = True
            for i in range(4):
                for jx in range(i + 1, 4):
                    dst = gs if first else ps_
                    kb.op("dve", lambda e: e.tensor_tensor(out=dst[:], in0=B4[:, :, :, i], in1=B4[:, :, :, jx], op=ALU.add), r=[Bz], w=[dst])
                    if not first:
                        kb.op("dve", lambda e: e.tensor_tensor(out=gs[:], in0=gs[:], in1=ps_[:], op=ALU.max), r=[gs, ps_], w=[gs])
                    first = False
            kb.op("dve", lambda e: e.tensor_reduce(out=gm[:], in_=gs[:], axis=AX.X, op=ALU.max), r=[gs], w=[gm])
            kb.op("dve", lambda e: e.tensor_tensor(out=ing[:], in0=gs[:], in1=gm[:].unsqueeze(2).to_broadcast([128, NBK, 4]), op=ALU.is_equal), r=[gs, gm], w=[ing])
            kb.op("dve", lambda e: e.tensor_scalar(out=mbias[:], in0=ing[:], scalar1=1e30, scalar2=-1e30, op0=ALU.mult, op1=ALU.add), r=[ing], w=[mbias])
            M4 = mb[:].rearrange("p b (g m) -> p b g m", m=4)
            kb.op("dve", lambda e: e.tensor_tensor(out=M4, in0=B4, in1=ing[:].unsqueeze(3).to_broadcast([128, NBK, 4, 4]), op=ALU.mult), r=[Bz, ing], w=[mb])
            kb.op("dve", lambda e: e.tensor_tensor(out=M4, in0=M4, in1=mbias[:].unsqueeze(3).to_broadcast([128, NBK, 4, 4]), op=ALU.add), r=[mb, mbias], w=[mb])
            bc16 = lambda t_: t_[:].unsqueeze(2).to_broadcast([128, NBK, 16])
            kb.op("dve", lambda e: e.tensor_reduce(out=m1[:], in_=mb[:], axis=AX.X, op=ALU.max), r=[mb], w=[m1])
            kb.op("dve", lambda e: e.tensor_tensor(out=s1[:], in0=mb[:], in1=bc16(m1), op=ALU.is_equal), r=[mb, m1], w=[s1])
            kb.op("dve", lambda e: e.scalar_tensor_tensor(out=mb[:], in0=s1[:], scalar=-1e30, in1=mb[:], op0=ALU.mult, op1=ALU.add), r=[s1, mb], w=[mb])
            kb.op("dve", lambda e: e.tensor_reduce(out=m1[:], in_=mb[:], axis=AX.X, op=ALU.max), r=[mb], w=[m1])
            kb.op("dve", lambda e: e.tensor_tensor(out=s2[:], in0=mb[:], in1=bc16(m1), op=ALU.is_equal), r=[mb, m1], w=[s2])
            kb.op("dve", lambda e: e.tensor_tensor(out=s1[:], in0=s1[:], in1=s2[:], op=ALU.add), r=[s1, s2], w=[s1])
            kb.op("dve", lambda e: e.tensor_tensor(out=s1[:], in0=s1[:], in1=A[:], op=ALU.mult), r=[s1, A], w=[s1])
            kb.op("dve", lambda e: e.tensor_reduce(out=gsum[:], in_=s1[:], axis=AX.X, op=ALU.add), r=[s1], w=[gsum])
            kb.op("dve", lambda e: e.tensor_scalar_max(out=gsum[:], in0=gsum[:], scalar1=1e-30), r=[gsum], w=[gsum])
            kb.op("dve", lambda e: e.reciprocal(out=gsum[:], in_=gsum[:]), r=[gsum], w=[gsum])
            kb.op("dve", lambda e: e.tensor_tensor(out=s1[:], in0=s1[:], in1=bc16(gsum), op=ALU.mult), r=[s1, gsum], w=[s1])
            kb.dma("sp", Gout[:].rearrange("(b p) e -> p b e", p=128), s1[:], r=[s1], store=True)
    return kb.finish()


def build_ffn(D=4096, DE=1024, R=1536):
    kb = KB()
    I = lambda name, shape, dt=F32: kb.dram(name, shape, dt, kind="ExternalInput")
    XT = I("XT", [D, R], BF16)
    wgu = I("wgu", [2 * DE // 128, D, 128])
    wd = I("wd", [D // 128, DE, 128])
    YT = kb.dram("YT", [D, R], kind="ExternalOutput")
    HT = kb.dram("HT", [DE, R], BF16)
    tiles = [(t0, min(512, R - t0), True) for t0 in range(0, R, 512)]
    with kb.scope():
        sg = [kb.sb("sg%d" % i, [128, 512]) for i in range(2)]
        hb = [kb.sb("hbb%d" % i, [128, 512], BF16) for i in range(2)]

        def ev1(j, t0, W, p):
            q = j // 2
            if j % 2 == 0:
                kb.op("act", lambda e: e.activation(out=sg[q % 2][:, 0:W], in_=p[:, 0:W], func=AF.Silu), r=[p], w=[sg[q % 2]])
            else:
                kb.op("dve", lambda e: e.tensor_tensor(out=hb[q % 2][:, 0:W], in0=p[:, 0:W], in1=sg[q % 2][:, 0:W], op=ALU.mult), r=[p, sg[q % 2]], w=[hb[q % 2]])
                kb.dma("sp", HT[q * 128:(q + 1) * 128, t0:t0 + W], hb[q % 2][:, 0:W], r=[hb[q % 2]], store=True)
        gemm_fm(kb, wgu, 2 * DE // 128, D, XT, R, tiles, ev1, dbuf=True)
    with kb.scope():
        ob = [kb.sb("fob%d" % i, [128, 512]) for i in range(4)]
        n = [0]

        def ev2(j, t0, W, p):
            o = ob[n[0] % 4]; n[0] += 1
            if n[0] % 2:
                kb.op("act", lambda e: e.activation(out=o[:, 0:W], in_=p[:, 0:W], func=AF.Copy), r=[p], w=[o])
            else:
                kb.op("dve", lambda e: e.tensor_copy(out=o[:, 0:W], in_=p[:, 0:W]), r=[p], w=[o])
            kb.dma("sp", YT[j * 128:(j + 1) * 128, t0:t0 + W], o[:, 0:W], r=[o], store=True)
        gemm_fm(kb, wd, D // 128, DE, HT, R, tiles, ev2, dbuf=True)
    return kb.finish()


def build_p5(D=4096, TC=256, TL=4096, NB=2):
    kb = KB()
    NT = TC + TL
    NTB = NB * NT
    NCK = NT // 128
    CKC = TC // 128
    NCH = 13
    I = lambda name, shape, dt=F32: kb.dram(name, shape, dt, kind="ExternalInput")
    hT = I("hT", [D, NTB], BF16); win = I("win", [NCH, D, 128]); cst_d = I("cst", [128, 20, 128])
    gb_d = I("gbias", [1, 4]); nw_d = I("nw", [1, 1024])
    Y = kb.dram("Y", [NB * TL, 512], kind="ExternalOutput")
    P = kb.dram("P5P", [NCH, 128, NTB]); PB = kb.dram("P5PB", [4, 128, NTB], BF16)
    KTM = kb.dram("KTM", [NB * NCK, 128, 256], BF16); VTM = kb.dram("VTM", [NB * NCK, 128, 512], BF16)
    OTM = kb.dram("OTM", [NB * NCK, 128, 512]); HF = kb.dram("HFs", [NB * NCK, 128, 512])
    tiles = []
    for b in range(NB):
        tiles += [(b * NT + t0, W, lat) for (t0, W, lat) in seq_tiles(TC, TL, 512)]
    with kb.scope():
        obs = [kb.sb("ob%d" % i, [128, 512]) for i in range(4)]
        obb = [kb.sb("obb%d" % i, [128, 512], BF16) for i in range(2)]
        n = [0]

        def ev(j, t0, W, p):
            o = obs[n[0] % 4]; n[0] += 1
            kb.op("act" if n[0] % 2 else "dve", (lambda e: e.activation(out=o[:, 0:W], in_=p[:, 0:W], func=AF.Copy)) if n[0] % 2 else (lambda e: e.tensor_copy(out=o[:, 0:W], in_=p[:, 0:W])), r=[p], w=[o])
            kb.dma("sp", P[j][:, t0:t0 + W], o[:, 0:W], r=[o], store=True)
            if j < 4:
                ob_ = obb[n[0] % 2]
                kb.op("pool", lambda e: e.tensor_copy(out=ob_[:, 0:W], in_=o[:, 0:W]), r=[o], w=[ob_])
                kb.dma("sp", PB[j][:, t0:t0 + W], ob_[:, 0:W], r=[ob_], store=True)
        gemm_fm(kb, win, NCH, D, hT, NTB, tiles, ev, GS=7)
    cst = kb.sb("cst", [128, 20, 128]); kb.dma("sp", cst[:], cst_d[:], w=[cst])
    IDN, ML_I, MU_I = cst[:, 0, :], cst[:, 3, :], cst[:, 4, :]
    TRI = [MU_I, ML_I]
    capm = [kb.sb("cap%d" % d, [128, 128]) for d in range(2)]
    for d in range(2):
        kb.op("dve", lambda e: e.tensor_scalar(out=capm[d][:], in0=TRI[d], scalar1=2e30, scalar2=-1e30, op0=ALU.mult, op1=ALU.add), r=[cst], w=[capm[d]])
    ones = kb.sb("ones", [128, 128]); kb.op("pool", lambda e: e.memset(ones[:], 1.0), w=[ones])
    onesb = kb.sb("onesb", [128, 1], BF16); kb.op("pool", lambda e: e.memset(onesb[:], 1.0), w=[onesb])
    G = kb.sb("Gtm", [128, NB * NCK, 4])
    banks = [kb.ps("bank%d" % i, [128, 512]) for i in range(8)]
    bk = lambda i, a, b_: Tl(banks[i].t[:, a:b_], banks[i].key)
    with kb.scope():
        ld = [kb.sb("tld%d" % i, [128, 11, 128]) for i in range(2)]
        kt = [kb.sb("tkt%d" % i, [128, 256], BF16) for i in range(2)]
        vt = [kb.sb("tvt%d" % i, [128, 512], BF16) for i in range(2)]
        ot = [kb.sb("tot%d" % i, [128, 512]) for i in range(2)]
        for cg in range(NB * NCK):
            l = ld[cg % 2]; k_ = kt[cg % 2]; v_ = vt[cg % 2]; o_ = ot[cg % 2]
            is_lat = (cg % NCK) >= CKC
            kb.dma("sp", l[:], P[2:13].rearrange("j p t -> p j t")[:, :, cg * 128:(cg + 1) * 128], w=[l])
            pk = bk(0, 0, 256); pv = banks[1]; po = banks[2]; pg = bk(3, 0, 128)

            def tr(e, dst, j0, nj):
                ins = None
                for q in range(nj):
                    ins = e.transpose(dst[:, q * 128:(q + 1) * 128], l[:, j0 + q, :], IDN)
                return ins
            kb.op("pe", lambda e: tr(e, pk, 0, 2), r=[l, cst], w=[pk])
            kb.op("act", lambda e: e.activation(out=k_[:], in_=pk[:], func=AF.Copy), r=[pk], w=[k_])
            kb.dma("sp", KTM[cg], k_[:], r=[k_], store=True)
            kb.op("pe", lambda e: tr(e, pv, 2, 4), r=[l, cst], w=[pv])
            kb.op("dve", lambda e: e.tensor_copy(out=v_[:], in_=pv[:]), r=[pv], w=[v_])
            kb.dma("sp", VTM[cg], v_[:], r=[v_], store=True)
            if is_lat:
                kb.op("pe", lambda e: tr(e, po, 6, 4), r=[l, cst], w=[po])
                kb.op("act", lambda e: e.activation(out=o_[:], in_=po[:], func=AF.Sigmoid), r=[po], w=[o_])
                kb.dma("sp", OTM[cg], o_[:], r=[o_], store=True)
            kb.op("pe", lambda e: tr(e, pg, 10, 1), r=[l, cst], w=[pg])
            kb.op("dve", lambda e: e.tensor_copy(out=G[:, cg, :], in_=pg[:, 0:4]), r=[pg], w=[G])
    gbb = kb.sb("gbb", [128, 4]); kb.dma("sp", gbb[:], gb_d[:].partition_broadcast(128), w=[gbb])
    SC = kb.sb("SC", [128, NB * NCK, 4]); NLF = kb.sb("NLF", [128, NB * NCK, 4])
    kb.op("dve", lambda e: e.tensor_tensor(out=SC[:], in0=G[:], in1=gbb[:].unsqueeze(1).to_broadcast([128, NB * NCK, 4]), op=ALU.add), r=[G, gbb], w=[SC])
    kb.op("act", lambda e: e.activation(out=SC[:], in_=SC[:], func=AF.Tanh, scale=1.0 / 15.0), r=[SC], w=[SC])
    kb.op("dve", lambda e: e.tensor_scalar_mul(out=SC[:], in0=SC[:], scalar1=15.0), r=[SC], w=[SC])
    kb.op("act", lambda e: e.activation(out=NLF[:], in_=SC[:], func=AF.Exp, scale=-1.0), r=[SC], w=[NLF])
    kb.op("act", lambda e: e.activation(out=NLF[:], in_=NLF[:], func=AF.Ln, bias=1.0), r=[NLF], w=[NLF])
    nwb = kb.sb("nwb", [128, 1024]); kb.dma("sp", nwb[:], nw_d[:].partition_broadcast(128), w=[nwb])
    kb.barrier()
    C = kb.sb("Cst", [128, 2, 512]); Cb = kb.sb("Cbf", [128, 2, 512], BF16); nst = kb.sb("nst", [128, 2]); nb16 = kb.sb("nb16", [128, 2], BF16)
    qk = [kb.sb("qk%d" % i, [128, 4, 128], BF16) for i in range(2)]
    ktm = [kb.sb("ktm%d" % i, [128, 256], BF16) for i in range(2)]
    vtm = [kb.sb("vtm%d" % i, [128, 512], BF16) for i in range(2)]
    kw = kb.sb("kw", [128, 256], BF16)
    rows = kb.sb("rows", [1, 256]); cols = kb.sb("cols", [128, 2]); ew = kb.sb("ew", [128, 1]); eb = kb.sb("eb", [128, 1]); ebL = kb.sb("ebL", [128, 1])
    Dm = kb.sb("Dm", [128, 128]); SD = kb.sb("SD", [128, 128], BF16)
    tq = kb.sb("tq", [128, 512]); Hc = kb.sb("Hc", [128, 512]); hf = kb.sb("hfl", [128, 512]); ol = kb.sb("ol", [128, 512]); sqj = kb.sb("sqj", [128, 512])
    dn = kb.sb("dn", [128, 2]); st = kb.sb("stt", [128, 4])
    pR = bk(0, 0, 256); pCo = bk(0, 256, 258); pD = bk(1, 0, 128); pSc = bk(1, 128, 256)
    pN = banks[2]; pQ = banks[3]; pDen = bk(4, 0, 2); pNn = bk(4, 2, 4); pC0 = banks[5]; pC1 = banks[6]
    order = [list(range(NCK)), list(range(CKC - 1, -1, -1)) + list(range(NCK - 1, CKC - 1, -1))]
    it = 0
    for b in range(NB):
        for d in range(2):
            kb.op("pool", lambda e: e.memset(C[:], 0.0), w=[C]); kb.op("pool", lambda e: e.memset(Cb[:], 0.0), w=[Cb])
            kb.op("pool", lambda e: e.memset(nst[:], 0.0), w=[nst]); kb.op("pool", lambda e: e.memset(nb16[:], 0.0), w=[nb16])
            for c in order[d]:
                cg = b * NCK + c
                lat = c >= CKC
                q_ = qk[it % 2]; k_ = ktm[it % 2]; v_ = vtm[it % 2]; it += 1
                kb.dma("sp", q_[:], PB[:].rearrange("j p t -> p j t")[:, :, cg * 128:(cg + 1) * 128], w=[q_])
                kb.dma("act", k_[:], KTM[cg], w=[k_]); kb.dma("sp", v_[:], VTM[cg], w=[v_])
                igc = SC[:, cg, 2 * d:2 * d + 1]; nlf = NLF[:, cg, 2 * d + 1:2 * d + 2]

                def small(e):
                    e.matmul(pR[0:1, 0:128], lhsT=nlf, rhs=TRI[d], start=True, stop=True)
                    e.matmul(pR[0:1, 128:256], lhsT=igc, rhs=IDN, start=True, stop=False)
                    e.matmul(pR[0:1, 128:256], lhsT=nlf, rhs=TRI[d], start=False, stop=True)
                    e.matmul(pCo[:, 0:1], lhsT=TRI[d], rhs=nlf, start=True, stop=True)
                    return e.matmul(pCo[:, 1:2], lhsT=ones[:], rhs=nlf, start=True, stop=True)
                kb.op("pe", small, r=[NLF, SC, cst, ones], w=[pR])
                kb.op("act", lambda e: e.activation(out=rows[0:1, 0:128], in_=pR[0:1, 0:128], func=AF.Copy, scale=-1.0), r=[pR], w=[rows])
                kb.op("act", lambda e: e.activation(out=rows[0:1, 128:256], in_=pR[0:1, 128:256], func=AF.Copy), r=[pR], w=[rows])
                kb.op("dve", lambda e: e.tensor_copy(out=cols[:], in_=pCo[:, 0:2]), r=[pR], w=[cols])
                kb.op("dve", lambda e: e.scalar_tensor_tensor(out=ew[:], in0=cols[:, 0:1], scalar=igc, in1=cols[:, 1:2], op0=ALU.add, op1=ALU.subtract), r=[cols, SC], w=[ew])
                kb.op("act", lambda e: e.activation(out=ew[:], in_=ew[:], func=AF.Exp), r=[ew], w=[ew])
                kb.op("act", lambda e: e.activation(out=ebL[:], in_=cols[:, 1:2], func=AF.Exp, scale=-1.0), r=[cols], w=[ebL])
                if lat:
                    kb.op("act", lambda e: e.activation(out=eb[:], in_=cols[:, 0:1], func=AF.Exp, scale=-1.0), r=[cols], w=[eb])
                    kb.op("dve", lambda e: e.tensor_scalar_mul(out=eb[:], in0=eb[:], scalar1=1.0 / 16.0), r=[eb], w=[eb])

                    def dlog(e):
                        e.matmul(pD[:], lhsT=ones[0:1, :], rhs=rows[0:1, 0:128], start=True, stop=False)
                        return e.matmul(pD[:], lhsT=rows[0:1, 128:256], rhs=ones[0:1, :], start=False, stop=True)
                    kb.op("pe", dlog, r=[rows, ones], w=[pD])
                    kb.op("dve", lambda e: e.tensor_tensor(out=Dm[:], in0=pD[:], in1=capm[d][:], op=ALU.min), r=[pD, capm[d]], w=[Dm])
                    kb.op("act", lambda e: e.activation(out=Dm[:], in_=Dm[:], func=AF.Exp), r=[Dm], w=[Dm])

                    def scm(e):
                        e.matmul(pSc[:], lhsT=q_[:, 2, :], rhs=q_[:, 0, :], start=True, stop=False)
                        return e.matmul(pSc[:], lhsT=q_[:, 3, :], rhs=q_[:, 1, :], start=False, stop=True)
                    kb.op("pe", scm, r=[q_], w=[pD])
                    kb.op("dve", lambda e: e.scalar_tensor_tensor(out=SD[:], in0=pSc[:], scalar=1.0 / 16.0, in1=Dm[:], op0=ALU.mult, op1=ALU.mult), r=[pD, Dm], w=[SD])
                    kb.op("pe", lambda e: e.matmul(pN[:], lhsT=SD[:], rhs=v_[:], start=True, stop=True), r=[SD, v_], w=[pN])

                    def qc(e):
                        e.matmul(pQ[:], lhsT=q_[:, 0, :], rhs=Cb[:, 0, :], start=True, stop=False)
                        return e.matmul(pQ[:], lhsT=q_[:, 1, :], rhs=Cb[:, 1, :], start=False, stop=True)
                    kb.op("pe", qc, r=[q_, Cb], w=[pQ])

                    def den(e):
                        e.matmul(pDen[:, 0:1], lhsT=SD[:], rhs=onesb[:], start=True, stop=True)
                        e.matmul(pDen[:, 1:2], lhsT=q_[:, 0, :], rhs=nb16[:, 0:1], start=True, stop=False)
                        return e.matmul(pDen[:, 1:2], lhsT=q_[:, 1, :], rhs=nb16[:, 1:2], start=False, stop=True)
                    kb.op("pe", den, r=[SD, onesb, q_, nb16], w=[pDen])
                    kb.op("act", lambda e: e.activation(out=tq[:], in_=pQ[:], func=AF.Identity, scale=eb[:, 0:1]), r=[pQ, eb], w=[tq])
                    kb.op("dve", lambda e: e.tensor_tensor(out=Hc[:], in0=pN[:], in1=tq[:], op=ALU.add), r=[pN, tq], w=[Hc])
                    kb.op("dve", lambda e: e.tensor_copy(out=dn[:], in_=pDen[:, 0:2]), r=[pDen], w=[dn])
                    kb.op("dve", lambda e: e.scalar_tensor_tensor(out=dn[:, 0:1], in0=dn[:, 1:2], scalar=eb[:, 0:1], in1=dn[:, 0:1], op0=ALU.mult, op1=ALU.add), r=[dn, eb], w=[dn])
                    kb.op("dve", lambda e: e.tensor_scalar_mul(out=dn[:, 1:2], in0=dn[:, 0:1], scalar1=-1.0), r=[dn], w=[dn])
                    kb.op("dve", lambda e: e.tensor_tensor(out=dn[:, 0:1], in0=dn[:, 0:1], in1=dn[:, 1:2], op=ALU.max), r=[dn], w=[dn])
                    kb.op("dve", lambda e: e.tensor_scalar_max(out=dn[:, 0:1], in0=dn[:, 0:1], scalar1=1.0), r=[dn], w=[dn])
                    kb.op("dve", lambda e: e.reciprocal(out=dn[:, 0:1], in_=dn[:, 0:1]), r=[dn], w=[dn])
                    kb.op("dve", lambda e: e.tensor_scalar_mul(out=Hc[:], in0=Hc[:], scalar1=dn[:, 0:1]), r=[Hc, dn], w=[Hc])
                    if d == 0:
                        kb.dma("sp", HF[cg], Hc[:], r=[Hc], store=True)
                    else:
                        kb.dma("sp", hf[:], HF[cg], w=[hf]); kb.dma("act", ol[:], OTM[cg], w=[ol])
                        kb.op("dve", lambda e: e.tensor_tensor(out=Hc[:], in0=Hc[:], in1=hf[:], op=ALU.add), r=[Hc, hf], w=[Hc])
                        kb.op("dve", lambda e: e.tensor_reduce(out=st[:, 0:1], in_=Hc[:], axis=AX.X, op=ALU.add), r=[Hc], w=[st])
                        kb.op("dve", lambda e: e.tensor_scalar_mul(out=st[:, 0:1], in0=st[:, 0:1], scalar1=1.0 / 512), r=[st], w=[st])
                        kb.op("dve", lambda e: e.tensor_scalar_sub(out=Hc[:], in0=Hc[:], scalar1=st[:, 0:1]), r=[Hc, st], w=[Hc])
                        kb.op("dve", lambda e: e.tensor_tensor(out=sqj[:], in0=Hc[:], in1=Hc[:], op=ALU.mult), r=[Hc], w=[sqj])
                        kb.op("dve", lambda e: e.tensor_reduce(out=st[:, 1:2], in_=sqj[:], axis=AX.X, op=ALU.add), r=[sqj], w=[st])
                        kb.op("dve", lambda e: e.tensor_scalar(out=st[:, 1:2], in0=st[:, 1:2], scalar1=1.0 / 512, scalar2=1e-6, op0=ALU.mult, op1=ALU.add), r=[st], w=[st])
                        kb.op("act", lambda e: e.activation(out=st[:, 1:2], in_=st[:, 1:2], func=AF.Sqrt), r=[st], w=[st])
                        kb.op("dve", lambda e: e.reciprocal(out=st[:, 1:2], in_=st[:, 1:2]), r=[st], w=[st])
                        kb.op("dve", lambda e: e.scalar_tensor_tensor(out=Hc[:], in0=Hc[:], scalar=st[:, 1:2], in1=nwb[:, 0:512], op0=ALU.mult, op1=ALU.mult), r=[Hc, st, nwb], w=[Hc])
                        kb.op("dve", lambda e: e.tensor_tensor(out=Hc[:], in0=Hc[:], in1=nwb[:, 512:1024], op=ALU.add), r=[Hc, nwb], w=[Hc])
                        kb.op("dve", lambda e: e.tensor_tensor(out=Hc[:], in0=Hc[:], in1=ol[:], op=ALU.mult), r=[Hc, ol], w=[Hc])
                        r0 = b * TL + (c - CKC) * 128
                        kb.dma("sp", Y[r0:r0 + 128, :], Hc[:], r=[Hc], store=True)
                kb.op("dve", lambda e: e.tensor_scalar_mul(out=kw[:], in0=k_[:], scalar1=ew[:, 0:1]), r=[k_, ew], w=[kw])
                kb.op("pe", lambda e: e.matmul(pC0[:], lhsT=kw[:, 0:128], rhs=v_[:], start=True, stop=True), r=[kw, v_], w=[pC0])
                kb.op("pe", lambda e: e.matmul(pC1[:], lhsT=kw[:, 128:256], rhs=v_[:], start=True, stop=True), r=[kw, v_], w=[pC1])

                def nmm(e):
                    e.matmul(pNn[:, 0:1], lhsT=kw[:, 0:128], rhs=onesb[:], start=True, stop=True)
                    return e.matmul(pNn[:, 1:2], lhsT=kw[:, 128:256], rhs=onesb[:], start=True, stop=True)
                kb.op("pe", nmm, r=[kw, onesb], w=[pDen])
                kb.op("dve", lambda e: e.scalar_tensor_tensor(out=C[:, 0, :], in0=C[:, 0, :], scalar=ebL[:, 0:1], in1=pC0[:], op0=ALU.mult, op1=ALU.add), r=[C, ebL, pC0], w=[C])
                kb.op("dve", lambda e: e.scalar_tensor_tensor(out=C[:, 1, :], in0=C[:, 1, :], scalar=ebL[:, 0:1], in1=pC1[:], op0=ALU.mult, op1=ALU.add), r=[C, ebL, pC1], w=[C])
                kb.op("act", lambda e: e.activation(out=Cb[:], in_=C[:], func=AF.Copy), r=[C], w=[Cb])
                kb.op("dve", lambda e: e.scalar_tensor_tensor(out=nst[:], in0=nst[:], scalar=ebL[:, 0:1], in1=pNn[:, 0:2], op0=ALU.mult, op1=ALU.add), r=[nst, ebL, pDen], w=[nst])
                kb.op("dve", lambda e: e.tensor_copy(out=nb16[:], in_=nst[:]), r=[nst], w=[nb16])
            kb.barrier()
    return kb.finish()


def p5_core_inputs(hd, hT_full, prm):
    w = prm["w_in"]; D = w.shape[0]
    H = prm["ig_b"].shape[1]
    QW = H * 256; VW = H * 512; QO = QW + VW
    cols = []
    for q in range(2): cols.append(np.arange(hd * 256 + q * 128, hd * 256 + (q + 1) * 128))
    for q in range(2): cols.append(np.arange(QO + hd * 256 + q * 128, QO + hd * 256 + (q + 1) * 128))
    for q in range(4): cols.append(np.arange(QO + QW + hd * 512 + q * 128, QO + QW + hd * 512 + (q + 1) * 128))
    for q in range(4): cols.append(np.arange(QW + hd * 512 + q * 128, QW + hd * 512 + (q + 1) * 128))
    gbase = QO + QW + VW
    cols.append(np.array([gbase + d * 2 * H + io * H + hd for d in range(2) for io in range(2)]))
    win = np.zeros((13, D, 128), np.float32)
    for j, cc in enumerate(cols):
        win[j, :, :len(cc)] = w[:, cc]
    gb = np.array([[prm["ig_b"][0, hd], prm["fg_b"][0, hd], prm["ig_b"][1, hd], prm["fg_b"][1, hd]]], np.float32)
    nw = np.concatenate([prm["norm_w"][hd * 512:(hd + 1) * 512], prm["norm_b"][hd * 512:(hd + 1) * 512]])[None].astype(np.float32)
    return {"hT": hT_full, "win": win, "cst": make_consts(), "gbias": gb, "nw": nw}


_HOOK = None


def _hook(name, arr):
    if _HOOK is not None:
        _HOOK(name, arr)


def run_p1(x, ctx, mods0, prm):
    B, TL, D = x.shape
    TC = ctx.shape[1]
    AW = prm["w0"].shape[-1]; BW = prm["lam"].shape[-1]
    G = 8 // B
    NBLK = AW // 128 // G; NLB = BW // 128 // G
    nc = build_p1(D=D, TC=TC, TL=TL, NBLK=NBLK, NLB=NLB)
    cores = [(b, g) for b in range(B) for g in range(G)]
    maps = [p1_core_inputs(b, g, x, ctx, mods0, prm, NBLK, NLB) for (b, g) in cores]
    res = _run(nc, maps)
    y = np.zeros((B, TC + TL, AW + BW), np.float32)
    for i, (b, g) in enumerate(cores):
        y[b, :, g * NBLK * 128:(g + 1) * NBLK * 128] = res[i]["ya"]
        y[b, :, AW + g * NLB * 128:AW + (g + 1) * NLB * 128] = res[i]["yb"].T
    return y


def _modp(vsets, KC):
    return np.ascontiguousarray(np.stack([np.stack([_pm(v, KC) for v in vs], -1) for vs in vsets], 2).astype(np.float32))


def _chunk_cols(w):
    K_, N = w.shape
    return np.ascontiguousarray(w.reshape(K_, N // 128, 128).transpose(1, 0, 2))


def moe_capacity(G_all):
    cnt = int((G_all > 0).sum(0).max())
    return int(min(3072, max(1024, -(-cnt // 128) * 128)))


def run_moe(ffn_nc, HT_all, G_all, wg, wu, wd, R):
    D, N = HT_all.shape
    ex = np.argsort(-G_all, axis=1, kind="stable")[:, :2]
    ex.sort(axis=1)
    gv = np.take_along_axis(G_all, ex, 1).astype(np.float32)
    items = []
    for e in range(G_all.shape[1]):
        idx = np.nonzero((ex == e).any(1))[0]
        for s0 in range(0, len(idx), R):
            items.append((e, idx[s0:s0 + R]))
    y1T = np.zeros((D, N), np.float32); y2T = np.zeros((D, N), np.float32)
    cache = {}

    def wl(e):
        if e not in cache:
            cache.clear()
            a = np.empty((2 * wg.shape[2] // 128, D, 128), np.float32)
            a[0::2] = _chunk_cols(wg[e]); a[1::2] = _chunk_cols(wu[e])
            cache[e] = (a, _chunk_cols(wd[e]))
        return cache[e]
    for l0 in range(0, len(items), 8):
        batch = items[l0:l0 + 8]
        full = batch + [(batch[0][0], np.zeros((0,), np.int64))] * (8 - len(batch))
        maps = []
        for (e, idx) in full:
            XT = np.zeros((D, R), HT_all.dtype)
            XT[:, :len(idx)] = HT_all[:, idx]
            a, b_ = wl(e)
            maps.append({"XT": XT, "wgu": a, "wd": b_})
        res = _run(ffn_nc, maps)
        for (e, idx), r in zip(batch, res):
            YT = r["YT"][:, :len(idx)]
            m0 = ex[idx, 0] == e
            y1T[:, idx[m0]] = YT[:, m0]
            y2T[:, idx[~m0]] = YT[:, ~m0]
    return y1T, y2T, np.ascontiguousarray(gv.T)


def kernel(x, c, ctx, c_ctx, ada_w, ada_b, norm_g, norm_b,
           ev_w_in, ev_w_out, rwkv_mu, rwkv_w0, rwkv_w2, rwkv_a0, rwkv_a2, rwkv_g2,
           rwkv_k_k, rwkv_k_a, rwkv_r_k, rwkv_lnx_w, rwkv_lnx_b,
           lru_conv_w, lru_conv_b, lru_wa, lru_ba, lru_wx, lru_bx, lru_lam,
           od_w_in, od_w_out, mlstm_ig_b, mlstm_fg_b, mlstm_norm_w, mlstm_norm_b,
           router_w, router_b, moe_w_gate, moe_w_up, moe_w_down):
    f = lambda a: np.asarray(a, dtype=np.float32)
    x = f(x); ctx = f(ctx)
    B, TL, D = x.shape
    TC = ctx.shape[1]
    KC = D // 128
    NQ = 8 // B
    LT = TL // NQ; CT = TC // NQ
    cores = [(b, q) for b in range(B) for q in range(NQ)]
    mods = run_p0(f(c), f(c_ctx), f(ada_w), f(ada_b))
    _hook("mods", mods)
    M = lambda l, v, k: mods[l, v, k * D:(k + 1) * D]
    prm = dict(w_in=f(ev_w_in[0]), mu=f(rwkv_mu[0]), w0=f(rwkv_w0[0]), w2=f(rwkv_w2[0]), a0=f(rwkv_a0[0]), a2=f(rwkv_a2[0]),
               g2=f(rwkv_g2[0]), k_k=f(rwkv_k_k[0]), k_a=f(rwkv_k_a[0]), r_k=f(rwkv_r_k[0]), lnx_w=f(rwkv_lnx_w[0]), lnx_b=f(rwkv_lnx_b[0]),
               conv_w=f(lru_conv_w[0]), conv_b=f(lru_conv_b[0]), wa=f(lru_wa[0]), ba=f(lru_ba[0]), wx=f(lru_wx[0]), bx=f(lru_bx[0]), lam=f(lru_lam[0]))
    y0 = run_p1(x, ctx, mods[0], prm)
    _hook("y0", y0)
    tab = sincos_tab(TL, D)
    rw_l = np.ascontiguousarray(f(router_w).reshape(KC, 128, 16).transpose(1, 0, 2)); rb_l = f(router_b)[None]
    cstc = make_consts()
    zrow = np.zeros((128, KC // 2, LT // 64), np.float32); zcol = np.zeros((128, KC // 2, 64), np.float32)
    ffn_cache = {}

    def get_ffn(R):
        if R not in ffn_cache:
            ffn_cache[R] = build_ffn(D=D, DE=moe_w_gate.shape[3], R=R)
        return ffn_cache[R]

    nc_a = build_post(D=D, KM=y0.shape[2], segs=((LT, 0), (CT, 1)), mode="proj", router=True, hnext=True)
    wout0 = _chunk_cols(f(ev_w_out[0]))
    lnp = lambda l, i: np.ascontiguousarray(np.stack([_pm(f(norm_g[l, i]), KC), _pm(f(norm_b[l, i]), KC)], -1))
    maps = []
    for (b, q) in cores:
        ls = slice(q * LT, (q + 1) * LT); cs_ = slice(q * CT, (q + 1) * CT)
        maps.append({"resT": np.ascontiguousarray(np.concatenate([x[b, ls], ctx[b, cs_]], 0).T),
                     "yT": np.ascontiguousarray(np.concatenate([y0[b, TC + q * LT:TC + (q + 1) * LT], y0[b, cs_]], 0).T),
                     "modp": _modp([(M(0, b, 2), M(0, b, 3), M(0, b, 4)), (M(0, 2, 2), M(0, 2, 3), M(0, 2, 4))], KC),
                     "lnp": lnp(0, 0), "wout": wout0, "rw": rw_l, "rb": rb_l, "cst": cstc,
                     "prow": np.ascontiguousarray(tab[:D // 2, q * (LT // 64):(q + 1) * (LT // 64)].reshape(KC // 2, 128, LT // 64).transpose(1, 0, 2)),
                     "pcol": np.ascontiguousarray(tab[D // 2:, :].reshape(KC // 2, 128, 64).transpose(1, 0, 2))})
    ra = _run(nc_a, maps)
    del maps, y0
    NTK = LT + CT
    resA = [r["res_out"] for r in ra]
    HT_all = np.concatenate([r["h_out"] for r in ra], 1)
    G_all = np.concatenate([r["G"][:NTK] for r in ra], 0)
    _hook("resA0", resA); _hook("G0", G_all)
    R_CAP = moe_capacity(G_all)
    y1T, y2T, g12 = run_moe(get_ffn(R_CAP), HT_all, G_all, f(moe_w_gate[0]), f(moe_w_up[0]), f(moe_w_down[0]), R_CAP)
    _hook("moe0", (y1T, y2T, g12))
    nc_b = build_post(D=D, segs=((LT, 0), (CT, 1)), mode="comb", router=False, hnext=True)
    maps = []
    for i, (b, q) in enumerate(cores):
        sl = slice(i * NTK, (i + 1) * NTK)
        maps.append({"resT": resA[i], "y1T": np.ascontiguousarray(y1T[:, sl]), "y2T": np.ascontiguousarray(y2T[:, sl]), "g12": np.ascontiguousarray(g12[:, sl]),
                     "modp": _modp([(M(0, b, 5), M(1, b, 0), M(1, b, 1)), (M(0, 2, 5), M(1, 2, 0), M(1, 2, 1))], KC),
                     "lnp": lnp(0, 1), "prow": zrow, "pcol": zcol})
    rb = _run(nc_b, maps)
    del maps, y1T, y2T
    resB = [r["res_out"] for r in rb]
    _hook("resB0", resB)
    NT = TC + TL
    hT_full = np.zeros((D, B * NT), rb[0]["h_out"].dtype)
    for i, (b, q) in enumerate(cores):
        h = rb[i]["h_out"]
        hT_full[:, b * NT + TC + q * LT:b * NT + TC + (q + 1) * LT] = h[:, 0:LT]
        hT_full[:, b * NT + q * CT:b * NT + (q + 1) * CT] = h[:, LT:LT + CT]
    prm5 = dict(w_in=f(od_w_in[0]), ig_b=f(mlstm_ig_b[0]), fg_b=f(mlstm_fg_b[0]), norm_w=f(mlstm_norm_w[0]), norm_b=f(mlstm_norm_b[0]))
    nc5 = build_p5(D=D, TC=TC, TL=TL, NB=B)
    r5 = _run(nc5, [p5_core_inputs(hd, hT_full, prm5) for hd in range(8)])
    y1 = np.zeros((B, TL, 8 * 512), np.float32)
    for hd in range(8):
        y1[:, :, hd * 512:(hd + 1) * 512] = r5[hd]["Y"].reshape(B, TL, 512)
    del hT_full, r5
    _hook("y1", y1)
    nc_c = build_post(D=D, KM=y1.shape[2], segs=((LT, 0),), mode="proj", router=True, hnext=True)
    wout1 = _chunk_cols(f(od_w_out[0]))
    maps = []
    for i, (b, q) in enumerate(cores):
        maps.append({"resT": np.ascontiguousarray(resB[i][:, 0:LT]), "yT": np.ascontiguousarray(y1[b, q * LT:(q + 1) * LT].T),
                     "modp": _modp([(M(1, b, 2), M(1, b, 3), M(1, b, 4))], KC), "lnp": lnp(1, 0), "wout": wout1,
                     "rw": rw_l, "rb": rb_l, "cst": cstc, "prow": zrow, "pcol": zcol})
    rc = _run(nc_c, maps)
    del maps, y1
    resC = [r["res_out"] for r in rc]
    HT_all = np.concatenate([r["h_out"] for r in rc], 1)
    G_all = np.concatenate([r["G"][:LT] for r in rc], 0)
    _hook("resC1", resC)
    R_CAP = moe_capacity(G_all)
    y1T, y2T, g12 = run_moe(get_ffn(R_CAP), HT_all, G_all, f(moe_w_gate[1]), f(moe_w_up[1]), f(moe_w_down[1]), R_CAP)
    nc_d = build_post(D=D, segs=((LT, 0),), mode="comb", router=False, hnext=False)
    maps = []
    for i, (b, q) in enumerate(cores):
        sl = slice(i * LT, (i + 1) * LT)
        maps.append({"resT": resC[i], "y1T": np.ascontiguousarray(y1T[:, sl]), "y2T": np.ascontiguousarray(y2T[:, sl]), "g12": np.ascontiguousarray(g12[:, sl]),
                     "modp": _modp([(M(1, b, 5), M(1, b, 5), M(1, b, 5))], KC), "lnp": lnp(1, 1), "prow": zrow, "pcol": zcol})
    rd = _run(nc_d, maps)
    out = np.zeros((B, TL, D), np.float32)
    for i, (b, q) in enumerate(cores):
        out[b, q * LT:(q + 1) * LT] = rd[i]["res_out"].T
    return out
```
